# Optimizing a Trainium2 kernel written in Bass

```python
import jax, jax.numpy as jnp
from jax import lax
import numpy as np

D_MODEL = 1024
BATCH = 8
SEQ = 4096
DEPTH = 2

HEAD_DIM = 64
ATTN_WIDTH = D_MODEL // 2
ATTN_HEADS = ATTN_WIDTH // HEAD_DIM
POOL_WIDTH = D_MODEL - ATTN_WIDTH
POOL_WINDOWS = (2, 4, 8, 16)
POOL_GROUPS = len(POOL_WINDOWS)
POOL_GROUP_DIM = POOL_WIDTH // POOL_GROUPS
MIX_WIDTH = ATTN_WIDTH + POOL_WIDTH
IN_WIDTH = 3 * ATTN_WIDTH + POOL_WIDTH
MOBA_BLOCK = 256
MOBA_TOP_K = 3
Q_CHUNK = 32
ROPE_THETA = 500000.0
ROT_DIM = HEAD_DIM // 4
D_FF = ((8 * D_MODEL // 3 + 255) // 256) * 256
CONV_WIDTH = 3
NORM_EPS = 1e-6
NEG_INF = -1e30

kernel_name = "hybrid_moba_pool_convffn_adaln"


def rms_norm(x, gain):
    xf = x.astype(jnp.float32)
    y = xf * lax.rsqrt(jnp.mean(xf * xf, axis=-1, keepdims=True) + NORM_EPS)
    return (y * gain.astype(jnp.float32)).astype(x.dtype)


def partial_rotary(x, cos, sin):
    half = ROT_DIM // 2
    x1 = x[..., :half]
    x2 = x[..., half:ROT_DIM]
    return jnp.concatenate([x1 * cos - x2 * sin, x2 * cos + x1 * sin, x[..., ROT_DIM:]], axis=-1)


def moba_attention(q, k, v):
    b, h, s, d = q.shape
    nb = -(-s // MOBA_BLOCK)
    pad = nb * MOBA_BLOCK - s
    kb = jnp.pad(k, ((0, 0), (0, 0), (0, pad), (0, 0))).reshape(b, h, nb, MOBA_BLOCK, d)
    vb = jnp.pad(v, ((0, 0), (0, 0), (0, pad), (0, 0))).reshape(b, h, nb, MOBA_BLOCK, d)
    kmean = jnp.mean(kb.astype(jnp.float32), axis=3)
    pos = jnp.arange(s)
    qblk = pos // MOBA_BLOCK
    gate = jnp.einsum('bhsd,bhnd->bhsn', q.astype(jnp.float32), kmean)
    past = jnp.arange(nb)[None, :] < qblk[:, None]
    gate = jnp.where(past, gate, NEG_INF)
    k_sel = min(MOBA_TOP_K, nb)
    _, top_idx = lax.top_k(gate, k_sel)
    top_valid = top_idx < qblk[:, None]
    own = jnp.broadcast_to(qblk[None, None, :, None], (b, h, s, 1))
    sel = jnp.concatenate([top_idx.astype(jnp.int32), own.astype(jnp.int32)], axis=-1)
    slot_valid = jnp.concatenate([top_valid, jnp.ones((b, h, s, 1), dtype=bool)], axis=-1)
    nc = s // Q_CHUNK

    def to_chunks(a):
        return jnp.moveaxis(a.reshape(b, h, nc, Q_CHUNK, *a.shape[3:]), 2, 0)

    bi = jnp.arange(b)[:, None, None, None]
    hi = jnp.arange(h)[None, :, None, None]
    offs = jnp.arange(MOBA_BLOCK)
    scale = d ** -0.5

    def chunk_fn(args):
        qc, selc, validc, tc = args
        kg = kb[bi, hi, selc]
        vg = vb[bi, hi, selc]
        sc = jnp.einsum('bhcd,bhcnkd->bhcnk', qc, kg).astype(jnp.float32) * scale
        kpos = selc[..., None] * MOBA_BLOCK + offs
        mask = validc[..., None] & (kpos <= tc[None, None, :, None, None])
        sc = jnp.where(mask, sc, NEG_INF)
        p = jax.nn.softmax(sc.reshape(b, h, Q_CHUNK, -1), axis=-1).reshape(sc.shape)
        return jnp.einsum('bhcnk,bhcnkd->bhcd', p.astype(vg.dtype), vg)

    out = lax.map(chunk_fn, (to_chunks(q), to_chunks(sel), to_chunks(slot_valid),
                             pos.reshape(nc, Q_CHUNK)))
    return jnp.moveaxis(out, 0, 2).reshape(b, h, s, d)


def multiscale_pool(p, w_pool, pool_scale):
    b, s, _ = p.shape
    pf = p.astype(jnp.float32)
    cs = jnp.pad(jnp.cumsum(pf, axis=1), ((0, 0), (1, 0), (0, 0)))
    t = jnp.arange(s)
    outs = []
    for g, w in enumerate(POOL_WINDOWS):
        sl = slice(g * POOL_GROUP_DIM, (g + 1) * POOL_GROUP_DIM)
        cg = cs[:, :, sl]
        lower = jnp.pad(cg[:, :s + 1 - w], ((0, 0), (w - 1, 0), (0, 0)))
        count = jnp.minimum(t + 1, w).astype(jnp.float32)[None, :, None]
        outs.append((cg[:, 1:] - lower) / count - pf[:, :, sl])
    pooled = jnp.stack(outs, axis=2).astype(p.dtype)
    y = jnp.einsum('bsgc,gce->bsge', pooled, w_pool).reshape(b, s, POOL_WIDTH)
    return y * pool_scale


def conv_glu_ffn(h, w_up, conv_w, conv_b, w_down):
    u = h @ w_up
    s = u.shape[1]
    up = jnp.pad(u, ((0, 0), (CONV_WIDTH - 1, 0), (0, 0)))
    uc = conv_b + conv_w[0] * up[:, 0:s]
    for j in range(1, CONV_WIDTH):
        uc = uc + conv_w[j] * up[:, j:j + s]
    a, g = jnp.split(uc, 2, axis=-1)
    return (jax.nn.silu(a) * g) @ w_down


def setup_inputs(seed: int = 0) -> dict:
    key = jax.random.key(seed)
    ks = jax.random.split(key, 20)
    f32 = jnp.float32
    L, D = DEPTH, D_MODEL

    def nrm(k, shape, fan_in):
        return jax.random.normal(k, shape, f32) * (fan_in ** -0.5)

    def gain(k, shape):
        return 1.0 + 0.05 * jax.random.normal(k, shape, f32)

    return {
        "x": jax.random.normal(ks[0], (BATCH, SEQ, D), f32),
        "c": jax.random.normal(ks[1], (BATCH, D), f32),
        "w_ada": nrm(ks[2], (L, D, 6 * D), D),
        "b_ada": 0.02 * jax.random.normal(ks[3], (L, 6 * D), f32),
        "g_pre_mix": gain(ks[4], (L, D)),
        "w_in": nrm(ks[5], (L, D, IN_WIDTH), D),
        "w_pool": nrm(ks[6], (L, POOL_GROUPS, POOL_GROUP_DIM, POOL_GROUP_DIM), POOL_GROUP_DIM),
        "pool_scale": gain(ks[7], (L, POOL_WIDTH)),
        "attn_out_gain": gain(ks[8], (L, ATTN_WIDTH)),
        "pool_out_gain": gain(ks[9], (L, POOL_WIDTH)),
        "w_out": nrm(ks[10], (L, MIX_WIDTH, D), MIX_WIDTH),
        "g_post_mix": gain(ks[11], (L, D)),
        "g_pre_ffn": gain(ks[12], (L, D)),
        "w_up": nrm(ks[13], (L, D, 2 * D_FF), D),
        "conv_w": nrm(ks[14], (L, CONV_WIDTH, 2 * D_FF), CONV_WIDTH),
        "conv_b": 0.02 * jax.random.normal(ks[15], (L, 2 * D_FF), f32),
        "w_down": nrm(ks[16], (L, D_FF, D), D_FF),
        "g_post_ffn": gain(ks[17], (L, D)),
    }


def reference(x, c, w_ada, b_ada, g_pre_mix, w_in, w_pool, pool_scale, attn_out_gain,
              pool_out_gain, w_out, g_post_mix, g_pre_ffn, w_up, conv_w, conv_b, w_down,
              g_post_ffn):
    b, s, _ = x.shape
    pos = jnp.arange(s, dtype=jnp.float32)
    inv_freq = jnp.power(jnp.float32(ROPE_THETA),
                         -jnp.arange(0, ROT_DIM, 2, dtype=jnp.float32) / ROT_DIM)
    ang = pos[:, None] * inv_freq[None, :]
    cos = jnp.cos(ang).astype(x.dtype)
    sin = jnp.sin(ang).astype(x.dtype)
    c_act = jax.nn.silu(c)

    def heads(t):
        return t.reshape(b, s, ATTN_HEADS, HEAD_DIM).transpose(0, 2, 1, 3)

    for l in range(DEPTH):
        mod = c_act @ w_ada[l] + b_ada[l]
        sh1, sc1, gt1, sh2, sc2, gt2 = jnp.split(mod, 6, axis=-1)

        h = rms_norm(x, g_pre_mix[l]) * (1.0 + sc1[:, None]) + sh1[:, None]
        z = h @ w_in[l]
        q = z[..., 0:ATTN_WIDTH]
        k = z[..., ATTN_WIDTH:2 * ATTN_WIDTH]
        v = z[..., 2 * ATTN_WIDTH:3 * ATTN_WIDTH]
        pz = z[..., 3 * ATTN_WIDTH:]
        qh = partial_rotary(heads(q), cos, sin)
        kh = partial_rotary(heads(k), cos, sin)
        ao = moba_attention(qh, kh, heads(v)).transpose(0, 2, 1, 3).reshape(b, s, ATTN_WIDTH)
        po = multiscale_pool(pz, w_pool[l], pool_scale[l])
        merged = jnp.concatenate([rms_norm(ao, attn_out_gain[l]),
                                  rms_norm(po, pool_out_gain[l])], axis=-1)
        y = merged @ w_out[l]
        x = x + gt1[:, None] * rms_norm(y, g_post_mix[l])

        h = rms_norm(x, g_pre_ffn[l]) * (1.0 + sc2[:, None]) + sh2[:, None]
        y = conv_glu_ffn(h, w_up[l], conv_w[l], conv_b[l], w_down[l])
        x = x + gt2[:, None] * rms_norm(y, g_post_ffn[l])
    return x
```

```python
import contextlib
import os
import numpy as np
import ml_dtypes
import concourse.bass as bass
import concourse.mybir as mybir
from concourse.bass_utils import run_bass_kernel_spmd

F32 = mybir.dt.float32
BF16 = mybir.dt.bfloat16
AF = mybir.ActivationFunctionType
ALU = mybir.AluOpType
AX = mybir.AxisListType

S_LEN = 4096
D = 1024
NT = S_LEN // 128
NH = 8
HD = 64
DFF = 2816
NFC = 2 * DFF // 128
NPAIR = DFF // 128
EPS = 1e-6
BIG = 30000.0
POOL_W = (2, 4, 8, 16)
VW = 68

ENGINES = ("tensor", "vector", "scalar", "gpsimd", "sync")
SEM_LIMIT = 30000


class Op:
    __slots__ = ("eng", "fn", "deps", "is_dma", "ticket", "signal", "idx")

    def __init__(self, eng, fn, is_dma):
        self.idx = 0
        self.eng = eng
        self.fn = fn
        self.is_dma = is_dma
        self.deps = []
        self.ticket = None
        self.signal = False


class Sched:
    def __init__(self, nc):
        self.nc = nc
        self.q = {e: [] for e in ENGINES}
        self.last_w = {}
        self.readers = {}
        self.dma_counts = {}
        self.dma_last = {}
        self.bar = {e: [] for e in ENGINES}

    def barrier(self):
        deps = []
        for e in ENGINES:
            for o in reversed(self.q[e]):
                if not o.is_dma:
                    deps.append(o)
                    break
        deps.extend(self.dma_last.values())
        for e in ENGINES:
            self.bar[e] = list(deps)
        self.last_w = {}
        self.readers = {}

    def op(self, eng, fn, reads=(), writes=(), dma=False, semkey=None):
        o = Op(eng, fn, dma)
        deps = set()
        for k in reads:
            w = self.last_w.get(k)
            if w is not None:
                deps.add(w)
        for k in writes:
            w = self.last_w.get(k)
            if w is not None:
                deps.add(w)
            for r in self.readers.get(k, ()):
                deps.add(r)
        for d in deps:
            if d is o:
                continue
            if (not d.is_dma) and (not dma) and d.eng == eng:
                if eng == "tensor":
                    continue
                raw = any(self.last_w.get(k) is d for k in reads)
                if not raw:
                    continue
            o.deps.append(d)
        if dma and semkey in self.dma_last:
            o.deps.append(self.dma_last[semkey])
        if self.bar[eng]:
            for d in self.bar[eng]:
                if d.is_dma or d.eng != eng:
                    o.deps.append(d)
            self.bar[eng] = []
        best = {}
        pruned = []
        for d in o.deps:
            if d.is_dma:
                pruned.append(d)
            else:
                b = best.get(d.eng)
                if b is None or d.idx > b.idx:
                    best[d.eng] = d
        o.deps = pruned + list(best.values())
        o.idx = len(self.q[eng])
        for k in writes:
            self.last_w[k] = o
            self.readers[k] = []
        for k in reads:
            self.readers.setdefault(k, []).append(o)
        if dma:
            c = self.dma_counts.get(semkey, 0) + 16
            self.dma_counts[semkey] = c
            o.ticket = (("dma", semkey), c)
            self.dma_last[semkey] = o
        self.q[eng].append(o)
        return o

    def emit(self, final_wait_semkeys=()):
        nc = self.nc
        for e in ENGINES:
            for o in self.q[e]:
                for d in o.deps:
                    if not d.is_dma:
                        d.signal = True
        semnames = set()
        for e in ENGINES:
            cnt = 0
            seg = 0
            for o in self.q[e]:
                if o.is_dma:
                    semnames.add(o.ticket[0])
                    continue
                if o.signal:
                    cnt += 1
                    if cnt > SEM_LIMIT:
                        seg += 1
                        cnt = 1
                    o.ticket = (("eng", e, seg), cnt)
                    semnames.add(o.ticket[0])
        semnames = sorted(semnames, key=str)
        with contextlib.ExitStack() as st:
            sems = {}
            for i, n in enumerate(semnames):
                sems[n] = st.enter_context(nc.semaphore("s%d" % i))
            block = st.enter_context(nc.Block())

            def run(engname):
                def body(eng):
                    waited = {}
                    for o in self.q[engname]:
                        need = {}
                        for d in o.deps:
                            s, v = d.ticket
                            if need.get(s, 0) < v:
                                need[s] = v
                        for s, v in need.items():
                            if waited.get(s, 0) < v:
                                eng.wait_ge(sems[s], v)
                                waited[s] = v
                        ins = o.fn(eng)
                        if o.is_dma:
                            ins.then_inc(sems[o.ticket[0]], 16)
                        elif o.signal:
                            ins.then_inc(sems[o.ticket[0]], 1)
                    if engname == "sync":
                        for k in final_wait_semkeys:
                            s = ("dma", k)
                            v = self.dma_counts[k]
                            if waited.get(s, 0) < v:
                                eng.wait_ge(sems[s], v)
                                waited[s] = v
                return body

            block.tensor(run("tensor"))
            block.vector(run("vector"))
            block.scalar(run("scalar"))
            block.gpsimd(run("gpsimd"))
            block.sync(run("sync"))
        return len(semnames)


def _const_tables():
    bf = ml_dtypes.bfloat16
    pos = np.arange(S_LEN, dtype=np.float32)
    inv_freq = np.power(np.float32(500000.0), -np.arange(0, 16, 2, dtype=np.float32) / np.float32(16))
    ang = (pos[:, None] * inv_freq[None, :]).astype(np.float32)
    cos = np.cos(ang).astype(np.float32)
    sin = np.sin(ang).astype(np.float32)
    def tm(a):
        return np.ascontiguousarray(a.reshape(NT, 128, -1).transpose(1, 0, 2))
    ropek = tm(np.concatenate([cos, sin], axis=1)).astype(np.float32)
    ropeq = (ropek * np.float32(0.125)).astype(np.float32)
    qt = np.arange(NT)
    j = qt // 2
    n = np.arange(16)
    past = (n[None, :] < j[:, None])
    pastmask = np.where(past, 0.0, -1e30).astype(np.float32).reshape(1, NT * 16)
    pastind = past.astype(np.float32).reshape(1, NT * 16)
    ownfut = np.where(n[None, :] > j[:, None], -BIG, 0.0).astype(np.float32).reshape(1, NT * 16)
    onehot = (np.arange(S_LEN)[None, :] // 256 == n[:, None]).astype(np.float32).astype(bf)
    k = np.arange(128)
    tri = (k[None, :] >= k[:, None]).astype(np.float32).astype(bf)
    identb = np.eye(128, dtype=np.float32).astype(bf)
    identf = np.eye(128, dtype=np.float32)
    onesb = np.ones((128, 128), dtype=np.float32).astype(bf)
    rc = np.zeros((1, 4, 16), dtype=np.float32)
    for g, w in enumerate(POOL_W):
        rc[0, g, :] = 1.0 / np.minimum(np.arange(16) + 1, w)
    return {
        "ropeq": ropeq, "ropek": ropek,
        "pastmask": np.ascontiguousarray(np.broadcast_to(pastmask, (128, NT * 16))),
        "pastind": np.ascontiguousarray(np.broadcast_to(pastind, (128, NT * 16))),
        "ownfut": np.ascontiguousarray(np.broadcast_to(ownfut, (128, NT * 16))),
        "onehot": onehot, "tri": tri, "identb": identb, "identf": identf, "onesb": onesb,
        "rc": np.ascontiguousarray(np.broadcast_to(rc, (128, 4, 16))),
    }


CONST_SPECS = {
    "ropeq": ([128, NT, 16], F32), "ropek": ([128, NT, 16], F32),
    "pastmask": ([128, NT * 16], F32), "pastind": ([128, NT * 16], F32), "ownfut": ([128, NT * 16], F32),
    "onehot": ([16, S_LEN], BF16), "tri": ([128, 128], BF16), "identb": ([128, 128], BF16),
    "identf": ([128, 128], F32), "onesb": ([128, 128], BF16), "rc": ([128, 4, 16], F32),
}

WEIGHT_SPECS = {
    "c": [1, D], "w_ada": [2, D, 6 * D], "b_ada": [2, 6 * D], "g_pre_mix": [2, D], "w_in": [2, D, 2048],
    "w_pool": [2, 4, 128, 128], "pool_scale": [2, 512], "attn_out_gain": [2, 512], "pool_out_gain": [2, 512],
    "w_out": [2, D, D], "g_post_mix": [2, D], "g_pre_ffn": [2, D], "w_up": [2, D, 2 * DFF],
    "conv_w": [2, 3, 2 * DFF], "conv_b": [2, 2 * DFF], "w_down": [2, DFF, D], "g_post_ffn": [2, D],
}


class Alloc:
    def __init__(self, nc):
        self.nc = nc
        self.base = (nc.sbuf_base + 63) // 64 * 64
        self.top = nc.sbuf_top
        self.cur = self.base
        self.n = 0

    def mark(self):
        return self.cur

    def reset(self, m):
        self.cur = m

    def __call__(self, shape, dt):
        sz = 1
        for s in shape[1:]:
            sz *= s
        nbytes = sz * (4 if dt == F32 else 2)
        nbytes = (nbytes + 63) // 64 * 64
        off = self.cur
        assert off + nbytes <= self.top, ("SBUF overflow", off, nbytes, self.top)
        self.cur += nbytes
        self.n += 1
        return self.nc.alloc_sbuf_tensor_at("sb%d" % self.n, list(shape), dt, offset=off)


def build(n_layers=2, stop_after=None, debug=False):
    nc = bass.Bass("TRN2", target_bir_lowering=False)
    I = {}
    I["x"] = nc.dram_tensor("x", [S_LEN, D], F32, kind="ExternalInput").ap()
    for k, shp in WEIGHT_SPECS.items():
        I[k] = nc.dram_tensor(k, shp, F32, kind="ExternalInput").ap()
    for k, (shp, dt) in CONST_SPECS.items():
        I[k] = nc.dram_tensor(k, shp, dt, kind="ExternalInput").ap()
    out = nc.dram_tensor("out", [S_LEN, D], F32, kind="ExternalOutput").ap()
    sk = "ExternalOutput" if debug else "Internal"
    qT_s = nc.dram_tensor("qT_s", [NH, HD, S_LEN], BF16, kind=sk).ap()
    kT_s = nc.dram_tensor("kT_s", [NH, HD, S_LEN], BF16, kind=sk).ap()
    v_s = nc.dram_tensor("v_s", [S_LEN, NH * VW], BF16, kind=sk).ap()
    pm_s = nc.dram_tensor("pm_s", [NT, 128, 512], BF16, kind=sk).ap()
    g_s = nc.dram_tensor("g_s", [2, 128, D], F32, kind=sk).ap()
    xa = nc.dram_tensor("xa", [S_LEN, D], F32, kind=sk).ap()
    xb = nc.dram_tensor("xb", [S_LEN, D], F32, kind=sk).ap()
    dbg = {}
    if debug:
        dbg["ao"] = nc.dram_tensor("dbg_ao", [S_LEN, 512], F32, kind="ExternalOutput").ap()

    S = Sched(nc)
    A = Alloc(nc)
    PB = [nc.alloc_psum_tensor("pb%d" % i, [128, 512], F32) for i in range(6)]
    PT = [nc.alloc_psum_tensor("pt%d" % i, [128, 1024], BF16) for i in range(2)]

    sec = [None]
    enabled = os.environ.get("KSEC")
    nt_run = int(os.environ.get("KNT", NT))

    def skip():
        return enabled is not None and sec[0] is not None and sec[0] not in enabled

    def E(eng, meth, reads, writes, **kw):
        if skip():
            return None
        return S.op(eng, lambda e: getattr(e, meth)(**kw), reads, writes)

    otc = [0]

    def OT():
        otc[0] += 1
        return "ot%d" % (otc[0] % 6)

    def DMA(eng, out_, in_, reads, writes, semkey, slow=False):
        if skip():
            return None
        if semkey is None:
            semkey = OT()
        if slow:
            return S.op(eng, lambda e: e.dma_start(out=out_, in_=in_, allow_slow_non_contiguous=True),
                        reads, writes, dma=True, semkey=semkey)
        return S.op(eng, lambda e: e.dma_start(out=out_, in_=in_), reads, writes, dma=True, semkey=semkey)

    identb = A([128, 128], BF16)
    identf = A([128, 128], F32)
    onesb = A([128, 128], BF16)
    tri = A([128, 128], BF16)
    for nm, t in (("identb", identb), ("identf", identf), ("onesb", onesb), ("tri", tri)):
        DMA("sync", t[:], I[nm], [], [nm], None)
    cols = A([128, 4, 8], F32)
    aog_col = A([128, 4], F32)
    ps_col = A([128, 4], F32)
    pg_col = A([128, 4], F32)
    cv_col = A([128, 4, NFC], F32)
    rstd_eps = EPS
    persist_mark = A.mark()

    def rstd_ops(ss, rstd, n, keys_r, keys_w, dim):
        E("scalar", "activation", keys_r, keys_w, out=rstd, in_=ss, func=AF.Ln, scale=1.0 / dim, bias=rstd_eps)
        E("scalar", "activation", keys_w, keys_w, out=rstd, in_=rstd, func=AF.Exp, scale=-0.5)

    x_src = I["x"]
    for l in range(n_layers):
        last_layer = (l == n_layers - 1)
        x_mid = xa
        x_dst = out if last_layer else xb
        S.barrier()
        A.reset(persist_mark)
        c_col = A([128, 8], F32)
        cact = A([128, 8], F32)
        cbc = A([128, 8, 128], F32)
        bada = A([128, 6 * D], F32)
        modbc = A([128, 6 * D], F32)
        wblk = [A([128, 8, 512], F32) for _ in range(2)]
        gbc = A([128, D], F32)
        gtmp = A([128, D], F32)
        dtmp = A([128, 8, 128], F32)
        stg = [A([64, 128], F32) for _ in range(2)]
        stc = [0]

        def load_col(vec1d, n, dst, wkey):
            i_ = stc[0] % 2
            stc[0] += 1
            sg = stg[i_]
            DMA("sync", sg[0:n, :], vec1d.rearrange("(c p) -> c p", p=128), [], ["stg%d" % i_], None)
            E("tensor", "matmul", ["stg%d" % i_, "identf"], ["PB2"], out=PB[2][:, 0:n], lhsT=sg[0:n, :], rhs=identf[0:n, 0:n],
              start=True, stop=True)
            E("vector", "tensor_copy", ["PB2"], [wkey], out=dst, in_=PB[2][:, 0:n])

        load_col(I["c"][0], 8, c_col[:], "c_col")
        DMA("sync", bada[:], I["b_ada"][l].partition_broadcast(128), [], ["bada"], None)
        load_col(I["attn_out_gain"][l], 4, aog_col[:], "aog")
        load_col(I["pool_scale"][l], 4, ps_col[:], "psc")
        load_col(I["pool_out_gain"][l], 4, pg_col[:], "pgc")
        for j3 in range(3):
            load_col(I["conv_w"][l, j3], NFC, cv_col[:, j3, :], "cv%d" % j3)
        load_col(I["conv_b"][l], NFC, cv_col[:, 3, :], "cv3")
        E("scalar", "activation", ["c_col"], ["cact"], out=cact[:], in_=c_col[:], func=AF.Silu)
        E("vector", "tensor_copy", ["cact"], ["cbc"], out=cbc[:], in_=cact[:].unsqueeze(2).broadcast_to([128, 8, 128]))
        for blk in range(12):
            wb = wblk[blk % 2]
            kb = "wblk%d" % (blk % 2)
            pm_ = PB[blk % 2]
            kp = "PB%d" % (blk % 2)
            DMA("sync", wb[:], I["w_ada"][l][:, blk * 512:(blk + 1) * 512].rearrange("(kc p) n -> p kc n", p=128),
                [], [kb], "wada%d" % (blk % 2))
            for kc in range(8):
                E("tensor", "matmul", [kb, "cbc"], [kp], out=pm_[:], lhsT=cbc[:, kc, :], rhs=wb[:, kc, :],
                  start=(kc == 0), stop=(kc == 7))
            E("vector", "tensor_tensor", [kp, "bada"], ["modbc"], out=modbc[:, blk * 512:(blk + 1) * 512], in0=pm_[:],
              in1=bada[:, blk * 512:(blk + 1) * 512], op=ALU.add)

        def diag_extract(src_ap, dst_ap, rkeys, wkey):
            E("vector", "tensor_tensor", rkeys + ["identf"], ["dtmp"], out=dtmp[:],
              in0=src_ap.rearrange("p (k q) -> p k q", k=8),
              in1=identf[:].unsqueeze(1).broadcast_to([128, 8, 128]), op=ALU.mult)
            E("vector", "tensor_reduce", ["dtmp"], [wkey], out=dst_ap, in_=dtmp[:], axis=AX.X, op=ALU.add)

        for gi, (gname, moff) in enumerate((("g_post_mix", 2 * D), ("g_post_ffn", 5 * D))):
            DMA("sync", gbc[:], I[gname][l].partition_broadcast(128), [], ["gbc"], None)
            E("vector", "tensor_tensor", ["gbc", "modbc"], ["gtmp"], out=gtmp[:], in0=modbc[:, moff:moff + D], in1=gbc[:], op=ALU.mult)
            DMA("sync", g_s[gi], gtmp[:], ["gtmp"], [], None)
        for ci, (gname, scoff, shoff) in enumerate((("g_pre_mix", 1 * D, 0), ("g_pre_ffn", 4 * D, 3 * D))):
            DMA("sync", gbc[:], I[gname][l].partition_broadcast(128), [], ["gbc"], None)
            E("vector", "scalar_tensor_tensor", ["gbc", "modbc"], ["gtmp"], out=gtmp[:], in0=modbc[:, scoff:scoff + D],
              scalar=1.0, in1=gbc[:], op0=ALU.add, op1=ALU.mult)
            diag_extract(gtmp[:], cols[:, 2 * ci, :], ["gtmp"], "cols%d" % (2 * ci))
            diag_extract(modbc[:, shoff:shoff + D], cols[:, 2 * ci + 1, :], ["modbc"], "cols%d" % (2 * ci + 1))
        if stop_after == "P":
            break

        S.barrier()
        A.reset(persist_mark)
        w_in = A([128, 8, 2048], BF16)
        w_pool = A([128, 4, 128], BF16)
        ropeq = A([128, NT, 16], F32)
        ropek = A([128, NT, 16], F32)
        rc = A([128, 4, 16], F32)
        xt = [A([128, D], F32) for _ in range(3)]
        junk = A([128, D], BF16)
        ss = [A([128, 1], F32) for _ in range(2)]
        rstd = [A([128, 1], F32) for _ in range(2)]
        xn = [A([128, D], BF16) for _ in range(2)]
        hT = [A([128, 8, 128], BF16) for _ in range(2)]
        q_tm = [A([128, 512], BF16) for _ in range(2)]
        k_tm = [A([128, 512], BF16) for _ in range(2)]
        rt = [A([128, 8, 8], F32) for _ in range(4)]
        zq = A([128, 8, 16], F32)
        qst = [A([64, 2, 4, 512], BF16) for _ in range(2)]
        kst = [A([64, 2, 4, 512], BF16) for _ in range(2)]
        vst = [A([128, 8, VW], BF16) for _ in range(2)]
        pmst = [A([128, 4, 128], BF16) for _ in range(2)]
        pz = A([128, 4, 144], F32)
        s2 = A([128, 4, 144], F32)
        s4 = A([128, 4, 144], F32)
        s8 = A([128, 4, 144], F32)
        s16 = A([128, 4, 144], F32)
        pooledT = A([128, 4, 128], BF16)
        po_sb = A([128, 4, 128], F32)
        sq = A([128, 4, 128], BF16)
        prs = A([128, 128], F32)
        ptmp = A([128, 4, 128], F32)
        etmp = A([128, 4, 16], F32)

        sec[0] = "L"
        for kc in range(8):
            DMA("gpsimd", w_in[:, kc, :], I["w_in"][l][kc * 128:(kc + 1) * 128, :], [], ["w_in"], "wl%d" % (kc % 4))
        DMA("gpsimd", w_pool[:], I["w_pool"][l].rearrange("g c e -> c g e"), [], ["w_pool"], "wl0")
        DMA("sync", ropeq[:], I["ropeq"], [], ["ropeq"], None)
        DMA("sync", ropek[:], I["ropek"], [], ["ropek"], None)
        DMA("sync", rc[:], I["rc"], [], ["rc"], None)
        for b_ in range(2):
            E("gpsimd", "memset", [], ["vst%d" % b_], ap=vst[b_][:], constant=1.0)
        E("gpsimd", "memset", [], ["pz"], ap=pz[:], constant=0.0)
        Pq, Pk, Pv, Pp, Py, Ps = PB
        T0, T1 = PT[0], PT[0]
        for t in range(nt_run):
            sec[0] = "A"
            b2 = t % 2
            xs = xt[t % 3]
            kx = "xt%d" % (t % 3)
            g4 = t // 4
            ti = t % 4
            DMA("sync", xs[:], x_src[t * 128:(t + 1) * 128, :], [], [kx], "xl%d" % (t % 3))
            E("scalar", "activation", [kx], ["junk", "ss%d" % b2], out=junk[:], in_=xs[:], func=AF.Square, accum_out=ss[b2][:])
            rstd_ops(ss[b2][:], rstd[b2][:], 1, ["ss%d" % b2], ["rstd%d" % b2], D)
            E("vector", "tensor_scalar", [kx, "rstd%d" % b2], ["xn%d" % b2], out=xn[b2][:], in0=xs[:], scalar1=rstd[b2][:, 0:1],
              scalar2=None, op0=ALU.mult)
            for kc in range(8):
                E("tensor", "transpose", ["xn%d" % b2, "identb"], ["T0"], out=T0[:, kc * 128:(kc + 1) * 128],
                  in_=xn[b2][:, kc * 128:(kc + 1) * 128], identity=identb[:])
            for kc in range(8):
                E("scalar", "activation", ["T0", "cols0", "cols1"], ["hT%d" % b2], out=hT[b2][:, kc, :], in_=T0[:, kc * 128:(kc + 1) * 128],
                  func=AF.Identity, scale=cols[:, 0, kc:kc + 1], bias=cols[:, 1, kc:kc + 1])
            sec[0] = "B"
            for P_, kP, c0 in ((Pq, "Pq", 0), (Pk, "Pk", 512), (Pv, "Pv", 1024)):
                for kc in range(8):
                    E("tensor", "matmul", ["hT%d" % b2, "w_in"], [kP], out=P_[:], lhsT=hT[b2][:, kc, :], rhs=w_in[:, kc, c0:c0 + 512],
                      start=(kc == 0), stop=(kc == 7))
            for g in range(4):
                for kc in range(8):
                    E("tensor", "matmul", ["hT%d" % b2, "w_in"], ["Pp"], out=Pp[:, g * 128:(g + 1) * 128],
                      lhsT=w_in[:, kc, 1536 + g * 128:1536 + (g + 1) * 128], rhs=hT[b2][:, kc, :], start=(kc == 0), stop=(kc == 7))
            sec[0] = "R"
            for P_, kP, tm_, ktm, rope, krope, sc_ in ((Pq, "Pq", q_tm[b2], "q_tm%d" % b2, ropeq, "ropeq", 0.125),
                                                       (Pk, "Pk", k_tm[b2], "k_tm%d" % b2, ropek, "ropek", 1.0)):
                E("scalar", "activation", [kP], [ktm], out=tm_[:], in_=P_[:], func=AF.Identity, scale=sc_)
                pv = P_[:].rearrange("p (h d) -> p h d", h=8)
                ov = tm_[:].rearrange("p (h d) -> p h d", h=8)
                E("scalar", "activation", [kP], ["zq"], out=zq[:], in_=pv[:, :, 0:16], func=AF.Copy)
                cosb = rope[:, t, 0:8].unsqueeze(1).broadcast_to([128, 8, 8])
                sinb = rope[:, t, 8:16].unsqueeze(1).broadcast_to([128, 8, 8])
                x1 = zq[:, :, 0:8]
                x2 = zq[:, :, 8:16]
                sec[0] = "Q"
                E("vector", "tensor_tensor", ["zq", krope], ["rt0"], out=rt[0][:], in0=x1, in1=cosb, op=ALU.mult)
                E("vector", "tensor_tensor", ["zq", krope], ["rt1"], out=rt[1][:], in0=x2, in1=sinb, op=ALU.mult)
                E("vector", "tensor_tensor", ["zq", krope], ["rt2"], out=rt[2][:], in0=x2, in1=cosb, op=ALU.mult)
                E("vector", "tensor_tensor", ["zq", krope], ["rt3"], out=rt[3][:], in0=x1, in1=sinb, op=ALU.mult)
                E("vector", "tensor_tensor", ["rt0", "rt1"], [ktm], out=ov[:, :, 0:8], in0=rt[0][:], in1=rt[1][:], op=ALU.subtract)
                E("vector", "tensor_tensor", ["rt2", "rt3"], [ktm], out=ov[:, :, 8:16], in0=rt[2][:], in1=rt[3][:], op=ALU.add)
                sec[0] = "R"
            sec[0] = "V"
            sb_ = g4 % 2
            vb = t % 2
            E("scalar", "activation", ["Pv"], ["vst%d" % vb], out=vst[vb][:, :, 0:64], in_=Pv[:].rearrange("p (h d) -> p h d", h=8),
              func=AF.Copy)
            DMA("sync", v_s[t * 128:(t + 1) * 128, :], vst[vb][:].rearrange("p h d -> p (h d)"), ["vst%d" % vb], [], "vs%d" % vb)
            sec[0] = "C"
            for tm_, ktm, st_, kst_ in ((q_tm[b2], "q_tm%d" % b2, qst[sb_], "qst%d" % sb_), (k_tm[b2], "k_tm%d" % b2, kst[sb_], "kst%d" % sb_)):
                for pr in range(4):
                    E("tensor", "transpose", [ktm, "identb"], ["T0"], out=T1[:, pr * 128:(pr + 1) * 128],
                      in_=tm_[:, pr * 128:(pr + 1) * 128], identity=identb[:])
                for pa in range(2):
                    sec[0] = "X"
                    E("vector", "tensor_copy", ["T0"], [kst_], out=st_[:, pa, :, ti * 128:(ti + 1) * 128],
                      in_=T1[pa * 64:(pa + 1) * 64, 0:512].rearrange("p (r t) -> p r t", r=4))
                    sec[0] = "C"
            sec[0] = "S"
            if ti == 3:
                for pa in range(2):
                    DMA("sync", qT_s.rearrange("(pr pa) d t -> pa d pr t", pa=2)[pa][:, :, g4 * 512:(g4 + 1) * 512], qst[sb_][:, pa, :, :],
                        ["qst%d" % sb_], [], "qs%d%d" % (sb_, pa))
                    DMA("sync", kT_s.rearrange("(pr pa) d t -> pa d pr t", pa=2)[pa][:, :, g4 * 512:(g4 + 1) * 512], kst[sb_][:, pa, :, :],
                        ["kst%d" % sb_], [], "ks%d%d" % (sb_, pa))
            sec[0] = "D"
            E("scalar", "activation", ["Pp"], ["pz"], out=pz[:, :, 16:144], in_=Pp[:].rearrange("p (g t) -> p g t", g=4), func=AF.Copy)
            E("gpsimd", "tensor_tensor", ["pz"], ["s2"], out=s2[:, :, 1:144], in0=pz[:, :, 1:144], in1=pz[:, :, 0:143], op=ALU.add)
            E("gpsimd", "tensor_tensor", ["s2"], ["s4"], out=s4[:, 1:4, 3:144], in0=s2[:, 1:4, 3:144], in1=s2[:, 1:4, 1:142], op=ALU.add)
            E("gpsimd", "tensor_tensor", ["s4"], ["s8"], out=s8[:, 2:4, 7:144], in0=s4[:, 2:4, 7:144], in1=s4[:, 2:4, 3:140], op=ALU.add)
            E("gpsimd", "tensor_tensor", ["s8"], ["s16"], out=s16[:, 3:4, 15:144], in0=s8[:, 3:4, 15:144], in1=s8[:, 3:4, 7:136], op=ALU.add)
            sums = (s2, s4, s8, s16)
            for g in range(4):
                E("vector", "scalar_tensor_tensor", ["s%d" % (2 << g), "pz"], ["pooledT"], out=pooledT[:, g, :], in0=sums[g][:, g, 16:144],
                  scalar=1.0 / POOL_W[g], in1=pz[:, g, 16:144], op0=ALU.mult, op1=ALU.subtract)
            if t == 0:
                for g in range(4):
                    E("gpsimd", "tensor_tensor", ["s%d" % (2 << g), "rc"], ["etmp"], out=etmp[:, g, :], in0=sums[g][:, g, 16:32], in1=rc[:, g, :], op=ALU.mult)
                    E("gpsimd", "tensor_tensor", ["etmp", "pz"], ["pooledT"], out=pooledT[:, g, 0:16], in0=etmp[:, g, :], in1=pz[:, g, 16:32], op=ALU.subtract)
            E("gpsimd", "tensor_copy", ["pz", "s2", "s4", "s8", "s16", "pooledT"], ["pz"], out=pz[:, :, 0:16], in_=pz[:, :, 128:144])
            for g in range(4):
                E("tensor", "matmul", ["pooledT", "w_pool"], ["Py"], out=Py[:, g * 128:(g + 1) * 128], lhsT=w_pool[:, g, :], rhs=pooledT[:, g, :],
                  start=True, stop=True)
            E("vector", "tensor_tensor", ["Py", "psc"], ["po_sb"], out=po_sb[:], in0=Py[:].rearrange("p (g t) -> p g t", g=4),
              in1=ps_col[:].unsqueeze(2).broadcast_to([128, 4, 128]), op=ALU.mult)
            E("scalar", "activation", ["po_sb"], ["sq"], out=sq[:], in_=po_sb[:], func=AF.Square)
            for g in range(4):
                E("tensor", "matmul", ["sq", "onesb"], ["Ps"], out=Ps[:, 0:128], lhsT=onesb[:], rhs=sq[:, g, :], start=(g == 0), stop=(g == 3))
            rstd_ops(Ps[:, 0:128], prs[:], 128, ["Ps"], ["prs"], 512)
            E("vector", "tensor_tensor", ["po_sb", "pgc"], ["ptmp"], out=ptmp[:], in0=po_sb[:],
              in1=pg_col[:].unsqueeze(2).broadcast_to([128, 4, 128]), op=ALU.mult)
            pb_ = t % 2
            E("vector", "tensor_tensor", ["ptmp", "prs"], ["pmst%d" % pb_], out=pmst[pb_][:], in0=ptmp[:],
              in1=prs[:].unsqueeze(1).broadcast_to([128, 4, 128]), op=ALU.mult)
            DMA("sync", pm_s[t], pmst[pb_][:].rearrange("p g t -> p (g t)"), ["pmst%d" % pb_], [], "pms%d" % pb_)
        sec[0] = None
        if stop_after == "M1":
            break

        S.barrier()
        A.reset(persist_mark)
        ao_all = A([128, NT, 512], F32)
        w_out = A([128, 8, D], BF16)
        G2 = A([128, D], F32)
        m3_mark = A.mark()
        vaug = A([128, NT, NH * VW], BF16)
        kaug = [A([128, S_LEN], BF16) for _ in range(2)]
        qaug = [A([128, S_LEN], BF16) for _ in range(2)]
        kmT = A([64, 16], F32)
        kmTb = A([64, 16], BF16)
        pastmask = A([128, NT * 16], F32)
        pastind = A([128, NT * 16], F32)
        ownfut = A([128, NT * 16], F32)
        Gm = A([128, NT * 16], F32)
        m8 = A([128, NT, 8], F32)
        sel = A([128, NT * 16], F32)
        biasW = A([128, NT, 80], BF16)
        PTb = [A([128, 512], BF16) for _ in range(3)]
        rl = A([128, 4], F32)
        osb = A([128, 512], F32)
        for v4 in range(4):
            DMA("sync", vaug[:, v4 * 8:(v4 + 1) * 8, :], v_s[v4 * 1024:(v4 + 1) * 1024, :].rearrange("(t p) f -> p t f", p=128), [], ["vaug"], None)
        DMA("sync", pastmask[:], I["pastmask"], [], ["pastmask"], None)
        DMA("sync", pastind[:], I["pastind"], [], ["pastind"], None)
        DMA("sync", ownfut[:], I["ownfut"], [], ["ownfut"], None)
        for b_ in range(2):
            DMA("sync", kaug[b_][64:80, :], I["onehot"], [], ["kaug%d" % b_], None)
        E("gpsimd", "memset", [], ["biasW"], ap=biasW[:], constant=0.0)
        for kc in range(8):
            DMA("gpsimd", w_out[:, kc, :], I["w_out"][l][kc * 128:(kc + 1) * 128, :], [], ["w_out"], "wl%d" % (kc % 4))
        DMA("sync", G2[:], g_s[0], [], ["G2"], None)
        SB = PB[0:3]
        OB = PB[3:5]
        GP = PB[5]
        BT = PT[0]
        sidx = 0
        for h in range(NH):
            hb = h % 2
            kk = "kaug%d" % hb
            kq = "qaug%d" % hb
            DMA("sync", kaug[hb][0:64, :], kT_s[h], [], [kk], "kl%d" % hb)
            DMA("sync", qaug[hb][0:64, :], qT_s[h], [], [kq], "ql%d" % hb)
            E("vector", "tensor_reduce", [kk], ["kmT"], out=kmT[:], in_=kaug[hb][0:64, :].rearrange("p (n k) -> p n k", n=16), axis=AX.X, op=ALU.add)
            E("vector", "tensor_scalar", ["kmT"], ["kmTb"], out=kmTb[:], in0=kmT[:], scalar1=1.0 / 256, scalar2=None, op0=ALU.mult)
            for qt_ in range(NT):
                E("tensor", "matmul", [kq, "kmTb"], ["GP"], out=GP[:, qt_ * 16:(qt_ + 1) * 16], lhsT=qaug[hb][0:64, qt_ * 128:(qt_ + 1) * 128],
                  rhs=kmTb[:], start=True, stop=True)
            E("vector", "tensor_tensor", ["GP", "pastmask"], ["Gm"], out=Gm[:], in0=GP[:], in1=pastmask[:], op=ALU.add)
            for qt_ in range(NT):
                E("vector", "max", ["Gm"], ["m8"], out=m8[:, qt_, :], in_=Gm[:, qt_ * 16:(qt_ + 1) * 16])
            E("vector", "tensor_tensor", ["Gm", "m8"], ["sel"], out=sel[:].rearrange("p (q n) -> p q n", n=16),
              in0=Gm[:].rearrange("p (q n) -> p q n", n=16), in1=m8[:, :, 2:3].broadcast_to([128, NT, 16]), op=ALU.is_ge)
            E("vector", "tensor_scalar", ["sel"], ["sel"], out=sel[:], in0=sel[:], scalar1=-1.0, scalar2=BIG, op0=ALU.add, op1=ALU.mult)
            E("vector", "tensor_tensor", ["sel", "pastind"], ["sel"], out=sel[:], in0=sel[:], in1=pastind[:], op=ALU.mult)
            E("vector", "tensor_tensor", ["sel", "ownfut"], ["biasW"], out=biasW[:, :, 64:80], in0=sel[:].rearrange("p (q n) -> p q n", n=16),
              in1=ownfut[:].rearrange("p (q n) -> p q n", n=16), op=ALU.add)
            for r4 in range(4):
                for q8 in range(8):
                    qt_ = r4 * 8 + q8
                    E("tensor", "transpose", ["biasW", "identb"], ["BT"], out=BT[0:80, q8 * 128:(q8 + 1) * 128], in_=biasW[:, qt_, :], identity=identb[:])
                E("vector", "tensor_copy", ["BT"], [kq], out=qaug[hb][64:80, r4 * 1024:(r4 + 1) * 1024], in_=BT[64:80, :])
            for g in range(8):
                ob = OB[g % 2]
                ko = "OB%d" % (g % 2)
                nkt = 4 * g + 4
                for kt in range(nkt):
                    sb_ = SB[sidx % 3]
                    ks = "SB%d" % (sidx % 3)
                    pt_ = PTb[sidx % 3]
                    kpt = "PTb%d" % (sidx % 3)
                    sidx += 1
                    E("tensor", "matmul", [kk, kq], [ks], out=sb_[:], lhsT=kaug[hb][0:80, kt * 128:(kt + 1) * 128],
                      rhs=qaug[hb][0:80, g * 512:(g + 1) * 512], start=True, stop=True)
                    E("scalar", "activation", [ks], [kpt], out=pt_[:], in_=sb_[:], func=AF.Exp)
                    r = kt - 4 * g
                    if r >= 0:
                        E("gpsimd", "tensor_tensor", [kpt, "tri"], [kpt], out=pt_[:, r * 128:(r + 1) * 128], in0=pt_[:, r * 128:(r + 1) * 128],
                          in1=tri[:], op=ALU.mult)
                    for qi in range(4):
                        if kt <= 4 * g + qi:
                            E("tensor", "matmul", [kpt, "vaug"], [ko], out=ob[:, qi * 128:qi * 128 + 65], lhsT=pt_[:, qi * 128:(qi + 1) * 128],
                              rhs=vaug[:, kt, h * VW:h * VW + 65], start=(kt == 0 and qi == 0), stop=(kt == 4 * g + qi))
                E("scalar", "activation", [ko], ["osb"], out=osb[:], in_=ob[:], func=AF.Copy)
                obv = osb[:].rearrange("p (q c) -> p q c", q=4)
                E("vector", "reciprocal", ["osb"], ["rl"], out=rl[:].unsqueeze(2), in_=obv[:, :, 64:65])
                E("vector", "tensor_tensor", ["osb", "rl"], ["ao_all"], out=ao_all[:, 4 * g:4 * g + 4, h * 64:(h + 1) * 64], in0=obv[:, :, 0:64],
                  in1=rl[:].unsqueeze(2).broadcast_to([128, 4, 64]), op=ALU.mult)
        if debug:
            for v4 in range(4):
                DMA("sync", dbg["ao"][v4 * 1024:(v4 + 1) * 1024, :].rearrange("(t p) f -> p t f", p=128), ao_all[:, v4 * 8:(v4 + 1) * 8, :], ["ao_all"], [], None)
        S.barrier()
        A.reset(m3_mark)
        xt = [A([128, D], F32) for _ in range(3)]
        junk = A([128, D], BF16)
        ss = [A([128, 1], F32) for _ in range(2)]
        rstd = [A([128, 1], F32) for _ in range(2)]
        an = [A([128, 512], BF16) for _ in range(2)]
        mTa = [A([128, 4, 128], BF16) for _ in range(2)]
        pmt = [A([128, 4, 128], BF16) for _ in range(2)]
        tt = [A([128, D], F32) for _ in range(2)]
        xo = [A([128, D], F32) for _ in range(2)]
        Y = [nc_y for nc_y in (PB[0], PB[1])]
        TA = PT[1]
        for t in range(NT):
            b2 = t % 2
            xs = xt[t % 3]
            kx = "xt%d" % (t % 3)
            DMA("sync", xs[:], x_src[t * 128:(t + 1) * 128, :], [], [kx], "xl%d" % (t % 3))
            DMA("sync", pmt[b2][:].rearrange("p g t -> p (g t)"), pm_s[t], [], ["pmt%d" % b2], "pml%d" % b2)
            E("scalar", "activation", ["ao_all"], ["junk", "ss%d" % b2], out=junk[:, 0:512], in_=ao_all[:, t, :], func=AF.Square, accum_out=ss[b2][:])
            rstd_ops(ss[b2][:], rstd[b2][:], 1, ["ss%d" % b2], ["rstd%d" % b2], 512)
            E("vector", "tensor_scalar", ["ao_all", "rstd%d" % b2], ["an%d" % b2], out=an[b2][:], in0=ao_all[:, t, :], scalar1=rstd[b2][:, 0:1],
              scalar2=None, op0=ALU.mult)
            for c4 in range(4):
                E("tensor", "transpose", ["an%d" % b2, "identb"], ["TA"], out=TA[:, c4 * 128:(c4 + 1) * 128], in_=an[b2][:, c4 * 128:(c4 + 1) * 128],
                  identity=identb[:])
            for c4 in range(4):
                E("scalar", "activation", ["TA", "aog"], ["mTa%d" % b2], out=mTa[b2][:, c4, :], in_=TA[:, c4 * 128:(c4 + 1) * 128], func=AF.Identity,
                  scale=aog_col[:, c4:c4 + 1])
            for nb in range(2):
                for c8 in range(8):
                    lhs = mTa[b2][:, c8, :] if c8 < 4 else pmt[b2][:, c8 - 4, :]
                    E("tensor", "matmul", ["mTa%d" % b2, "pmt%d" % b2, "w_out"], ["PB%d" % nb], out=Y[nb][:], lhsT=lhs,
                      rhs=w_out[:, c8, nb * 512:(nb + 1) * 512], start=(c8 == 0), stop=(c8 == 7))
            post_norm_residual(E, rstd_ops, Y, ["PB0", "PB1"], junk, "junk", ss[b2], "ss%d" % b2, rstd[b2], "rstd%d" % b2, G2, "G2",
                               tt[b2], "tt%d" % b2, xs, kx, xo[b2], "xo%d" % b2)
            DMA("sync", x_mid[t * 128:(t + 1) * 128, :], xo[b2][:], ["xo%d" % b2], [], "xst%d" % b2)
        if stop_after == "M3":
            break

        S.barrier()
        A.reset(persist_mark)
        w_up = A([128, 8, 2 * DFF], BF16)
        w_dn = A([128, NPAIR, D], BF16)
        G4 = A([128, D], F32)
        xt = [A([128, D], F32) for _ in range(3)]
        junk = A([128, D], BF16)
        ss = [A([128, 1], F32) for _ in range(2)]
        rstd = [A([128, 1], F32) for _ in range(2)]
        xn = [A([128, D], BF16) for _ in range(2)]
        hTg = A([128, 8, 512], BF16)
        mT = A([128, NPAIR, 512], BF16)
        acc = [[A([128, 512], F32) for _ in range(2)] for _ in range(2)]
        hist = A([128, NFC, 2], F32)
        tt = [A([128, D], F32)]
        xo = [A([128, D], F32) for _ in range(2)]
        for kc in range(8):
            for hf in range(2):
                DMA("gpsimd", w_up[:, kc, hf * DFF:(hf + 1) * DFF], I["w_up"][l][kc * 128:(kc + 1) * 128, hf * DFF:(hf + 1) * DFF], [], ["w_up"], "wl%d" % ((2 * kc + hf) % 4))
        for c4 in range(0, NPAIR, 2):
            DMA("gpsimd", w_dn[:, c4:c4 + 2, :], I["w_down"][l][c4 * 128:(c4 + 2) * 128, :].rearrange("(c p) n -> p c n", p=128), [], ["w_dn"], "wl%d" % ((c4 // 2) % 4))
        DMA("sync", G4[:], g_s[1], [], ["G4"], None)
        E("gpsimd", "memset", [], ["hist"], ap=hist[:], constant=0.0)
        TF = PT[0]
        UB = [[PB[0], PB[1]], [PB[2], PB[3]]]
        Y = [PB[4], PB[5]]
        pidx = 0
        for g in range(8):
            for ti in range(4):
                t = g * 4 + ti
                b2 = t % 2
                xs = xt[t % 3]
                kx = "xt%d" % (t % 3)
                DMA("sync", xs[:], x_mid[t * 128:(t + 1) * 128, :], [], [kx], "xl%d" % (t % 3))
                E("scalar", "activation", [kx], ["junk", "ss%d" % b2], out=junk[:], in_=xs[:], func=AF.Square, accum_out=ss[b2][:])
                rstd_ops(ss[b2][:], rstd[b2][:], 1, ["ss%d" % b2], ["rstd%d" % b2], D)
                E("vector", "tensor_scalar", [kx, "rstd%d" % b2], ["xn%d" % b2], out=xn[b2][:], in0=xs[:], scalar1=rstd[b2][:, 0:1],
                  scalar2=None, op0=ALU.mult)
                for kc in range(8):
                    E("tensor", "transpose", ["xn%d" % b2, "identb"], ["TF"], out=TF[:, kc * 128:(kc + 1) * 128],
                      in_=xn[b2][:, kc * 128:(kc + 1) * 128], identity=identb[:])
                for kc in range(8):
                    E("scalar", "activation", ["TF", "cols2", "cols3"], ["hTg"], out=hTg[:, kc, ti * 128:(ti + 1) * 128],
                      in_=TF[:, kc * 128:(kc + 1) * 128], func=AF.Identity, scale=cols[:, 2, kc:kc + 1], bias=cols[:, 3, kc:kc + 1])
            for i in range(NPAIR):
                pb = pidx % 2
                pidx += 1
                for half, ch in ((0, i), (1, NPAIR + i)):
                    U = UB[pb][half]
                    kU = "PB%d" % (2 * pb + half)
                    ac = acc[pb][half]
                    ka = "acc%d%d" % (pb, half)
                    for kc in range(8):
                        E("tensor", "matmul", ["hTg", "w_up"], [kU], out=U[:], lhsT=w_up[:, kc, ch * 128:(ch + 1) * 128], rhs=hTg[:, kc, :],
                          start=(kc == 0), stop=(kc == 7))
                    E("scalar", "activation", [kU, "cv2", "cv3"], [ka], out=ac[:], in_=U[:], func=AF.Identity, scale=cv_col[:, 2, ch:ch + 1],
                      bias=cv_col[:, 3, ch:ch + 1])
                    E("vector", "scalar_tensor_tensor", [kU, ka, "cv1"], [ka], out=ac[:, 1:512], in0=U[:, 0:511], scalar=cv_col[:, 1, ch:ch + 1],
                      in1=ac[:, 1:512], op0=ALU.mult, op1=ALU.add)
                    E("vector", "scalar_tensor_tensor", [kU, ka, "cv0"], [ka], out=ac[:, 2:512], in0=U[:, 0:510], scalar=cv_col[:, 0, ch:ch + 1],
                      in1=ac[:, 2:512], op0=ALU.mult, op1=ALU.add)
                    if g > 0:
                        E("vector", "scalar_tensor_tensor", ["hist%d" % ch, ka, "cv1"], [ka], out=ac[:, 0:1], in0=hist[:, ch, 1:2],
                          scalar=cv_col[:, 1, ch:ch + 1], in1=ac[:, 0:1], op0=ALU.mult, op1=ALU.add)
                        E("vector", "scalar_tensor_tensor", ["hist%d" % ch, ka, "cv0"], [ka], out=ac[:, 0:2], in0=hist[:, ch, 0:2],
                          scalar=cv_col[:, 0, ch:ch + 1], in1=ac[:, 0:2], op0=ALU.mult, op1=ALU.add)
                    if g < 7:
                        E("vector", "tensor_copy", [kU, ka], ["hist%d" % ch], out=hist[:, ch, :], in_=U[:, 510:512])
                ka0 = "acc%d0" % pb
                ka1 = "acc%d1" % pb
                E("scalar", "activation", [ka0], [ka0], out=acc[pb][0][:], in_=acc[pb][0][:], func=AF.Silu)
                E("gpsimd", "tensor_tensor", [ka0, ka1], ["mT"], out=mT[:, i, :], in0=acc[pb][0][:], in1=acc[pb][1][:], op=ALU.mult)
            for ti in range(4):
                t = g * 4 + ti
                b2 = t % 2
                xs = xt[t % 3]
                kx = "xt%d" % (t % 3)
                for nb in range(2):
                    for i in range(NPAIR):
                        E("tensor", "matmul", ["mT", "w_dn"], ["PB%d" % (4 + nb)], out=Y[nb][:], lhsT=mT[:, i, ti * 128:(ti + 1) * 128],
                          rhs=w_dn[:, i, nb * 512:(nb + 1) * 512], start=(i == 0), stop=(i == NPAIR - 1))
                DMA("sync", xs[:], x_mid[t * 128:(t + 1) * 128, :], [], [kx], "xl%d" % (t % 3))
                post_norm_residual(E, rstd_ops, Y, ["PB4", "PB5"], junk, "junk", ss[b2], "ss%d" % b2, rstd[b2], "rstd%d" % b2, G4, "G4",
                                   tt[0], "tt0", xs, kx, xo[b2], "xo%d" % b2)
                DMA("sync", x_dst[t * 128:(t + 1) * 128, :], xo[b2][:], ["xo%d" % b2], [], "xst%d" % b2)
        x_src = x_dst

    final = [k for k in S.dma_counts.keys()]
    nsem = S.emit(final_wait_semkeys=final)
    return nc


def post_norm_residual(E, rstd_ops, Y, kY, junk, kjunk, ss, kss, rstd, krstd, G, kG, tt, ktt, xs, kx, xo, kxo):
    ssb = ss
    E("scalar", "activation", [kY[0]], [kjunk, kss], out=junk[:, 0:512], in_=Y[0][:], func=AF.Square, accum_out=ssb[:])
    E("scalar", "activation", [kY[1]], [kjunk, krstd], out=junk[:, 512:1024], in_=Y[1][:], func=AF.Square, accum_out=rstd[:])
    E("vector", "tensor_tensor", [kss, krstd], [kss], out=ssb[:], in0=ssb[:], in1=rstd[:], op=ALU.add)
    rstd_ops(ssb[:], rstd[:], 1, [kss], [krstd], D)
    for nb in range(2):
        E("vector", "scalar_tensor_tensor", [kY[nb], krstd, kG], [ktt], out=tt[:, nb * 512:(nb + 1) * 512], in0=Y[nb][:], scalar=rstd[:, 0:1],
          in1=G[:, nb * 512:(nb + 1) * 512], op0=ALU.mult, op1=ALU.mult)
    E("gpsimd", "tensor_tensor", [ktt, kx], [kxo], out=xo[:], in0=tt[:], in1=xs[:], op=ALU.add)


_CONSTS = None


def make_in_maps(inputs):
    global _CONSTS
    if _CONSTS is None:
        _CONSTS = _const_tables()
    x = np.asarray(inputs["x"], dtype=np.float32)
    c = np.asarray(inputs["c"], dtype=np.float32)
    shared = {k: np.ascontiguousarray(np.asarray(inputs[k], dtype=np.float32)) for k in WEIGHT_SPECS if k != "c"}
    maps = []
    for b in range(8):
        m = {"x": np.ascontiguousarray(x[b]), "c": np.ascontiguousarray(c[b:b + 1])}
        m.update(shared)
        m.update(_CONSTS)
        maps.append(m)
    return maps


def kernel(**inputs):
    nc = build()
    maps = make_in_maps(inputs)
    res = run_bass_kernel_spmd(nc, maps, core_ids=list(range(8)))
    return np.stack([np.asarray(r["out"]) for r in res.results], axis=0).astype(np.float32)
```

```python
import contextlib
import os
import numpy as np
import ml_dtypes
import concourse.bass as bass
import concourse.mybir as mybir
from concourse.bass_utils import run_bass_kernel_spmd

F32 = mybir.dt.float32
BF16 = mybir.dt.bfloat16
AF = mybir.ActivationFunctionType
ALU = mybir.AluOpType
AX = mybir.AxisListType

S_LEN = 4096
D = 1024
NT = S_LEN // 128
NH = 8
HD = 64
DFF = 2816
NFC = 2 * DFF // 128
NPAIR = DFF // 128
EPS = 1e-6
BIG = 30000.0
POOL_W = (2, 4, 8, 16)
VW = 68

ENGINES = ("tensor", "vector", "scalar", "gpsimd", "sync")
SEM_LIMIT = 30000


class Op:
    __slots__ = ("eng", "fn", "deps", "is_dma", "ticket", "signal", "idx")

    def __init__(self, eng, fn, is_dma):
        self.idx = 0
        self.eng = eng
        self.fn = fn
        self.is_dma = is_dma
        self.deps = []
        self.ticket = None
        self.signal = False


class Sched:
    def __init__(self, nc):
        self.nc = nc
        self.q = {e: [] for e in ENGINES}
        self.last_w = {}
        self.readers = {}
        self.dma_counts = {}
        self.dma_last = {}
        self.bar = {e: [] for e in ENGINES}

    def barrier(self):
        deps = []
        for e in ENGINES:
            for o in reversed(self.q[e]):
                if not o.is_dma:
                    deps.append(o)
                    break
        deps.extend(self.dma_last.values())
        for e in ENGINES:
            self.bar[e] = list(deps)
        self.last_w = {}
        self.readers = {}

    def op(self, eng, fn, reads=(), writes=(), dma=False, semkey=None):
        o = Op(eng, fn, dma)
        deps = set()
        for k in reads:
            w = self.last_w.get(k)
            if w is not None:
                deps.add(w)
        for k in writes:
            w = self.last_w.get(k)
            if w is not None:
                deps.add(w)
            for r in self.readers.get(k, ()):
                deps.add(r)
        for d in deps:
            if d is o:
                continue
            if (not d.is_dma) and (not dma) and d.eng == eng:
                if eng == "tensor":
                    continue
                raw = any(self.last_w.get(k) is d for k in reads)
                if not raw:
                    continue
            o.deps.append(d)
        if dma and semkey in self.dma_last:
            o.deps.append(self.dma_last[semkey])
        if self.bar[eng]:
            for d in self.bar[eng]:
                if d.is_dma or d.eng != eng:
                    o.deps.append(d)
            self.bar[eng] = []
        best = {}
        pruned = []
        for d in o.deps:
            if d.is_dma:
                pruned.append(d)
            else:
                b = best.get(d.eng)
                if b is None or d.idx > b.idx:
                    best[d.eng] = d
        o.deps = pruned + list(best.values())
        o.idx = len(self.q[eng])
        for k in writes:
            self.last_w[k] = o
            self.readers[k] = []
        for k in reads:
            self.readers.setdefault(k, []).append(o)
        if dma:
            c = self.dma_counts.get(semkey, 0) + 16
            self.dma_counts[semkey] = c
            o.ticket = (("dma", semkey), c)
            self.dma_last[semkey] = o
        self.q[eng].append(o)
        return o

    def emit(self, final_wait_semkeys=()):
        nc = self.nc
        for e in ENGINES:
            for o in self.q[e]:
                for d in o.deps:
                    if not d.is_dma:
                        d.signal = True
        semnames = set()
        for e in ENGINES:
            cnt = 0
            seg = 0
            for o in self.q[e]:
                if o.is_dma:
                    semnames.add(o.ticket[0])
                    continue
                if o.signal:
                    cnt += 1
                    if cnt > SEM_LIMIT:
                        seg += 1
                        cnt = 1
                    o.ticket = (("eng", e, seg), cnt)
                    semnames.add(o.ticket[0])
        semnames = sorted(semnames, key=str)
        with contextlib.ExitStack() as st:
            sems = {}
            for i, n in enumerate(semnames):
                sems[n] = st.enter_context(nc.semaphore("s%d" % i))
            block = st.enter_context(nc.Block())

            def run(engname):
                def body(eng):
                    waited = {}
                    for o in self.q[engname]:
                        need = {}
                        for d in o.deps:
                            s, v = d.ticket
                            if need.get(s, 0) < v:
                                need[s] = v
                        for s, v in need.items():
                            if waited.get(s, 0) < v:
                                eng.wait_ge(sems[s], v)
                                waited[s] = v
                        ins = o.fn(eng)
                        if o.is_dma:
                            ins.then_inc(sems[o.ticket[0]], 16)
                        elif o.signal:
                            ins.then_inc(sems[o.ticket[0]], 1)
                    if engname == "sync":
                        for k in final_wait_semkeys:
                            s = ("dma", k)
                            v = self.dma_counts[k]
                            if waited.get(s, 0) < v:
                                eng.wait_ge(sems[s], v)
                                waited[s] = v
                return body

            block.tensor(run("tensor"))
            block.vector(run("vector"))
            block.scalar(run("scalar"))
            block.gpsimd(run("gpsimd"))
            block.sync(run("sync"))
        return len(semnames)


def _const_tables():
    bf = ml_dtypes.bfloat16
    pos = np.arange(S_LEN, dtype=np.float32)
    inv_freq = np.power(np.float32(500000.0), -np.arange(0, 16, 2, dtype=np.float32) / np.float32(16))
    ang = (pos[:, None] * inv_freq[None, :]).astype(np.float32)
    cos = np.cos(ang).astype(np.float32)
    sin = np.sin(ang).astype(np.float32)
    def tm(a):
        return np.ascontiguousarray(a.reshape(NT, 128, -1).transpose(1, 0, 2))
    ropek = tm(np.concatenate([cos, sin], axis=1)).astype(np.float32)
    ropeq = (ropek * np.float32(0.125)).astype(np.float32)
    qt = np.arange(NT)
    j = qt // 2
    n = np.arange(16)
    past = (n[None, :] < j[:, None])
    pastmask = np.where(past, 0.0, -1e30).astype(np.float32).reshape(1, NT * 16)
    pastind = past.astype(np.float32).reshape(1, NT * 16)
    ownfut = np.where(n[None, :] > j[:, None], -BIG, 0.0).astype(np.float32).reshape(1, NT * 16)
    onehot = (np.arange(S_LEN)[None, :] // 256 == n[:, None]).astype(np.float32).astype(bf)
    k = np.arange(128)
    tri = (k[None, :] >= k[:, None]).astype(np.float32).astype(bf)
    identb = np.eye(128, dtype=np.float32).astype(bf)
    identf = np.eye(128, dtype=np.float32)
    onesb = np.ones((128, 128), dtype=np.float32).astype(bf)
    rc = np.zeros((1, 4, 16), dtype=np.float32)
    for g, w in enumerate(POOL_W):
        rc[0, g, :] = 1.0 / np.minimum(np.arange(16) + 1, w)
    return {
        "ropeq": ropeq, "ropek": ropek,
        "pastmask": np.ascontiguousarray(np.broadcast_to(pastmask, (128, NT * 16))),
        "pastind": np.ascontiguousarray(np.broadcast_to(pastind, (128, NT * 16))),
        "ownfut": np.ascontiguousarray(np.broadcast_to(ownfut, (128, NT * 16))),
        "onehot": onehot, "tri": tri, "identb": identb, "identf": identf, "onesb": onesb,
        "rc": np.ascontiguousarray(np.broadcast_to(rc, (128, 4, 16))),
    }


CONST_SPECS = {
    "ropeq": ([128, NT, 16], F32), "ropek": ([128, NT, 16], F32),
    "pastmask": ([128, NT * 16], F32), "pastind": ([128, NT * 16], F32), "ownfut": ([128, NT * 16], F32),
    "onehot": ([16, S_LEN], BF16), "tri": ([128, 128], BF16), "identb": ([128, 128], BF16),
    "identf": ([128, 128], F32), "onesb": ([128, 128], BF16), "rc": ([128, 4, 16], F32),
}

WEIGHT_SPECS = {
    "c": [1, D], "w_ada": [2, D, 6 * D], "b_ada": [2, 6 * D], "g_pre_mix": [2, D], "w_in": [2, D, 2048],
    "w_pool": [2, 4, 128, 128], "pool_scale": [2, 512], "attn_out_gain": [2, 512], "pool_out_gain": [2, 512],
    "w_out": [2, D, D], "g_post_mix": [2, D], "g_pre_ffn": [2, D], "w_up": [2, D, 2 * DFF],
    "conv_w": [2, 3, 2 * DFF], "conv_b": [2, 2 * DFF], "w_down": [2, DFF, D], "g_post_ffn": [2, D],
}


class Alloc:
    def __init__(self, nc):
        self.nc = nc
        self.base = (nc.sbuf_base + 63) // 64 * 64
        self.top = nc.sbuf_top
        self.cur = self.base
        self.n = 0

    def mark(self):
        return self.cur

    def reset(self, m):
        self.cur = m

    def __call__(self, shape, dt):
        sz = 1
        for s in shape[1:]:
            sz *= s
        nbytes = sz * (4 if dt == F32 else 2)
        nbytes = (nbytes + 63) // 64 * 64
        off = self.cur
        assert off + nbytes <= self.top, ("SBUF overflow", off, nbytes, self.top)
        self.cur += nbytes
        self.n += 1
        return self.nc.alloc_sbuf_tensor_at("sb%d" % self.n, list(shape), dt, offset=off)


def build(n_layers=2, stop_after=None, debug=False):
    nc = bass.Bass("TRN2", target_bir_lowering=False)
    I = {}
    I["x"] = nc.dram_tensor("x", [S_LEN, D], F32, kind="ExternalInput").ap()
    for k, shp in WEIGHT_SPECS.items():
        I[k] = nc.dram_tensor(k, shp, F32, kind="ExternalInput").ap()
    for k, (shp, dt) in CONST_SPECS.items():
        I[k] = nc.dram_tensor(k, shp, dt, kind="ExternalInput").ap()
    out = nc.dram_tensor("out", [S_LEN, D], F32, kind="ExternalOutput").ap()
    sk = "ExternalOutput" if debug else "Internal"
    qT_s = nc.dram_tensor("qT_s", [NH, HD, S_LEN], BF16, kind=sk).ap()
    kT_s = nc.dram_tensor("kT_s", [NH, HD, S_LEN], BF16, kind=sk).ap()
    v_s = nc.dram_tensor("v_s", [S_LEN, NH * VW], BF16, kind=sk).ap()
    pm_s = nc.dram_tensor("pm_s", [NT, 128, 512], BF16, kind=sk).ap()
    g_s = nc.dram_tensor("g_s", [2, 128, D], F32, kind=sk).ap()
    xa = nc.dram_tensor("xa", [S_LEN, D], F32, kind=sk).ap()
    xb = nc.dram_tensor("xb", [S_LEN, D], F32, kind=sk).ap()
    dbg = {}
    if debug:
        dbg["ao"] = nc.dram_tensor("dbg_ao", [S_LEN, 512], F32, kind="ExternalOutput").ap()

    S = Sched(nc)
    A = Alloc(nc)
    PB = [nc.alloc_psum_tensor("pb%d" % i, [128, 512], F32) for i in range(6)]
    PT = [nc.alloc_psum_tensor("pt%d" % i, [128, 1024], BF16) for i in range(2)]

    sec = [None]
    enabled = os.environ.get("KSEC")
    nt_run = int(os.environ.get("KNT", NT))

    def skip():
        return enabled is not None and sec[0] is not None and sec[0] not in enabled

    def E(eng, meth, reads, writes, **kw):
        if skip():
            return None
        return S.op(eng, lambda e: getattr(e, meth)(**kw), reads, writes)

    otc = [0]

    def OT():
        otc[0] += 1
        return "ot%d" % (otc[0] % 6)

    def DMA(eng, out_, in_, reads, writes, semkey, slow=False):
        if skip():
            return None
        if semkey is None:
            semkey = OT()
        if slow:
            return S.op(eng, lambda e: e.dma_start(out=out_, in_=in_, allow_slow_non_contiguous=True),
                        reads, writes, dma=True, semkey=semkey)
        return S.op(eng, lambda e: e.dma_start(out=out_, in_=in_), reads, writes, dma=True, semkey=semkey)

    identb = A([128, 128], BF16)
    identf = A([128, 128], F32)
    onesb = A([128, 128], BF16)
    tri = A([128, 128], BF16)
    for nm, t in (("identb", identb), ("identf", identf), ("onesb", onesb), ("tri", tri)):
        DMA("sync", t[:], I[nm], [], [nm], None)
    cols = A([128, 4, 8], F32)
    aog_col = A([128, 4], F32)
    ps_col = A([128, 4], F32)
    pg_col = A([128, 4], F32)
    cv_col = A([128, 4, NFC], F32)
    rstd_eps = EPS
    persist_mark = A.mark()

    def rstd_ops(ss, rstd, n, keys_r, keys_w, dim):
        E("scalar", "activation", keys_r, keys_w, out=rstd, in_=ss, func=AF.Ln, scale=1.0 / dim, bias=rstd_eps)
        E("scalar", "activation", keys_w, keys_w, out=rstd, in_=rstd, func=AF.Exp, scale=-0.5)

    x_src = I["x"]
    for l in range(n_layers):
        last_layer = (l == n_layers - 1)
        x_mid = xa
        x_dst = out if last_layer else xb
        S.barrier()
        A.reset(persist_mark)
        c_col = A([128, 8], F32)
        cact = A([128, 8], F32)
        cbc = A([128, 8, 128], F32)
        bada = A([128, 6 * D], F32)
        modbc = A([128, 6 * D], F32)
        wblk = [A([128, 8, 512], F32) for _ in range(2)]
        gbc = A([128, D], F32)
        gtmp = A([128, D], F32)
        dtmp = A([128, 8, 128], F32)
        stg = [A([64, 128], F32) for _ in range(2)]
        stc = [0]

        def load_col(vec1d, n, dst, wkey):
            i_ = stc[0] % 2
            stc[0] += 1
            sg = stg[i_]
            DMA("sync", sg[0:n, :], vec1d.rearrange("(c p) -> c p", p=128), [], ["stg%d" % i_], None)
            E("tensor", "matmul", ["stg%d" % i_, "identf"], ["PB2"], out=PB[2][:, 0:n], lhsT=sg[0:n, :], rhs=identf[0:n, 0:n],
              start=True, stop=True)
            E("vector", "tensor_copy", ["PB2"], [wkey], out=dst, in_=PB[2][:, 0:n])

        load_col(I["c"][0], 8, c_col[:], "c_col")
        DMA("sync", bada[:], I["b_ada"][l].partition_broadcast(128), [], ["bada"], None)
        load_col(I["attn_out_gain"][l], 4, aog_col[:], "aog")
        load_col(I["pool_scale"][l], 4, ps_col[:], "psc")
        load_col(I["pool_out_gain"][l], 4, pg_col[:], "pgc")
        for j3 in range(3):
            load_col(I["conv_w"][l, j3], NFC, cv_col[:, j3, :], "cv%d" % j3)
        load_col(I["conv_b"][l], NFC, cv_col[:, 3, :], "cv3")
        E("scalar", "activation", ["c_col"], ["cact"], out=cact[:], in_=c_col[:], func=AF.Silu)
        E("vector", "tensor_copy", ["cact"], ["cbc"], out=cbc[:], in_=cact[:].unsqueeze(2).broadcast_to([128, 8, 128]))
        for blk in range(12):
            wb = wblk[blk % 2]
            kb = "wblk%d" % (blk % 2)
            pm_ = PB[blk % 2]
            kp = "PB%d" % (blk % 2)
            DMA("sync", wb[:], I["w_ada"][l][:, blk * 512:(blk + 1) * 512].rearrange("(kc p) n -> p kc n", p=128),
                [], [kb], "wada%d" % (blk % 2))
            for kc in range(8):
                E("tensor", "matmul", [kb, "cbc"], [kp], out=pm_[:], lhsT=cbc[:, kc, :], rhs=wb[:, kc, :],
                  start=(kc == 0), stop=(kc == 7))
            E("vector", "tensor_tensor", [kp, "bada"], ["modbc"], out=modbc[:, blk * 512:(blk + 1) * 512], in0=pm_[:],
              in1=bada[:, blk * 512:(blk + 1) * 512], op=ALU.add)

        def diag_extract(src_ap, dst_ap, rkeys, wkey):
            E("vector", "tensor_tensor", rkeys + ["identf"], ["dtmp"], out=dtmp[:],
              in0=src_ap.rearrange("p (k q) -> p k q", k=8),
              in1=identf[:].unsqueeze(1).broadcast_to([128, 8, 128]), op=ALU.mult)
            E("vector", "tensor_reduce", ["dtmp"], [wkey], out=dst_ap, in_=dtmp[:], axis=AX.X, op=ALU.add)

        for gi, (gname, moff) in enumerate((("g_post_mix", 2 * D), ("g_post_ffn", 5 * D))):
            DMA("sync", gbc[:], I[gname][l].partition_broadcast(128), [], ["gbc"], None)
            E("vector", "tensor_tensor", ["gbc", "modbc"], ["gtmp"], out=gtmp[:], in0=modbc[:, moff:moff + D], in1=gbc[:], op=ALU.mult)
            DMA("sync", g_s[gi], gtmp[:], ["gtmp"], [], None)
        for ci, (gname, scoff, shoff) in enumerate((("g_pre_mix", 1 * D, 0), ("g_pre_ffn", 4 * D, 3 * D))):
            DMA("sync", gbc[:], I[gname][l].partition_broadcast(128), [], ["gbc"], None)
            E("vector", "scalar_tensor_tensor", ["gbc", "modbc"], ["gtmp"], out=gtmp[:], in0=modbc[:, scoff:scoff + D],
              scalar=1.0, in1=gbc[:], op0=ALU.add, op1=ALU.mult)
            diag_extract(gtmp[:], cols[:, 2 * ci, :], ["gtmp"], "cols%d" % (2 * ci))
            diag_extract(modbc[:, shoff:shoff + D], cols[:, 2 * ci + 1, :], ["modbc"], "cols%d" % (2 * ci + 1))
        if stop_after == "P":
            break

        S.barrier()
        A.reset(persist_mark)
        w_in = A([128, 8, 2048], BF16)
        w_pool = A([128, 4, 128], BF16)
        ropeq = A([128, NT, 16], F32)
        ropek = A([128, NT, 16], F32)
        rc = A([128, 4, 16], F32)
        xt = [A([128, D], F32) for _ in range(3)]
        junk = A([128, D], BF16)
        ss = [A([128, 1], F32) for _ in range(2)]
        rstd = [A([128, 1], F32) for _ in range(2)]
        xn = [A([128, D], BF16) for _ in range(2)]
        hT = [A([128, 8, 128], BF16) for _ in range(2)]
        q_tm = [A([128, 512], BF16) for _ in range(2)]
        k_tm = [A([128, 512], BF16) for _ in range(2)]
        rt = [A([128, 8, 8], F32) for _ in range(4)]
        zq = A([128, 8, 16], F32)
        qst = [A([64, 2, 4, 512], BF16) for _ in range(2)]
        kst = [A([64, 2, 4, 512], BF16) for _ in range(2)]
        vst = [A([128, 8, VW], BF16) for _ in range(2)]
        pmst = [A([128, 4, 128], BF16) for _ in range(2)]
        pz = A([128, 4, 144], F32)
        s2 = A([128, 4, 144], F32)
        s4 = A([128, 4, 144], F32)
        s8 = A([128, 4, 144], F32)
        s16 = A([128, 4, 144], F32)
        pooledT = A([128, 4, 128], BF16)
        po_sb = A([128, 4, 128], F32)
        sq = A([128, 4, 128], BF16)
        prs = A([128, 128], F32)
        ptmp = A([128, 4, 128], F32)
        etmp = A([128, 4, 16], F32)

        sec[0] = "L"
        for kc in range(8):
            DMA("gpsimd", w_in[:, kc, :], I["w_in"][l][kc * 128:(kc + 1) * 128, :], [], ["w_in"], "wl%d" % (kc % 4))
        DMA("gpsimd", w_pool[:], I["w_pool"][l].rearrange("g c e -> c g e"), [], ["w_pool"], "wl0")
        DMA("sync", ropeq[:], I["ropeq"], [], ["ropeq"], None)
        DMA("sync", ropek[:], I["ropek"], [], ["ropek"], None)
        DMA("sync", rc[:], I["rc"], [], ["rc"], None)
        for b_ in range(2):
            E("gpsimd", "memset", [], ["vst%d" % b_], ap=vst[b_][:], constant=1.0)
        E("gpsimd", "memset", [], ["pz"], ap=pz[:], constant=0.0)
        Pq, Pk, Pv, Pp, Py, Ps = PB
        T0, T1 = PT[0], PT[1]
        for t in range(nt_run):
            sec[0] = "A"
            b2 = t % 2
            xs = xt[t % 3]
            kx = "xt%d" % (t % 3)
            g4 = t // 4
            ti = t % 4
            DMA("sync", xs[:], x_src[t * 128:(t + 1) * 128, :], [], [kx], "xl%d" % (t % 3))
            E("scalar", "activation", [kx], ["junk", "ss%d" % b2], out=junk[:], in_=xs[:], func=AF.Square, accum_out=ss[b2][:])
            rstd_ops(ss[b2][:], rstd[b2][:], 1, ["ss%d" % b2], ["rstd%d" % b2], D)
            E("vector", "tensor_scalar", [kx, "rstd%d" % b2], ["xn%d" % b2], out=xn[b2][:], in0=xs[:], scalar1=rstd[b2][:, 0:1],
              scalar2=None, op0=ALU.mult)
            for kc in range(8):
                E("tensor", "transpose", ["xn%d" % b2, "identb"], ["T0"], out=T0[:, kc * 128:(kc + 1) * 128],
                  in_=xn[b2][:, kc * 128:(kc + 1) * 128], identity=identb[:])
            for kc in range(8):
                E("scalar", "activation", ["T0", "cols0", "cols1"], ["hT%d" % b2], out=hT[b2][:, kc, :], in_=T0[:, kc * 128:(kc + 1) * 128],
                  func=AF.Identity, scale=cols[:, 0, kc:kc + 1], bias=cols[:, 1, kc:kc + 1])
            sec[0] = "B"
            for P_, kP, c0 in ((Pq, "Pq", 0), (Pk, "Pk", 512), (Pv, "Pv", 1024)):
                for kc in range(8):
                    E("tensor", "matmul", ["hT%d" % b2, "w_in"], [kP], out=P_[:], lhsT=hT[b2][:, kc, :], rhs=w_in[:, kc, c0:c0 + 512],
                      start=(kc == 0), stop=(kc == 7))
            for g in range(4):
                for kc in range(8):
                    E("tensor", "matmul", ["hT%d" % b2, "w_in"], ["Pp"], out=Pp[:, g * 128:(g + 1) * 128],
                      lhsT=w_in[:, kc, 1536 + g * 128:1536 + (g + 1) * 128], rhs=hT[b2][:, kc, :], start=(kc == 0), stop=(kc == 7))
            sec[0] = "R"
            for P_, kP, tm_, ktm, rope, krope, sc_ in ((Pq, "Pq", q_tm[b2], "q_tm%d" % b2, ropeq, "ropeq", 0.125),
                                                       (Pk, "Pk", k_tm[b2], "k_tm%d" % b2, ropek, "ropek", 1.0)):
                E("scalar", "activation", [kP], [ktm], out=tm_[:], in_=P_[:], func=AF.Identity, scale=sc_)
                pv = P_[:].rearrange("p (h d) -> p h d", h=8)
                ov = tm_[:].rearrange("p (h d) -> p h d", h=8)
                E("scalar", "activation", [kP], ["zq"], out=zq[:], in_=pv[:, :, 0:16], func=AF.Copy)
                cosb = rope[:, t, 0:8].unsqueeze(1).broadcast_to([128, 8, 8])
                sinb = rope[:, t, 8:16].unsqueeze(1).broadcast_to([128, 8, 8])
                x1 = zq[:, :, 0:8]
                x2 = zq[:, :, 8:16]
                sec[0] = "Q"
                E("vector", "tensor_tensor", ["zq", krope], ["rt0"], out=rt[0][:], in0=x1, in1=cosb, op=ALU.mult)
                E("vector", "tensor_tensor", ["zq", krope], ["rt1"], out=rt[1][:], in0=x2, in1=sinb, op=ALU.mult)
                E("vector", "tensor_tensor", ["zq", krope], ["rt2"], out=rt[2][:], in0=x2, in1=cosb, op=ALU.mult)
                E("vector", "tensor_tensor", ["zq", krope], ["rt3"], out=rt[3][:], in0=x1, in1=sinb, op=ALU.mult)
                E("vector", "tensor_tensor", ["rt0", "rt1"], [ktm], out=ov[:, :, 0:8], in0=rt[0][:], in1=rt[1][:], op=ALU.subtract)
                E("vector", "tensor_tensor", ["rt2", "rt3"], [ktm], out=ov[:, :, 8:16], in0=rt[2][:], in1=rt[3][:], op=ALU.add)
                sec[0] = "R"
            sec[0] = "V"
            sb_ = g4 % 2
            vb = t % 2
            E("scalar", "activation", ["Pv"], ["vst%d" % vb], out=vst[vb][:, :, 0:64], in_=Pv[:].rearrange("p (h d) -> p h d", h=8),
              func=AF.Copy)
            DMA("sync", v_s[t * 128:(t + 1) * 128, :], vst[vb][:].rearrange("p h d -> p (h d)"), ["vst%d" % vb], [], "vs%d" % vb)
            sec[0] = "C"
            for tm_, ktm, st_, kst_ in ((q_tm[b2], "q_tm%d" % b2, qst[sb_], "qst%d" % sb_), (k_tm[b2], "k_tm%d" % b2, kst[sb_], "kst%d" % sb_)):
                for pr in range(4):
                    E("tensor", "transpose", [ktm, "identb"], ["T1"], out=T1[:, pr * 128:(pr + 1) * 128],
                      in_=tm_[:, pr * 128:(pr + 1) * 128], identity=identb[:])
                for pa in range(2):
                    sec[0] = "X"
                    E("vector", "tensor_copy", ["T1"], [kst_], out=st_[:, pa, :, ti * 128:(ti + 1) * 128],
                      in_=T1[pa * 64:(pa + 1) * 64, 0:512].rearrange("p (r t) -> p r t", r=4))
                    sec[0] = "C"
            sec[0] = "S"
            if ti == 3:
                for pa in range(2):
                    DMA("sync", qT_s.rearrange("(pr pa) d t -> pa d pr t", pa=2)[pa][:, :, g4 * 512:(g4 + 1) * 512], qst[sb_][:, pa, :, :],
                        ["qst%d" % sb_], [], "qs%d%d" % (sb_, pa))
                    DMA("sync", kT_s.rearrange("(pr pa) d t -> pa d pr t", pa=2)[pa][:, :, g4 * 512:(g4 + 1) * 512], kst[sb_][:, pa, :, :],
                        ["kst%d" % sb_], [], "ks%d%d" % (sb_, pa))
            sec[0] = "D"
            E("scalar", "activation", ["Pp"], ["pz"], out=pz[:, :, 16:144], in_=Pp[:].rearrange("p (g t) -> p g t", g=4), func=AF.Copy)
            E("gpsimd", "tensor_tensor", ["pz"], ["s2"], out=s2[:, :, 1:144], in0=pz[:, :, 1:144], in1=pz[:, :, 0:143], op=ALU.add)
            E("gpsimd", "tensor_tensor", ["s2"], ["s4"], out=s4[:, 1:4, 3:144], in0=s2[:, 1:4, 3:144], in1=s2[:, 1:4, 1:142], op=ALU.add)
            E("gpsimd", "tensor_tensor", ["s4"], ["s8"], out=s8[:, 2:4, 7:144], in0=s4[:, 2:4, 7:144], in1=s4[:, 2:4, 3:140], op=ALU.add)
            E("gpsimd", "tensor_tensor", ["s8"], ["s16"], out=s16[:, 3:4, 15:144], in0=s8[:, 3:4, 15:144], in1=s8[:, 3:4, 7:136], op=ALU.add)
            sums = (s2, s4, s8, s16)
            for g in range(4):
                E("vector", "scalar_tensor_tensor", ["s%d" % (2 << g), "pz"], ["pooledT"], out=pooledT[:, g, :], in0=sums[g][:, g, 16:144],
                  scalar=1.0 / POOL_W[g], in1=pz[:, g, 16:144], op0=ALU.mult, op1=ALU.subtract)
            if t == 0:
                for g in range(4):
                    E("gpsimd", "tensor_tensor", ["s%d" % (2 << g), "rc"], ["etmp"], out=etmp[:, g, :], in0=sums[g][:, g, 16:32], in1=rc[:, g, :], op=ALU.mult)
                    E("gpsimd", "tensor_tensor", ["etmp", "pz"], ["pooledT"], out=pooledT[:, g, 0:16], in0=etmp[:, g, :], in1=pz[:, g, 16:32], op=ALU.subtract)
            E("gpsimd", "tensor_copy", ["pz", "s2", "s4", "s8", "s16", "pooledT"], ["pz"], out=pz[:, :, 0:16], in_=pz[:, :, 128:144])
            for g in range(4):
                E("tensor", "matmul", ["pooledT", "w_pool"], ["Py"], out=Py[:, g * 128:(g + 1) * 128], lhsT=w_pool[:, g, :], rhs=pooledT[:, g, :],
                  start=True, stop=True)
            E("vector", "tensor_tensor", ["Py", "psc"], ["po_sb"], out=po_sb[:], in0=Py[:].rearrange("p (g t) -> p g t", g=4),
              in1=ps_col[:].unsqueeze(2).broadcast_to([128, 4, 128]), op=ALU.mult)
            E("scalar", "activation", ["po_sb"], ["sq"], out=sq[:], in_=po_sb[:], func=AF.Square)
            for g in range(4):
                E("tensor", "matmul", ["sq", "onesb"], ["Ps"], out=Ps[:, 0:128], lhsT=onesb[:], rhs=sq[:, g, :], start=(g == 0), stop=(g == 3))
            rstd_ops(Ps[:, 0:128], prs[:], 128, ["Ps"], ["prs"], 512)
            E("vector", "tensor_tensor", ["po_sb", "pgc"], ["ptmp"], out=ptmp[:], in0=po_sb[:],
              in1=pg_col[:].unsqueeze(2).broadcast_to([128, 4, 128]), op=ALU.mult)
            pb_ = t % 2
            E("vector", "tensor_tensor", ["ptmp", "prs"], ["pmst%d" % pb_], out=pmst[pb_][:], in0=ptmp[:],
              in1=prs[:].unsqueeze(1).broadcast_to([128, 4, 128]), op=ALU.mult)
            DMA("sync", pm_s[t], pmst[pb_][:].rearrange("p g t -> p (g t)"), ["pmst%d" % pb_], [], "pms%d" % pb_)
        sec[0] = None
        if stop_after == "M1":
            break

        S.barrier()
        A.reset(persist_mark)
        ao_all = A([128, NT, 512], F32)
        w_out = A([128, 8, D], BF16)
        G2 = A([128, D], F32)
        m3_mark = A.mark()
        vaug = A([128, NT, NH * VW], BF16)
        kaug = [A([128, S_LEN], BF16) for _ in range(2)]
        qaug = [A([128, S_LEN], BF16) for _ in range(2)]
        kmT = A([64, 16], F32)
        kmTb = A([64, 16], BF16)
        pastmask = A([128, NT * 16], F32)
        pastind = A([128, NT * 16], F32)
        ownfut = A([128, NT * 16], F32)
        Gm = A([128, NT * 16], F32)
        m8 = A([128, NT, 8], F32)
        sel = A([128, NT * 16], F32)
        biasW = A([128, NT, 80], BF16)
        PTb = [A([128, 512], BF16) for _ in range(3)]
        rl = A([128, 4], F32)
        osb = A([128, 512], F32)
        for v4 in range(4):
            DMA("sync", vaug[:, v4 * 8:(v4 + 1) * 8, :], v_s[v4 * 1024:(v4 + 1) * 1024, :].rearrange("(t p) f -> p t f", p=128), [], ["vaug"], None)
        DMA("sync", pastmask[:], I["pastmask"], [], ["pastmask"], None)
        DMA("sync", pastind[:], I["pastind"], [], ["pastind"], None)
        DMA("sync", ownfut[:], I["ownfut"], [], ["ownfut"], None)
        for b_ in range(2):
            DMA("sync", kaug[b_][64:80, :], I["onehot"], [], ["kaug%d" % b_], None)
        E("gpsimd", "memset", [], ["biasW"], ap=biasW[:], constant=0.0)
        for kc in range(8):
            DMA("gpsimd", w_out[:, kc, :], I["w_out"][l][kc * 128:(kc + 1) * 128, :], [], ["w_out"], "wl%d" % (kc % 4))
        DMA("sync", G2[:], g_s[0], [], ["G2"], None)
        SB = PB[0:3]
        OB = PB[3:5]
        GP = PB[5]
        BT = PT[0]
        LA = 2

        def prologue_A(h):
            hb = h % 2
            kk = "kaug%d" % hb
            kq = "qaug%d" % hb
            DMA("sync", kaug[hb][0:64, :], kT_s[h], [], [kk], "kl%d" % hb)
            DMA("sync", qaug[hb][0:64, :], qT_s[h], [], [kq], "ql%d" % hb)
            E("vector", "tensor_reduce", [kk], ["kmT"], out=kmT[:], in_=kaug[hb][0:64, :].rearrange("p (n k) -> p n k", n=16), axis=AX.X, op=ALU.add)
            E("vector", "tensor_scalar", ["kmT"], ["kmTb"], out=kmTb[:], in0=kmT[:], scalar1=1.0 / 256, scalar2=None, op0=ALU.mult)
            for qt_ in range(NT):
                E("tensor", "matmul", [kq, "kmTb"], ["GP"], out=GP[:, qt_ * 16:(qt_ + 1) * 16], lhsT=qaug[hb][0:64, qt_ * 128:(qt_ + 1) * 128],
                  rhs=kmTb[:], start=True, stop=True)
            E("vector", "tensor_tensor", ["GP", "pastmask"], ["Gm"], out=Gm[:], in0=GP[:], in1=pastmask[:], op=ALU.add)
            for qt_ in range(NT):
                E("vector", "max", ["Gm"], ["m8"], out=m8[:, qt_, :], in_=Gm[:, qt_ * 16:(qt_ + 1) * 16])
            E("vector", "tensor_tensor", ["Gm", "m8"], ["sel"], out=sel[:].rearrange("p (q n) -> p q n", n=16),
              in0=Gm[:].rearrange("p (q n) -> p q n", n=16), in1=m8[:, :, 2:3].broadcast_to([128, NT, 16]), op=ALU.is_ge)
            E("vector", "tensor_scalar", ["sel"], ["sel"], out=sel[:], in0=sel[:], scalar1=-1.0, scalar2=BIG, op0=ALU.add, op1=ALU.mult)
            E("vector", "tensor_tensor", ["sel", "pastind"], ["sel"], out=sel[:], in0=sel[:], in1=pastind[:], op=ALU.mult)
            E("vector", "tensor_tensor", ["sel", "ownfut"], ["biasW"], out=biasW[:, :, 64:80], in0=sel[:].rearrange("p (q n) -> p q n", n=16),
              in1=ownfut[:].rearrange("p (q n) -> p q n", n=16), op=ALU.add)

        def prologue_B(h):
            hb = h % 2
            kq = "qaug%d" % hb
            for r4 in range(4):
                for q8 in range(8):
                    qt_ = r4 * 8 + q8
                    E("tensor", "transpose", ["biasW", "identb"], ["BT"], out=BT[0:80, q8 * 128:(q8 + 1) * 128], in_=biasW[:, qt_, :], identity=identb[:])
                E("vector", "tensor_copy", ["BT"], [kq], out=qaug[hb][64:80, r4 * 1024:(r4 + 1) * 1024], in_=BT[64:80, :])

        steps = [(h, g, kt) for h in range(NH) for g in range(8) for kt in range(4 * g + 4)]
        NPH = len(steps) // NH

        def emit_score(i):
            h, g, kt = steps[i]
            hb = h % 2
            kk = "kaug%d" % hb
            kq = "qaug%d" % hb
            sb_ = SB[i % 3]
            ks = "SB%d" % (i % 3)
            pt_ = PTb[i % 3]
            kpt = "PTb%d" % (i % 3)
            E("tensor", "matmul", [kk, kq], [ks], out=sb_[:], lhsT=kaug[hb][0:80, kt * 128:(kt + 1) * 128],
              rhs=qaug[hb][0:80, g * 512:(g + 1) * 512], start=True, stop=True)
            E("scalar", "activation", [ks], [kpt], out=pt_[:], in_=sb_[:], func=AF.Exp)
            r = kt - 4 * g
            if r >= 0:
                E("gpsimd", "tensor_tensor", [kpt, "tri"], [kpt], out=pt_[:, r * 128:(r + 1) * 128], in0=pt_[:, r * 128:(r + 1) * 128],
                  in1=tri[:], op=ALU.mult)

        def emit_pv(i):
            h, g, kt = steps[i]
            gg = h * 8 + g
            ob = OB[gg % 2]
            ko = "OB%d" % (gg % 2)
            pt_ = PTb[i % 3]
            kpt = "PTb%d" % (i % 3)
            for qi in range(4):
                if kt <= 4 * g + qi:
                    E("tensor", "matmul", [kpt, "vaug"], [ko], out=ob[:, qi * 128:qi * 128 + 65], lhsT=pt_[:, qi * 128:(qi + 1) * 128],
                      rhs=vaug[:, kt, h * VW:h * VW + 65], start=(kt == 0 and qi == 0), stop=(kt == 4 * g + qi))
            if kt == 4 * g + 3:
                E("vector", "tensor_copy", [ko], ["osb"], out=osb[:], in_=ob[:])
                obv = osb[:].rearrange("p (q c) -> p q c", q=4)
                E("vector", "reciprocal", ["osb"], ["rl"], out=rl[:].unsqueeze(2), in_=obv[:, :, 64:65])
                E("vector", "tensor_tensor", ["osb", "rl"], ["ao_all"], out=ao_all[:, 4 * g:4 * g + 4, h * 64:(h + 1) * 64], in0=obv[:, :, 0:64],
                  in1=rl[:].unsqueeze(2).broadcast_to([128, 4, 64]), op=ALU.mult)

        prologue_A(0)
        prologue_B(0)
        for i in range(len(steps) + LA):
            if i < len(steps):
                emit_score(i)
            j = i - LA
            if j >= 0:
                emit_pv(j)
                h = steps[j][0]
                pos = j - h * NPH
                if h + 1 < NH:
                    if pos == NPH // 2:
                        prologue_A(h + 1)
                    if pos == NPH - 16:
                        prologue_B(h + 1)
        if debug:
            for v4 in range(4):
                DMA("sync", dbg["ao"][v4 * 1024:(v4 + 1) * 1024, :].rearrange("(t p) f -> p t f", p=128), ao_all[:, v4 * 8:(v4 + 1) * 8, :], ["ao_all"], [], None)
        S.barrier()
        A.reset(m3_mark)
        xt = [A([128, D], F32) for _ in range(3)]
        junk = A([128, D], BF16)
        ss = [A([128, 1], F32) for _ in range(2)]
        rstd = [A([128, 1], F32) for _ in range(2)]
        an = [A([128, 512], BF16) for _ in range(2)]
        mTa = [A([128, 4, 128], BF16) for _ in range(2)]
        pmt = [A([128, 4, 128], BF16) for _ in range(2)]
        tt = [A([128, D], F32) for _ in range(2)]
        xo = [A([128, D], F32) for _ in range(2)]
        Y = [nc_y for nc_y in (PB[0], PB[1])]
        TA = PT[1]
        for t in range(NT):
            b2 = t % 2
            xs = xt[t % 3]
            kx = "xt%d" % (t % 3)
            DMA("sync", xs[:], x_src[t * 128:(t + 1) * 128, :], [], [kx], "xl%d" % (t % 3))
            DMA("sync", pmt[b2][:].rearrange("p g t -> p (g t)"), pm_s[t], [], ["pmt%d" % b2], "pml%d" % b2)
            E("scalar", "activation", ["ao_all"], ["junk", "ss%d" % b2], out=junk[:, 0:512], in_=ao_all[:, t, :], func=AF.Square, accum_out=ss[b2][:])
            rstd_ops(ss[b2][:], rstd[b2][:], 1, ["ss%d" % b2], ["rstd%d" % b2], 512)
            E("vector", "tensor_scalar", ["ao_all", "rstd%d" % b2], ["an%d" % b2], out=an[b2][:], in0=ao_all[:, t, :], scalar1=rstd[b2][:, 0:1],
              scalar2=None, op0=ALU.mult)
            for c4 in range(4):
                E("tensor", "transpose", ["an%d" % b2, "identb"], ["TA"], out=TA[:, c4 * 128:(c4 + 1) * 128], in_=an[b2][:, c4 * 128:(c4 + 1) * 128],
                  identity=identb[:])
            for c4 in range(4):
                E("scalar", "activation", ["TA", "aog"], ["mTa%d" % b2], out=mTa[b2][:, c4, :], in_=TA[:, c4 * 128:(c4 + 1) * 128], func=AF.Identity,
                  scale=aog_col[:, c4:c4 + 1])
            for nb in range(2):
                for c8 in range(8):
                    lhs = mTa[b2][:, c8, :] if c8 < 4 else pmt[b2][:, c8 - 4, :]
                    E("tensor", "matmul", ["mTa%d" % b2, "pmt%d" % b2, "w_out"], ["PB%d" % nb], out=Y[nb][:], lhsT=lhs,
                      rhs=w_out[:, c8, nb * 512:(nb + 1) * 512], start=(c8 == 0), stop=(c8 == 7))
            post_norm_residual(E, rstd_ops, Y, ["PB0", "PB1"], junk, "junk", ss[b2], "ss%d" % b2, rstd[b2], "rstd%d" % b2, G2, "G2",
                               tt[b2], "tt%d" % b2, xs, kx, xo[b2], "xo%d" % b2)
            DMA("sync", x_mid[t * 128:(t + 1) * 128, :], xo[b2][:], ["xo%d" % b2], [], "xst%d" % b2)
        if stop_after == "M3":
            break

        S.barrier()
        A.reset(persist_mark)
        w_up = A([128, 8, 2 * DFF], BF16)
        w_dn = A([128, NPAIR, D], BF16)
        G4 = A([128, D], F32)
        xt = [A([128, D], F32) for _ in range(3)]
        junk = A([128, D], BF16)
        ss = [A([128, 1], F32) for _ in range(2)]
        rstd = [A([128, 1], F32) for _ in range(2)]
        xn = [A([128, D], BF16) for _ in range(2)]
        hTg = A([128, 8, 512], BF16)
        mT = A([128, NPAIR, 512], BF16)
        acc = [[A([128, 512], F32) for _ in range(2)] for _ in range(2)]
        hist = A([128, NFC, 2], F32)
        tt = [A([128, D], F32)]
        xo = [A([128, D], F32) for _ in range(2)]
        for kc in range(8):
            for hf in range(2):
                DMA("gpsimd", w_up[:, kc, hf * DFF:(hf + 1) * DFF], I["w_up"][l][kc * 128:(kc + 1) * 128, hf * DFF:(hf + 1) * DFF], [], ["w_up"], "wl%d" % ((2 * kc + hf) % 4))
        for c4 in range(0, NPAIR, 2):
            DMA("gpsimd", w_dn[:, c4:c4 + 2, :], I["w_down"][l][c4 * 128:(c4 + 2) * 128, :].rearrange("(c p) n -> p c n", p=128), [], ["w_dn"], "wl%d" % ((c4 // 2) % 4))
        DMA("sync", G4[:], g_s[1], [], ["G4"], None)
        E("gpsimd", "memset", [], ["hist"], ap=hist[:], constant=0.0)
        TF = PT[0]
        UB = [[PB[0], PB[1]], [PB[2], PB[3]]]
        Y = [PB[4], PB[5]]
        pidx = 0
        for g in range(8):
            for ti in range(4):
                t = g * 4 + ti
                b2 = t % 2
                xs = xt[t % 3]
                kx = "xt%d" % (t % 3)
                DMA("sync", xs[:], x_mid[t * 128:(t + 1) * 128, :], [], [kx], "xl%d" % (t % 3))
                E("scalar", "activation", [kx], ["junk", "ss%d" % b2], out=junk[:], in_=xs[:], func=AF.Square, accum_out=ss[b2][:])
                rstd_ops(ss[b2][:], rstd[b2][:], 1, ["ss%d" % b2], ["rstd%d" % b2], D)
                E("vector", "tensor_scalar", [kx, "rstd%d" % b2], ["xn%d" % b2], out=xn[b2][:], in0=xs[:], scalar1=rstd[b2][:, 0:1],
                  scalar2=None, op0=ALU.mult)
                for kc in range(8):
                    E("tensor", "transpose", ["xn%d" % b2, "identb"], ["TF"], out=TF[:, kc * 128:(kc + 1) * 128],
                      in_=xn[b2][:, kc * 128:(kc + 1) * 128], identity=identb[:])
                for kc in range(8):
                    E("scalar", "activation", ["TF", "cols2", "cols3"], ["hTg"], out=hTg[:, kc, ti * 128:(ti + 1) * 128],
                      in_=TF[:, kc * 128:(kc + 1) * 128], func=AF.Identity, scale=cols[:, 2, kc:kc + 1], bias=cols[:, 3, kc:kc + 1])
            for i in range(NPAIR):
                pb = pidx % 2
                pidx += 1
                for half, ch in ((0, i), (1, NPAIR + i)):
                    U = UB[pb][half]
                    kU = "PB%d" % (2 * pb + half)
                    ac = acc[pb][half]
                    ka = "acc%d%d" % (pb, half)
                    for kc in range(8):
                        E("tensor", "matmul", ["hTg", "w_up"], [kU], out=U[:], lhsT=w_up[:, kc, ch * 128:(ch + 1) * 128], rhs=hTg[:, kc, :],
                          start=(kc == 0), stop=(kc == 7))
                    E("scalar", "activation", [kU, "cv2", "cv3"], [ka], out=ac[:], in_=U[:], func=AF.Identity, scale=cv_col[:, 2, ch:ch + 1],
                      bias=cv_col[:, 3, ch:ch + 1])
                    E("vector", "scalar_tensor_tensor", [kU, ka, "cv1"], [ka], out=ac[:, 1:512], in0=U[:, 0:511], scalar=cv_col[:, 1, ch:ch + 1],
                      in1=ac[:, 1:512], op0=ALU.mult, op1=ALU.add)
                    E("vector", "scalar_tensor_tensor", [kU, ka, "cv0"], [ka], out=ac[:, 2:512], in0=U[:, 0:510], scalar=cv_col[:, 0, ch:ch + 1],
                      in1=ac[:, 2:512], op0=ALU.mult, op1=ALU.add)
                    if g > 0:
                        E("vector", "scalar_tensor_tensor", ["hist%d" % ch, ka, "cv1"], [ka], out=ac[:, 0:1], in0=hist[:, ch, 1:2],
                          scalar=cv_col[:, 1, ch:ch + 1], in1=ac[:, 0:1], op0=ALU.mult, op1=ALU.add)
                        E("vector", "scalar_tensor_tensor", ["hist%d" % ch, ka, "cv0"], [ka], out=ac[:, 0:2], in0=hist[:, ch, 0:2],
                          scalar=cv_col[:, 0, ch:ch + 1], in1=ac[:, 0:2], op0=ALU.mult, op1=ALU.add)
                    if g < 7:
                        E("vector", "tensor_copy", [kU, ka], ["hist%d" % ch], out=hist[:, ch, :], in_=U[:, 510:512])
                ka0 = "acc%d0" % pb
                ka1 = "acc%d1" % pb
                E("scalar", "activation", [ka0], [ka0], out=acc[pb][0][:], in_=acc[pb][0][:], func=AF.Silu)
                E("gpsimd", "tensor_tensor", [ka0, ka1], ["mT"], out=mT[:, i, :], in0=acc[pb][0][:], in1=acc[pb][1][:], op=ALU.mult)
            for ti in range(4):
                t = g * 4 + ti
                b2 = t % 2
                xs = xt[t % 3]
                kx = "xt%d" % (t % 3)
                for nb in range(2):
                    for i in range(NPAIR):
                        E("tensor", "matmul", ["mT", "w_dn"], ["PB%d" % (4 + nb)], out=Y[nb][:], lhsT=mT[:, i, ti * 128:(ti + 1) * 128],
                          rhs=w_dn[:, i, nb * 512:(nb + 1) * 512], start=(i == 0), stop=(i == NPAIR - 1))
                DMA("sync", xs[:], x_mid[t * 128:(t + 1) * 128, :], [], [kx], "xl%d" % (t % 3))
                post_norm_residual(E, rstd_ops, Y, ["PB4", "PB5"], junk, "junk", ss[b2], "ss%d" % b2, rstd[b2], "rstd%d" % b2, G4, "G4",
                                   tt[0], "tt0", xs, kx, xo[b2], "xo%d" % b2)
                DMA("sync", x_dst[t * 128:(t + 1) * 128, :], xo[b2][:], ["xo%d" % b2], [], "xst%d" % b2)
        x_src = x_dst

    final = [k for k in S.dma_counts.keys()]
    nsem = S.emit(final_wait_semkeys=final)
    return nc


def post_norm_residual(E, rstd_ops, Y, kY, junk, kjunk, ss, kss, rstd, krstd, G, kG, tt, ktt, xs, kx, xo, kxo):
    ssb = ss
    E("scalar", "activation", [kY[0]], [kjunk, kss], out=junk[:, 0:512], in_=Y[0][:], func=AF.Square, accum_out=ssb[:])
    E("scalar", "activation", [kY[1]], [kjunk, krstd], out=junk[:, 512:1024], in_=Y[1][:], func=AF.Square, accum_out=rstd[:])
    E("vector", "tensor_tensor", [kss, krstd], [kss], out=ssb[:], in0=ssb[:], in1=rstd[:], op=ALU.add)
    rstd_ops(ssb[:], rstd[:], 1, [kss], [krstd], D)
    for nb in range(2):
        E("vector", "scalar_tensor_tensor", [kY[nb], krstd, kG], [ktt], out=tt[:, nb * 512:(nb + 1) * 512], in0=Y[nb][:], scalar=rstd[:, 0:1],
          in1=G[:, nb * 512:(nb + 1) * 512], op0=ALU.mult, op1=ALU.mult)
    E("gpsimd", "tensor_tensor", [ktt, kx], [kxo], out=xo[:], in0=tt[:], in1=xs[:], op=ALU.add)


_CONSTS = None


def make_in_maps(inputs):
    global _CONSTS
    if _CONSTS is None:
        _CONSTS = _const_tables()
    x = np.asarray(inputs["x"], dtype=np.float32)
    c = np.asarray(inputs["c"], dtype=np.float32)
    shared = {k: np.ascontiguousarray(np.asarray(inputs[k], dtype=np.float32)) for k in WEIGHT_SPECS if k != "c"}
    maps = []
    for b in range(8):
        m = {"x": np.ascontiguousarray(x[b]), "c": np.ascontiguousarray(c[b:b + 1])}
        m.update(shared)
        m.update(_CONSTS)
        maps.append(m)
    return maps


def kernel(**inputs):
    nc = build()
    maps = make_in_maps(inputs)
    res = run_bass_kernel_spmd(nc, maps, core_ids=list(range(8)))
    return np.stack([np.asarray(r["out"]) for r in res.results], axis=0).astype(np.float32)
```

```python
import contextlib
import os
import numpy as np
import ml_dtypes
import concourse.bass as bass
import concourse.mybir as mybir
from concourse.bass_utils import run_bass_kernel_spmd

F32 = mybir.dt.float32
BF16 = mybir.dt.bfloat16
AF = mybir.ActivationFunctionType
ALU = mybir.AluOpType
AX = mybir.AxisListType

S_LEN = 4096
D = 1024
NT = S_LEN // 128
NH = 8
HD = 64
DFF = 2816
NFC = 2 * DFF // 128
NPAIR = DFF // 128
EPS = 1e-6
BIG = 30000.0
POOL_W = (2, 4, 8, 16)
VW = 68

ENGINES = ("tensor", "vector", "scalar", "gpsimd", "sync")
SEM_LIMIT = 30000


class Op:
    __slots__ = ("eng", "fn", "deps", "is_dma", "ticket", "signal", "idx")

    def __init__(self, eng, fn, is_dma):
        self.idx = 0
        self.eng = eng
        self.fn = fn
        self.is_dma = is_dma
        self.deps = []
        self.ticket = None
        self.signal = False


class Sched:
    def __init__(self, nc):
        self.nc = nc
        self.q = {e: [] for e in ENGINES}
        self.last_w = {}
        self.readers = {}
        self.dma_counts = {}
        self.dma_last = {}
        self.bar = {e: [] for e in ENGINES}

    def barrier(self):
        deps = []
        for e in ENGINES:
            for o in reversed(self.q[e]):
                if not o.is_dma:
                    deps.append(o)
                    break
        deps.extend(self.dma_last.values())
        for e in ENGINES:
            self.bar[e] = list(deps)
        self.last_w = {}
        self.readers = {}

    def op(self, eng, fn, reads=(), writes=(), dma=False, semkey=None):
        o = Op(eng, fn, dma)
        deps = set()
        for k in reads:
            w = self.last_w.get(k)
            if w is not None:
                deps.add(w)
        for k in writes:
            w = self.last_w.get(k)
            if w is not None:
                deps.add(w)
            for r in self.readers.get(k, ()):
                deps.add(r)
        for d in deps:
            if d is o:
                continue
            if (not d.is_dma) and (not dma) and d.eng == eng:
                if eng == "tensor":
                    continue
                raw = any(self.last_w.get(k) is d for k in reads)
                if not raw:
                    continue
            o.deps.append(d)
        if dma and semkey in self.dma_last:
            o.deps.append(self.dma_last[semkey])
        if self.bar[eng]:
            for d in self.bar[eng]:
                if d.is_dma or d.eng != eng:
                    o.deps.append(d)
            self.bar[eng] = []
        best = {}
        pruned = []
        for d in o.deps:
            if d.is_dma:
                pruned.append(d)
            else:
                b = best.get(d.eng)
                if b is None or d.idx > b.idx:
                    best[d.eng] = d
        o.deps = pruned + list(best.values())
        o.idx = len(self.q[eng])
        for k in writes:
            self.last_w[k] = o
            self.readers[k] = []
        for k in reads:
            self.readers.setdefault(k, []).append(o)
        if dma:
            c = self.dma_counts.get(semkey, 0) + 16
            self.dma_counts[semkey] = c
            o.ticket = (("dma", semkey), c)
            self.dma_last[semkey] = o
        self.q[eng].append(o)
        return o

    def emit(self, final_wait_semkeys=()):
        nc = self.nc
        for e in ENGINES:
            for o in self.q[e]:
                for d in o.deps:
                    if not d.is_dma:
                        d.signal = True
        semnames = set()
        for e in ENGINES:
            cnt = 0
            seg = 0
            for o in self.q[e]:
                if o.is_dma:
                    semnames.add(o.ticket[0])
                    continue
                if o.signal:
                    cnt += 1
                    if cnt > SEM_LIMIT:
                        seg += 1
                        cnt = 1
                    o.ticket = (("eng", e, seg), cnt)
                    semnames.add(o.ticket[0])
        semnames = sorted(semnames, key=str)
        with contextlib.ExitStack() as st:
            sems = {}
            for i, n in enumerate(semnames):
                sems[n] = st.enter_context(nc.semaphore("s%d" % i))
            block = st.enter_context(nc.Block())

            def run(engname):
                def body(eng):
                    waited = {}
                    for o in self.q[engname]:
                        need = {}
                        for d in o.deps:
                            s, v = d.ticket
                            if need.get(s, 0) < v:
                                need[s] = v
                        for s, v in need.items():
                            if waited.get(s, 0) < v:
                                eng.wait_ge(sems[s], v)
                                waited[s] = v
                        ins = o.fn(eng)
                        if o.is_dma:
                            ins.then_inc(sems[o.ticket[0]], 16)
                        elif o.signal:
                            ins.then_inc(sems[o.ticket[0]], 1)
                    if engname == "sync":
                        for k in final_wait_semkeys:
                            s = ("dma", k)
                            v = self.dma_counts[k]
                            if waited.get(s, 0) < v:
                                eng.wait_ge(sems[s], v)
                                waited[s] = v
                return body

            block.tensor(run("tensor"))
            block.vector(run("vector"))
            block.scalar(run("scalar"))
            block.gpsimd(run("gpsimd"))
            block.sync(run("sync"))
        return len(semnames)


def _const_tables():
    bf = ml_dtypes.bfloat16
    pos = np.arange(S_LEN, dtype=np.float32)
    inv_freq = np.power(np.float32(500000.0), -np.arange(0, 16, 2, dtype=np.float32) / np.float32(16))
    ang = (pos[:, None] * inv_freq[None, :]).astype(np.float32)
    cos = np.cos(ang).astype(np.float32)
    sin = np.sin(ang).astype(np.float32)
    def tm(a):
        return np.ascontiguousarray(a.reshape(NT, 128, -1).transpose(1, 0, 2))
    ropek = tm(np.concatenate([cos, sin], axis=1)).astype(np.float32)
    ropeq = (ropek * np.float32(0.125)).astype(np.float32)
    qt = np.arange(NT)
    j = qt // 2
    n = np.arange(16)
    past = (n[None, :] < j[:, None])
    pastmask = np.where(past, 0.0, -1e30).astype(np.float32).reshape(1, NT * 16)
    pastind = past.astype(np.float32).reshape(1, NT * 16)
    ownfut = np.where(n[None, :] > j[:, None], -BIG, 0.0).astype(np.float32).reshape(1, NT * 16)
    onehot = (np.arange(S_LEN)[None, :] // 256 == n[:, None]).astype(np.float32).astype(bf)
    k = np.arange(128)
    tri = (k[None, :] >= k[:, None]).astype(np.float32).astype(bf)
    identb = np.eye(128, dtype=np.float32).astype(bf)
    identf = np.eye(128, dtype=np.float32)
    onesb = np.ones((128, 128), dtype=np.float32).astype(bf)
    rc = np.zeros((1, 4, 16), dtype=np.float32)
    for g, w in enumerate(POOL_W):
        rc[0, g, :] = 1.0 / np.minimum(np.arange(16) + 1, w)
    return {
        "ropeq": ropeq, "ropek": ropek,
        "pastmask": np.ascontiguousarray(np.broadcast_to(pastmask, (128, NT * 16))),
        "pastind": np.ascontiguousarray(np.broadcast_to(pastind, (128, NT * 16))),
        "ownfut": np.ascontiguousarray(np.broadcast_to(ownfut, (128, NT * 16))),
        "onehot": onehot, "tri": tri, "identb": identb, "identf": identf, "onesb": onesb,
        "rc": np.ascontiguousarray(np.broadcast_to(rc, (128, 4, 16))),
    }


CONST_SPECS = {
    "ropeq": ([128, NT, 16], F32), "ropek": ([128, NT, 16], F32),
    "pastmask": ([128, NT * 16], F32), "pastind": ([128, NT * 16], F32), "ownfut": ([128, NT * 16], F32),
    "onehot": ([16, S_LEN], BF16), "tri": ([128, 128], BF16), "identb": ([128, 128], BF16),
    "identf": ([128, 128], F32), "onesb": ([128, 128], BF16), "rc": ([128, 4, 16], F32),
}

WEIGHT_SPECS = {
    "c": [1, D], "w_ada": [2, D, 6 * D], "b_ada": [2, 6 * D], "g_pre_mix": [2, D], "w_in": [2, D, 2048],
    "w_pool": [2, 4, 128, 128], "pool_scale": [2, 512], "attn_out_gain": [2, 512], "pool_out_gain": [2, 512],
    "w_out": [2, D, D], "g_post_mix": [2, D], "g_pre_ffn": [2, D], "w_up": [2, D, 2 * DFF],
    "conv_w": [2, 3, 2 * DFF], "conv_b": [2, 2 * DFF], "w_down": [2, DFF, D], "g_post_ffn": [2, D],
}


class Alloc:
    def __init__(self, nc):
        self.nc = nc
        self.base = (nc.sbuf_base + 63) // 64 * 64
        self.top = nc.sbuf_top
        self.cur = self.base
        self.n = 0

    def mark(self):
        return self.cur

    def reset(self, m):
        self.cur = m

    def __call__(self, shape, dt):
        sz = 1
        for s in shape[1:]:
            sz *= s
        nbytes = sz * (4 if dt == F32 else 2)
        nbytes = (nbytes + 63) // 64 * 64
        off = self.cur
        assert off + nbytes <= self.top, ("SBUF overflow", off, nbytes, self.top)
        self.cur += nbytes
        self.n += 1
        return self.nc.alloc_sbuf_tensor_at("sb%d" % self.n, list(shape), dt, offset=off)


def build(n_layers=2, stop_after=None, debug=False):
    nc = bass.Bass("TRN2", target_bir_lowering=False)
    I = {}
    I["x"] = nc.dram_tensor("x", [S_LEN, D], F32, kind="ExternalInput").ap()
    for k, shp in WEIGHT_SPECS.items():
        I[k] = nc.dram_tensor(k, shp, F32, kind="ExternalInput").ap()
    for k, (shp, dt) in CONST_SPECS.items():
        I[k] = nc.dram_tensor(k, shp, dt, kind="ExternalInput").ap()
    out = nc.dram_tensor("out", [S_LEN, D], F32, kind="ExternalOutput").ap()
    sk = "ExternalOutput" if debug else "Internal"
    qT_s = nc.dram_tensor("qT_s", [NH, HD, S_LEN], BF16, kind=sk).ap()
    kT_s = nc.dram_tensor("kT_s", [NH, HD, S_LEN], BF16, kind=sk).ap()
    v_s = nc.dram_tensor("v_s", [S_LEN, NH * VW], BF16, kind=sk).ap()
    pm_s = nc.dram_tensor("pm_s", [NT, 128, 512], BF16, kind=sk).ap()
    g_s = nc.dram_tensor("g_s", [2, 128, D], F32, kind=sk).ap()
    xa = nc.dram_tensor("xa", [S_LEN, D], F32, kind=sk).ap()
    xb = nc.dram_tensor("xb", [S_LEN, D], F32, kind=sk).ap()
    dbg = {}
    if debug:
        dbg["ao"] = nc.dram_tensor("dbg_ao", [S_LEN, 512], F32, kind="ExternalOutput").ap()

    S = Sched(nc)
    A = Alloc(nc)
    PB = [nc.alloc_psum_tensor("pb%d" % i, [128, 512], F32) for i in range(6)]
    PT = [nc.alloc_psum_tensor("pt%d" % i, [128, 1024], BF16) for i in range(2)]

    sec = [None]
    enabled = os.environ.get("KSEC")
    nt_run = int(os.environ.get("KNT", NT))

    def skip():
        return enabled is not None and sec[0] is not None and sec[0] not in enabled

    def E(eng, meth, reads, writes, **kw):
        if skip():
            return None
        return S.op(eng, lambda e: getattr(e, meth)(**kw), reads, writes)

    otc = [0]

    def OT():
        otc[0] += 1
        return "ot%d" % (otc[0] % 6)

    def DMA(eng, out_, in_, reads, writes, semkey, slow=False):
        if skip():
            return None
        if semkey is None:
            semkey = OT()
        if slow:
            return S.op(eng, lambda e: e.dma_start(out=out_, in_=in_, allow_slow_non_contiguous=True),
                        reads, writes, dma=True, semkey=semkey)
        return S.op(eng, lambda e: e.dma_start(out=out_, in_=in_), reads, writes, dma=True, semkey=semkey)

    identb = A([128, 128], BF16)
    identf = A([128, 128], F32)
    onesb = A([128, 128], BF16)
    tri = A([128, 128], BF16)
    for nm, t in (("identb", identb), ("identf", identf), ("onesb", onesb), ("tri", tri)):
        DMA("sync", t[:], I[nm], [], [nm], None)
    cols = A([128, 4, 8], F32)
    aog_col = A([128, 4], F32)
    ps_col = A([128, 4], F32)
    pg_col = A([128, 4], F32)
    cv_col = A([128, 4, NFC], F32)
    rstd_eps = EPS
    persist_mark = A.mark()

    def rstd_ops(ss, rstd, n, keys_r, keys_w, dim):
        E("scalar", "activation", keys_r, keys_w, out=rstd, in_=ss, func=AF.Ln, scale=1.0 / dim, bias=rstd_eps)
        E("scalar", "activation", keys_w, keys_w, out=rstd, in_=rstd, func=AF.Exp, scale=-0.5)

    x_src = I["x"]
    for l in range(n_layers):
        last_layer = (l == n_layers - 1)
        x_mid = xa
        x_dst = out if last_layer else xb
        S.barrier()
        A.reset(persist_mark)
        c_col = A([128, 8], F32)
        cact = A([128, 8], F32)
        cbc = A([128, 8, 128], F32)
        bada = A([128, 6 * D], F32)
        modbc = A([128, 6 * D], F32)
        wblk = [A([128, 8, 512], F32) for _ in range(2)]
        gbc = A([128, D], F32)
        gtmp = A([128, D], F32)
        dtmp = A([128, 8, 128], F32)
        stg = [A([64, 128], F32) for _ in range(2)]
        stc = [0]

        def load_col(vec1d, n, dst, wkey):
            i_ = stc[0] % 2
            stc[0] += 1
            sg = stg[i_]
            DMA("sync", sg[0:n, :], vec1d.rearrange("(c p) -> c p", p=128), [], ["stg%d" % i_], None)
            E("tensor", "matmul", ["stg%d" % i_, "identf"], ["PB2"], out=PB[2][:, 0:n], lhsT=sg[0:n, :], rhs=identf[0:n, 0:n],
              start=True, stop=True)
            E("vector", "tensor_copy", ["PB2"], [wkey], out=dst, in_=PB[2][:, 0:n])

        load_col(I["c"][0], 8, c_col[:], "c_col")
        DMA("sync", bada[:], I["b_ada"][l].partition_broadcast(128), [], ["bada"], None)
        load_col(I["attn_out_gain"][l], 4, aog_col[:], "aog")
        load_col(I["pool_scale"][l], 4, ps_col[:], "psc")
        load_col(I["pool_out_gain"][l], 4, pg_col[:], "pgc")
        for j3 in range(3):
            load_col(I["conv_w"][l, j3], NFC, cv_col[:, j3, :], "cv%d" % j3)
        load_col(I["conv_b"][l], NFC, cv_col[:, 3, :], "cv3")
        E("scalar", "activation", ["c_col"], ["cact"], out=cact[:], in_=c_col[:], func=AF.Silu)
        E("vector", "tensor_copy", ["cact"], ["cbc"], out=cbc[:], in_=cact[:].unsqueeze(2).broadcast_to([128, 8, 128]))
        for blk in range(12):
            wb = wblk[blk % 2]
            kb = "wblk%d" % (blk % 2)
            pm_ = PB[blk % 2]
            kp = "PB%d" % (blk % 2)
            DMA("sync", wb[:], I["w_ada"][l][:, blk * 512:(blk + 1) * 512].rearrange("(kc p) n -> p kc n", p=128),
                [], [kb], "wada%d" % (blk % 2))
            for kc in range(8):
                E("tensor", "matmul", [kb, "cbc"], [kp], out=pm_[:], lhsT=cbc[:, kc, :], rhs=wb[:, kc, :],
                  start=(kc == 0), stop=(kc == 7))
            E("vector", "tensor_tensor", [kp, "bada"], ["modbc"], out=modbc[:, blk * 512:(blk + 1) * 512], in0=pm_[:],
              in1=bada[:, blk * 512:(blk + 1) * 512], op=ALU.add)

        def diag_extract(src_ap, dst_ap, rkeys, wkey):
            E("vector", "tensor_tensor", rkeys + ["identf"], ["dtmp"], out=dtmp[:],
              in0=src_ap.rearrange("p (k q) -> p k q", k=8),
              in1=identf[:].unsqueeze(1).broadcast_to([128, 8, 128]), op=ALU.mult)
            E("vector", "tensor_reduce", ["dtmp"], [wkey], out=dst_ap, in_=dtmp[:], axis=AX.X, op=ALU.add)

        for gi, (gname, moff) in enumerate((("g_post_mix", 2 * D), ("g_post_ffn", 5 * D))):
            DMA("sync", gbc[:], I[gname][l].partition_broadcast(128), [], ["gbc"], None)
            E("vector", "tensor_tensor", ["gbc", "modbc"], ["gtmp"], out=gtmp[:], in0=modbc[:, moff:moff + D], in1=gbc[:], op=ALU.mult)
            DMA("sync", g_s[gi], gtmp[:], ["gtmp"], [], None)
        for ci, (gname, scoff, shoff) in enumerate((("g_pre_mix", 1 * D, 0), ("g_pre_ffn", 4 * D, 3 * D))):
            DMA("sync", gbc[:], I[gname][l].partition_broadcast(128), [], ["gbc"], None)
            E("vector", "scalar_tensor_tensor", ["gbc", "modbc"], ["gtmp"], out=gtmp[:], in0=modbc[:, scoff:scoff + D],
              scalar=1.0, in1=gbc[:], op0=ALU.add, op1=ALU.mult)
            diag_extract(gtmp[:], cols[:, 2 * ci, :], ["gtmp"], "cols%d" % (2 * ci))
            diag_extract(modbc[:, shoff:shoff + D], cols[:, 2 * ci + 1, :], ["modbc"], "cols%d" % (2 * ci + 1))
        if stop_after == "P":
            break

        S.barrier()
        A.reset(persist_mark)
        w_in = A([128, 8, 2048], BF16)
        w_pool = A([128, 4, 128], BF16)
        ropeq = A([128, NT, 16], F32)
        ropek = A([128, NT, 16], F32)
        rc = A([128, 4, 16], F32)
        xt = [A([128, D], F32) for _ in range(3)]
        junk = A([128, D], BF16)
        ss = [A([128, 1], F32) for _ in range(3)]
        rstd = [A([128, 1], F32) for _ in range(3)]
        xn = [A([128, D], BF16) for _ in range(3)]
        hT = [A([128, 8, 128], BF16) for _ in range(2)]
        q_tm = [A([128, 512], BF16) for _ in range(2)]
        k_tm = [A([128, 512], BF16) for _ in range(2)]
        rt = [A([128, 8, 8], F32) for _ in range(4)]
        zq = A([128, 8, 16], F32)
        qst = [A([64, 2, 4, 512], BF16) for _ in range(2)]
        kst = [A([64, 2, 4, 512], BF16) for _ in range(2)]
        vst = [A([128, 8, VW], BF16) for _ in range(2)]
        pmst = [A([128, 4, 128], BF16) for _ in range(2)]
        pz = A([128, 4, 144], F32)
        s2 = A([128, 4, 144], F32)
        s4 = A([128, 4, 144], F32)
        s8 = A([128, 4, 144], F32)
        s16 = A([128, 4, 144], F32)
        pooledT = [A([128, 4, 128], BF16) for _ in range(2)]
        po_sb = A([128, 4, 128], F32)
        sq = A([128, 4, 128], BF16)
        prs = A([128, 128], F32)
        ptmp = A([128, 4, 128], F32)
        etmp = A([128, 4, 16], F32)

        sec[0] = "L"
        for kc in range(8):
            DMA("gpsimd", w_in[:, kc, :], I["w_in"][l][kc * 128:(kc + 1) * 128, :], [], ["w_in"], "wl%d" % (kc % 4))
        DMA("gpsimd", w_pool[:], I["w_pool"][l].rearrange("g c e -> c g e"), [], ["w_pool"], "wl0")
        DMA("sync", ropeq[:], I["ropeq"], [], ["ropeq"], None)
        DMA("sync", ropek[:], I["ropek"], [], ["ropek"], None)
        DMA("sync", rc[:], I["rc"], [], ["rc"], None)
        for b_ in range(2):
            E("gpsimd", "memset", [], ["vst%d" % b_], ap=vst[b_][:], constant=1.0)
        E("gpsimd", "memset", [], ["pz"], ap=pz[:], constant=0.0)
        Pq, Pk, Pv, Pp, Py, Ps = PB
        T0, T1 = PT[0], PT[1]
        def m1_L(t):
            DMA("sync", xt[t % 3][:], x_src[t * 128:(t + 1) * 128, :], [], ["xt%d" % (t % 3)], "xl%d" % (t % 3))

        def m1_N(t):
            b3 = t % 3
            xs = xt[b3]
            kx = "xt%d" % b3
            E("scalar", "activation", [kx], ["junk", "ss%d" % b3], out=junk[:], in_=xs[:], func=AF.Square, accum_out=ss[b3][:])
            rstd_ops(ss[b3][:], rstd[b3][:], 1, ["ss%d" % b3], ["rstd%d" % b3], D)
            E("vector", "tensor_scalar", [kx, "rstd%d" % b3], ["xn%d" % b3], out=xn[b3][:], in0=xs[:], scalar1=rstd[b3][:, 0:1],
              scalar2=None, op0=ALU.mult)

        def m1_T(t):
            b3 = t % 3
            b2 = t % 2
            for kc in range(8):
                E("tensor", "transpose", ["xn%d" % b3, "identb"], ["T0"], out=T0[:, kc * 128:(kc + 1) * 128],
                  in_=xn[b3][:, kc * 128:(kc + 1) * 128], identity=identb[:])
            for kc in range(8):
                E("scalar", "activation", ["T0", "cols0", "cols1"], ["hT%d" % b2], out=hT[b2][:, kc, :], in_=T0[:, kc * 128:(kc + 1) * 128],
                  func=AF.Identity, scale=cols[:, 0, kc:kc + 1], bias=cols[:, 1, kc:kc + 1])

        def m1_B1(t):
            b2 = t % 2
            for P_, kP, c0 in ((Pq, "Pq", 0), (Pk, "Pk", 512), (Pv, "Pv", 1024)):
                for kc in range(8):
                    E("tensor", "matmul", ["hT%d" % b2, "w_in"], [kP], out=P_[:], lhsT=hT[b2][:, kc, :], rhs=w_in[:, kc, c0:c0 + 512],
                      start=(kc == 0), stop=(kc == 7))
            for g in range(4):
                for kc in range(8):
                    E("tensor", "matmul", ["hT%d" % b2, "w_in"], ["Pp"], out=Pp[:, g * 128:(g + 1) * 128],
                      lhsT=w_in[:, kc, 1536 + g * 128:1536 + (g + 1) * 128], rhs=hT[b2][:, kc, :], start=(kc == 0), stop=(kc == 7))
            for P_, kP, tm_, ktm, rope, krope, sc_ in ((Pq, "Pq", q_tm[b2], "q_tm%d" % b2, ropeq, "ropeq", 0.125),
                                                       (Pk, "Pk", k_tm[b2], "k_tm%d" % b2, ropek, "ropek", 1.0)):
                E("scalar", "activation", [kP], [ktm], out=tm_[:], in_=P_[:], func=AF.Identity, scale=sc_)
                pv = P_[:].rearrange("p (h d) -> p h d", h=8)
                ov = tm_[:].rearrange("p (h d) -> p h d", h=8)
                E("scalar", "activation", [kP], ["zq"], out=zq[:], in_=pv[:, :, 0:16], func=AF.Copy)
                cosb = rope[:, t, 0:8].unsqueeze(1).broadcast_to([128, 8, 8])
                sinb = rope[:, t, 8:16].unsqueeze(1).broadcast_to([128, 8, 8])
                x1 = zq[:, :, 0:8]
                x2 = zq[:, :, 8:16]
                E("vector", "tensor_tensor", ["zq", krope], ["rt0"], out=rt[0][:], in0=x1, in1=cosb, op=ALU.mult)
                E("vector", "tensor_tensor", ["zq", krope], ["rt1"], out=rt[1][:], in0=x2, in1=sinb, op=ALU.mult)
                E("vector", "tensor_tensor", ["zq", krope], ["rt2"], out=rt[2][:], in0=x2, in1=cosb, op=ALU.mult)
                E("vector", "tensor_tensor", ["zq", krope], ["rt3"], out=rt[3][:], in0=x1, in1=sinb, op=ALU.mult)
                E("vector", "tensor_tensor", ["rt0", "rt1"], [ktm], out=ov[:, :, 0:8], in0=rt[0][:], in1=rt[1][:], op=ALU.subtract)
                E("vector", "tensor_tensor", ["rt2", "rt3"], [ktm], out=ov[:, :, 8:16], in0=rt[2][:], in1=rt[3][:], op=ALU.add)
            vb = t % 2
            E("scalar", "activation", ["Pv"], ["vst%d" % vb], out=vst[vb][:, :, 0:64], in_=Pv[:].rearrange("p (h d) -> p h d", h=8),
              func=AF.Copy)
            DMA("sync", v_s[t * 128:(t + 1) * 128, :], vst[vb][:].rearrange("p h d -> p (h d)"), ["vst%d" % vb], [], "vs%d" % vb)
            pT = pooledT[b2]
            kpT = "pooledT%d" % b2
            E("scalar", "activation", ["Pp"], ["pz"], out=pz[:, :, 16:144], in_=Pp[:].rearrange("p (g t) -> p g t", g=4), func=AF.Copy)
            E("gpsimd", "tensor_tensor", ["pz"], ["s2"], out=s2[:, :, 1:144], in0=pz[:, :, 1:144], in1=pz[:, :, 0:143], op=ALU.add)
            E("gpsimd", "tensor_tensor", ["s2"], ["s4"], out=s4[:, 1:4, 3:144], in0=s2[:, 1:4, 3:144], in1=s2[:, 1:4, 1:142], op=ALU.add)
            E("gpsimd", "tensor_tensor", ["s4"], ["s8"], out=s8[:, 2:4, 7:144], in0=s4[:, 2:4, 7:144], in1=s4[:, 2:4, 3:140], op=ALU.add)
            E("gpsimd", "tensor_tensor", ["s8"], ["s16"], out=s16[:, 3:4, 15:144], in0=s8[:, 3:4, 15:144], in1=s8[:, 3:4, 7:136], op=ALU.add)
            sums = (s2, s4, s8, s16)
            for g in range(4):
                E("vector", "scalar_tensor_tensor", ["s%d" % (2 << g), "pz"], [kpT], out=pT[:, g, :], in0=sums[g][:, g, 16:144],
                  scalar=1.0 / POOL_W[g], in1=pz[:, g, 16:144], op0=ALU.mult, op1=ALU.subtract)
            if t == 0:
                for g in range(4):
                    E("gpsimd", "tensor_tensor", ["s%d" % (2 << g), "rc"], ["etmp"], out=etmp[:, g, :], in0=sums[g][:, g, 16:32], in1=rc[:, g, :], op=ALU.mult)
                    E("gpsimd", "tensor_tensor", ["etmp", "pz"], [kpT], out=pT[:, g, 0:16], in0=etmp[:, g, :], in1=pz[:, g, 16:32], op=ALU.subtract)
            E("gpsimd", "tensor_copy", ["pz", "s2", "s4", "s8", "s16", kpT], ["pz"], out=pz[:, :, 0:16], in_=pz[:, :, 128:144])

        def m1_B2(t):
            b2 = t % 2
            g4 = t // 4
            ti = t % 4
            sb_ = g4 % 2
            pT = pooledT[b2]
            kpT = "pooledT%d" % b2
            for g in range(4):
                E("tensor", "matmul", [kpT, "w_pool"], ["Py"], out=Py[:, g * 128:(g + 1) * 128], lhsT=w_pool[:, g, :], rhs=pT[:, g, :],
                  start=True, stop=True)
            for hi, (tm_, ktm, st_, kst_) in enumerate(((q_tm[b2], "q_tm%d" % b2, qst[sb_], "qst%d" % sb_), (k_tm[b2], "k_tm%d" % b2, kst[sb_], "kst%d" % sb_))):
                kT1 = "T1"
                for pr in range(4):
                    E("tensor", "transpose", [ktm, "identb"], [kT1], out=T1[:, hi * 512 + pr * 128:hi * 512 + (pr + 1) * 128],
                      in_=tm_[:, pr * 128:(pr + 1) * 128], identity=identb[:])
                for pa in range(2):
                    E("vector", "tensor_copy", [kT1], [kst_], out=st_[:, pa, :, ti * 128:(ti + 1) * 128],
                      in_=T1[pa * 64:(pa + 1) * 64, hi * 512:(hi + 1) * 512].rearrange("p (r t) -> p r t", r=4))
            if ti == 3:
                for pa in range(2):
                    DMA("sync", qT_s.rearrange("(pr pa) d t -> pa d pr t", pa=2)[pa][:, :, g4 * 512:(g4 + 1) * 512], qst[sb_][:, pa, :, :],
                        ["qst%d" % sb_], [], "qs%d%d" % (sb_, pa))
                    DMA("sync", kT_s.rearrange("(pr pa) d t -> pa d pr t", pa=2)[pa][:, :, g4 * 512:(g4 + 1) * 512], kst[sb_][:, pa, :, :],
                        ["kst%d" % sb_], [], "ks%d%d" % (sb_, pa))
            E("vector", "tensor_tensor", ["Py", "psc"], ["po_sb"], out=po_sb[:], in0=Py[:].rearrange("p (g t) -> p g t", g=4),
              in1=ps_col[:].unsqueeze(2).broadcast_to([128, 4, 128]), op=ALU.mult)
            E("scalar", "activation", ["po_sb"], ["sq"], out=sq[:], in_=po_sb[:], func=AF.Square)
            for g in range(4):
                E("tensor", "matmul", ["sq", "onesb"], ["Ps"], out=Ps[:, 0:128], lhsT=onesb[:], rhs=sq[:, g, :], start=(g == 0), stop=(g == 3))
            rstd_ops(Ps[:, 0:128], prs[:], 128, ["Ps"], ["prs"], 512)
            E("vector", "tensor_tensor", ["po_sb", "pgc"], ["ptmp"], out=ptmp[:], in0=po_sb[:],
              in1=pg_col[:].unsqueeze(2).broadcast_to([128, 4, 128]), op=ALU.mult)
            pb_ = t % 2
            E("vector", "tensor_tensor", ["ptmp", "prs"], ["pmst%d" % pb_], out=pmst[pb_][:], in0=ptmp[:],
              in1=prs[:].unsqueeze(1).broadcast_to([128, 4, 128]), op=ALU.mult)
            DMA("sync", pm_s[t], pmst[pb_][:].rearrange("p g t -> p (g t)"), ["pmst%d" % pb_], [], "pms%d" % pb_)

        sec[0] = None
        for t0_ in range(min(3, NT)):
            m1_L(t0_)
        m1_N(0)
        m1_N(1)
        m1_T(0)
        for t in range(NT):
            if t + 3 < NT:
                m1_L(t + 3)
            if t + 2 < NT:
                m1_N(t + 2)
            if t + 1 < NT:
                m1_T(t + 1)
            m1_B1(t)
            if t >= 1:
                m1_B2(t - 1)
        m1_B2(NT - 1)
        sec[0] = None
        if stop_after == "M1":
            break

        S.barrier()
        A.reset(persist_mark)
        ao_all = A([128, NT, 512], F32)
        w_out = A([128, 8, D], BF16)
        G2 = A([128, D], F32)
        m3_mark = A.mark()
        vaug = A([128, NT, NH * VW], BF16)
        kaug = [A([128, S_LEN], BF16) for _ in range(2)]
        qaug = [A([128, S_LEN], BF16) for _ in range(2)]
        kmT = A([64, 16], F32)
        kmTb = A([64, 16], BF16)
        pastmask = A([128, NT * 16], F32)
        pastind = A([128, NT * 16], F32)
        ownfut = A([128, NT * 16], F32)
        Gm = A([128, NT * 16], F32)
        m8 = A([128, NT, 8], F32)
        sel = A([128, NT * 16], F32)
        biasW = A([128, NT, 80], BF16)
        PTb = [A([128, 512], BF16) for _ in range(3)]
        rl = A([128, 4], F32)
        osb = A([128, 512], F32)
        for v4 in range(4):
            DMA("sync", vaug[:, v4 * 8:(v4 + 1) * 8, :], v_s[v4 * 1024:(v4 + 1) * 1024, :].rearrange("(t p) f -> p t f", p=128), [], ["vaug"], None)
        DMA("sync", pastmask[:], I["pastmask"], [], ["pastmask"], None)
        DMA("sync", pastind[:], I["pastind"], [], ["pastind"], None)
        DMA("sync", ownfut[:], I["ownfut"], [], ["ownfut"], None)
        for b_ in range(2):
            DMA("sync", kaug[b_][64:80, :], I["onehot"], [], ["kaug%d" % b_], None)
        E("gpsimd", "memset", [], ["biasW"], ap=biasW[:], constant=0.0)
        for kc in range(8):
            DMA("gpsimd", w_out[:, kc, :], I["w_out"][l][kc * 128:(kc + 1) * 128, :], [], ["w_out"], "wl%d" % (kc % 4))
        DMA("sync", G2[:], g_s[0], [], ["G2"], None)
        SB = PB[0:3]
        OB = PB[3:5]
        GP = PB[5]
        BT = PT[0]
        LA = 2

        def prologue_A(h):
            hb = h % 2
            kk = "kaug%d" % hb
            kq = "qaug%d" % hb
            DMA("sync", kaug[hb][0:64, :], kT_s[h], [], [kk], "kl%d" % hb)
            DMA("sync", qaug[hb][0:64, :], qT_s[h], [], [kq], "ql%d" % hb)
            E("vector", "tensor_reduce", [kk], ["kmT"], out=kmT[:], in_=kaug[hb][0:64, :].rearrange("p (n k) -> p n k", n=16), axis=AX.X, op=ALU.add)
            E("vector", "tensor_scalar", ["kmT"], ["kmTb"], out=kmTb[:], in0=kmT[:], scalar1=1.0 / 256, scalar2=None, op0=ALU.mult)
            for qt_ in range(NT):
                E("tensor", "matmul", [kq, "kmTb"], ["GP"], out=GP[:, qt_ * 16:(qt_ + 1) * 16], lhsT=qaug[hb][0:64, qt_ * 128:(qt_ + 1) * 128],
                  rhs=kmTb[:], start=True, stop=True)
            E("vector", "tensor_tensor", ["GP", "pastmask"], ["Gm"], out=Gm[:], in0=GP[:], in1=pastmask[:], op=ALU.add)
            for qt_ in range(NT):
                E("vector", "max", ["Gm"], ["m8"], out=m8[:, qt_, :], in_=Gm[:, qt_ * 16:(qt_ + 1) * 16])
            E("vector", "tensor_tensor", ["Gm", "m8"], ["sel"], out=sel[:].rearrange("p (q n) -> p q n", n=16),
              in0=Gm[:].rearrange("p (q n) -> p q n", n=16), in1=m8[:, :, 2:3].broadcast_to([128, NT, 16]), op=ALU.is_ge)
            E("vector", "tensor_scalar", ["sel"], ["sel"], out=sel[:], in0=sel[:], scalar1=-1.0, scalar2=BIG, op0=ALU.add, op1=ALU.mult)
            E("vector", "tensor_tensor", ["sel", "pastind"], ["sel"], out=sel[:], in0=sel[:], in1=pastind[:], op=ALU.mult)
            E("vector", "tensor_tensor", ["sel", "ownfut"], ["biasW"], out=biasW[:, :, 64:80], in0=sel[:].rearrange("p (q n) -> p q n", n=16),
              in1=ownfut[:].rearrange("p (q n) -> p q n", n=16), op=ALU.add)

        def prologue_B(h):
            hb = h % 2
            kq = "qaug%d" % hb
            for r4 in range(4):
                for q8 in range(8):
                    qt_ = r4 * 8 + q8
                    E("tensor", "transpose", ["biasW", "identb"], ["BT"], out=BT[0:80, q8 * 128:(q8 + 1) * 128], in_=biasW[:, qt_, :], identity=identb[:])
                E("vector", "tensor_copy", ["BT"], [kq], out=qaug[hb][64:80, r4 * 1024:(r4 + 1) * 1024], in_=BT[64:80, :])

        steps = [(h, g, kt) for h in range(NH) for g in range(8) for kt in range(4 * g + 4)]
        NPH = len(steps) // NH

        def emit_score(i):
            h, g, kt = steps[i]
            hb = h % 2
            kk = "kaug%d" % hb
            kq = "qaug%d" % hb
            sb_ = SB[i % 3]
            ks = "SB%d" % (i % 3)
            pt_ = PTb[i % 3]
            kpt = "PTb%d" % (i % 3)
            E("tensor", "matmul", [kk, kq], [ks], out=sb_[:], lhsT=kaug[hb][0:80, kt * 128:(kt + 1) * 128],
              rhs=qaug[hb][0:80, g * 512:(g + 1) * 512], start=True, stop=True)
            E("scalar", "activation", [ks], [kpt], out=pt_[:], in_=sb_[:], func=AF.Exp)
            r = kt - 4 * g
            if r >= 0:
                E("gpsimd", "tensor_tensor", [kpt, "tri"], [kpt], out=pt_[:, r * 128:(r + 1) * 128], in0=pt_[:, r * 128:(r + 1) * 128],
                  in1=tri[:], op=ALU.mult)

        def emit_pv(i):
            h, g, kt = steps[i]
            gg = h * 8 + g
            ob = OB[gg % 2]
            ko = "OB%d" % (gg % 2)
            pt_ = PTb[i % 3]
            kpt = "PTb%d" % (i % 3)
            for qi in range(4):
                if kt <= 4 * g + qi:
                    E("tensor", "matmul", [kpt, "vaug"], [ko], out=ob[:, qi * 128:qi * 128 + 65], lhsT=pt_[:, qi * 128:(qi + 1) * 128],
                      rhs=vaug[:, kt, h * VW:h * VW + 65], start=(kt == 0 and qi == 0), stop=(kt == 4 * g + qi))
            if kt == 4 * g + 3:
                E("vector", "tensor_copy", [ko], ["osb"], out=osb[:], in_=ob[:])
                obv = osb[:].rearrange("p (q c) -> p q c", q=4)
                E("vector", "reciprocal", ["osb"], ["rl"], out=rl[:].unsqueeze(2), in_=obv[:, :, 64:65])
                E("vector", "tensor_tensor", ["osb", "rl"], ["ao_all"], out=ao_all[:, 4 * g:4 * g + 4, h * 64:(h + 1) * 64], in0=obv[:, :, 0:64],
                  in1=rl[:].unsqueeze(2).broadcast_to([128, 4, 64]), op=ALU.mult)

        prologue_A(0)
        prologue_B(0)
        for i in range(len(steps) + LA):
            if i < len(steps):
                emit_score(i)
            j = i - LA
            if j >= 0:
                emit_pv(j)
                h = steps[j][0]
                pos = j - h * NPH
                if h + 1 < NH:
                    if pos == NPH // 2:
                        prologue_A(h + 1)
                    if pos == NPH - 16:
                        prologue_B(h + 1)
        if debug:
            for v4 in range(4):
                DMA("sync", dbg["ao"][v4 * 1024:(v4 + 1) * 1024, :].rearrange("(t p) f -> p t f", p=128), ao_all[:, v4 * 8:(v4 + 1) * 8, :], ["ao_all"], [], None)
        S.barrier()
        A.reset(m3_mark)
        xt = [A([128, D], F32) for _ in range(3)]
        junk = A([128, D], BF16)
        junk2 = A([128, 512], BF16)
        ssa = [A([128, 1], F32) for _ in range(2)]
        rstda = [A([128, 1], F32) for _ in range(2)]
        ss = [A([128, 1], F32) for _ in range(2)]
        rstd = [A([128, 1], F32) for _ in range(2)]
        an = [A([128, 512], BF16) for _ in range(2)]
        mTa = [A([128, 4, 128], BF16) for _ in range(2)]
        pmt = [A([128, 4, 128], BF16) for _ in range(3)]
        tt = [A([128, D], F32) for _ in range(2)]
        xo = [A([128, D], F32) for _ in range(2)]
        YY = [[PB[0], PB[1]], [PB[2], PB[3]]]
        TA = PT[1]

        def m3_L(t):
            DMA("sync", xt[t % 3][:], x_src[t * 128:(t + 1) * 128, :], [], ["xt%d" % (t % 3)], "xl%d" % (t % 3))
            DMA("sync", pmt[t % 3][:].rearrange("p g t -> p (g t)"), pm_s[t], [], ["pmt%d" % (t % 3)], "pml%d" % (t % 3))

        def m3_A(t):
            b2 = t % 2
            E("scalar", "activation", ["ao_all"], ["junk2", "ssa%d" % b2], out=junk2[:], in_=ao_all[:, t, :], func=AF.Square, accum_out=ssa[b2][:])
            rstd_ops(ssa[b2][:], rstda[b2][:], 1, ["ssa%d" % b2], ["rstda%d" % b2], 512)
            E("vector", "tensor_scalar", ["ao_all", "rstda%d" % b2], ["an%d" % b2], out=an[b2][:], in0=ao_all[:, t, :], scalar1=rstda[b2][:, 0:1],
              scalar2=None, op0=ALU.mult)
            for c4 in range(4):
                E("tensor", "transpose", ["an%d" % b2, "identb"], ["TA"], out=TA[:, c4 * 128:(c4 + 1) * 128], in_=an[b2][:, c4 * 128:(c4 + 1) * 128],
                  identity=identb[:])
            for c4 in range(4):
                E("scalar", "activation", ["TA", "aog"], ["mTa%d" % b2], out=mTa[b2][:, c4, :], in_=TA[:, c4 * 128:(c4 + 1) * 128], func=AF.Identity,
                  scale=aog_col[:, c4:c4 + 1])

        def m3_B(t):
            b2 = t % 2
            xs = xt[t % 3]
            kx = "xt%d" % (t % 3)
            Y = YY[b2]
            kY = ["PB%d" % (2 * b2), "PB%d" % (2 * b2 + 1)]
            for nb in range(2):
                for c8 in range(8):
                    lhs = mTa[b2][:, c8, :] if c8 < 4 else pmt[t % 3][:, c8 - 4, :]
                    E("tensor", "matmul", ["mTa%d" % b2, "pmt%d" % (t % 3), "w_out"], [kY[nb]], out=Y[nb][:], lhsT=lhs,
                      rhs=w_out[:, c8, nb * 512:(nb + 1) * 512], start=(c8 == 0), stop=(c8 == 7))
            post_norm_residual(E, rstd_ops, Y, kY, junk, "junk", ss[b2], "ss%d" % b2, rstd[b2], "rstd%d" % b2, G2, "G2",
                               tt[b2], "tt%d" % b2, xs, kx, xo[b2], "xo%d" % b2)
            DMA("sync", x_mid[t * 128:(t + 1) * 128, :], xo[b2][:], ["xo%d" % b2], [], "xst%d" % b2)

        m3_L(0)
        m3_L(1)
        m3_A(0)
        for t in range(NT):
            if t + 2 < NT:
                m3_L(t + 2)
            if t + 1 < NT:
                m3_A(t + 1)
            m3_B(t)
        if stop_after == "M3":
            break

        S.barrier()
        A.reset(persist_mark)
        w_up = A([128, 8, 2 * DFF], BF16)
        w_dn = A([128, NPAIR, D], BF16)
        G4 = A([128, D], F32)
        xt = [A([128, D], F32) for _ in range(2)]
        junk = A([128, D], BF16)
        ssa = [A([128, 1], F32) for _ in range(2)]
        rstda = [A([128, 1], F32) for _ in range(2)]
        ss = [A([128, 1], F32) for _ in range(2)]
        rstd = [A([128, 1], F32) for _ in range(2)]
        xn = [A([128, D], BF16) for _ in range(2)]
        hTg = A([128, 8, 512], BF16)
        mT = A([128, NPAIR, 512], BF16)
        acc = [[A([128, 512], F32) for _ in range(2)] for _ in range(2)]
        usb = [[A([128, 514], F32) for _ in range(2)] for _ in range(2)]
        hist = A([128, NFC, 2], F32)
        tt = [A([128, D], F32) for _ in range(2)]
        for kc in range(8):
            for hf in range(2):
                DMA("gpsimd", w_up[:, kc, hf * DFF:(hf + 1) * DFF], I["w_up"][l][kc * 128:(kc + 1) * 128, hf * DFF:(hf + 1) * DFF], [], ["w_up"], "wl%d" % ((2 * kc + hf) % 4))
        for c4 in range(0, NPAIR, 2):
            DMA("gpsimd", w_dn[:, c4:c4 + 2, :], I["w_down"][l][c4 * 128:(c4 + 2) * 128, :].rearrange("(c p) n -> p c n", p=128), [], ["w_dn"], "wl%d" % ((c4 // 2) % 4))
        DMA("sync", G4[:], g_s[1], [], ["G4"], None)
        E("gpsimd", "memset", [], ["hist"], ap=hist[:], constant=0.0)
        TF = PT[0]
        UB = [[PB[0], PB[1]], [PB[2], PB[3]]]
        Y = [PB[4], PB[5]]
        xcnt = [0]

        def xload(t):
            sl = xcnt[0] % 2
            xcnt[0] += 1
            DMA("sync", xt[sl][:], x_mid[t * 128:(t + 1) * 128, :], [], ["xt%d" % sl], "xl%d" % sl)
            return xt[sl], "xt%d" % sl

        def f_A(g, ti):
            t = g * 4 + ti
            b2 = t % 2
            xs, kx = xload(t)
            E("scalar", "activation", [kx], ["junk", "ssa%d" % b2], out=junk[:], in_=xs[:], func=AF.Square, accum_out=ssa[b2][:])
            rstd_ops(ssa[b2][:], rstda[b2][:], 1, ["ssa%d" % b2], ["rstda%d" % b2], D)
            E("vector", "tensor_scalar", [kx, "rstda%d" % b2], ["xn%d" % b2], out=xn[b2][:], in0=xs[:], scalar1=rstda[b2][:, 0:1],
              scalar2=None, op0=ALU.mult)
            for kc in range(8):
                E("tensor", "transpose", ["xn%d" % b2, "identb"], ["TF"], out=TF[:, kc * 128:(kc + 1) * 128],
                  in_=xn[b2][:, kc * 128:(kc + 1) * 128], identity=identb[:])
            for kc in range(8):
                E("scalar", "activation", ["TF", "cols2", "cols3"], ["hTg"], out=hTg[:, kc, ti * 128:(ti + 1) * 128],
                  in_=TF[:, kc * 128:(kc + 1) * 128], func=AF.Identity, scale=cols[:, 2, kc:kc + 1], bias=cols[:, 3, kc:kc + 1])

        pidx = 0
        for ti in range(4):
            f_A(0, ti)
        for g in range(8):
            for i in range(NPAIR):
                pb = pidx % 2
                pidx += 1
                halves = ((0, i), (1, NPAIR + i))
                for half, ch in halves:
                    U = UB[pb][half]
                    kU = "PB%d" % (2 * pb + half)
                    for kc in range(8):
                        E("tensor", "matmul", ["hTg", "w_up"], [kU], out=U[:], lhsT=w_up[:, kc, ch * 128:(ch + 1) * 128], rhs=hTg[:, kc, :],
                          start=(kc == 0), stop=(kc == 7))
                for half, ch in halves:
                    U = UB[pb][half]
                    kU = "PB%d" % (2 * pb + half)
                    ub = usb[pb][half]
                    ku = "usb%d%d" % (pb, half)
                    E("gpsimd", "tensor_copy", ["hist%d" % ch], [ku + "h"], out=ub[:, 0:2], in_=hist[:, ch, :])
                    E("scalar", "activation", [kU], [ku], out=ub[:, 2:514], in_=U[:], func=AF.Copy)
                    if g < 7:
                        E("gpsimd", "tensor_copy", [ku], ["hist%d" % ch], out=hist[:, ch, :], in_=ub[:, 512:514])
                for half, ch in halves:
                    ub = usb[pb][half]
                    ku = "usb%d%d" % (pb, half)
                    ac = acc[pb][half]
                    ka = "acc%d%d" % (pb, half)
                    E("vector", "tensor_scalar", [ku, "cv2", "cv3"], [ka], out=ac[:], in0=ub[:, 2:514], scalar1=cv_col[:, 2, ch:ch + 1],
                      scalar2=cv_col[:, 3, ch:ch + 1], op0=ALU.mult, op1=ALU.add)
                for half, ch in halves:
                    ub = usb[pb][half]
                    ku = "usb%d%d" % (pb, half)
                    ac = acc[pb][half]
                    ka = "acc%d%d" % (pb, half)
                    E("vector", "scalar_tensor_tensor", [ku, ku + "h", ka, "cv1"], [ka], out=ac[:], in0=ub[:, 1:513], scalar=cv_col[:, 1, ch:ch + 1],
                      in1=ac[:], op0=ALU.mult, op1=ALU.add)
                for half, ch in halves:
                    ub = usb[pb][half]
                    ku = "usb%d%d" % (pb, half)
                    ac = acc[pb][half]
                    ka = "acc%d%d" % (pb, half)
                    E("vector", "scalar_tensor_tensor", [ku, ku + "h", ka, "cv0"], [ka], out=ac[:], in0=ub[:, 0:512], scalar=cv_col[:, 0, ch:ch + 1],
                      in1=ac[:], op0=ALU.mult, op1=ALU.add)
                ka0 = "acc%d0" % pb
                ka1 = "acc%d1" % pb
                E("scalar", "activation", [ka0], [ka0], out=acc[pb][0][:], in_=acc[pb][0][:], func=AF.Silu)
                E("gpsimd", "tensor_tensor", [ka0, ka1], ["mT"], out=mT[:, i, :], in0=acc[pb][0][:], in1=acc[pb][1][:], op=ALU.mult)
            for ti in range(4):
                t = g * 4 + ti
                b2 = t % 2
                for nb in range(2):
                    for i in range(NPAIR):
                        E("tensor", "matmul", ["mT", "w_dn"], ["PB%d" % (4 + nb)], out=Y[nb][:], lhsT=mT[:, i, ti * 128:(ti + 1) * 128],
                          rhs=w_dn[:, i, nb * 512:(nb + 1) * 512], start=(i == 0), stop=(i == NPAIR - 1))
                if g + 1 < 8:
                    f_A(g + 1, ti)
                xs, kx = xload(t)
                post_norm_residual(E, rstd_ops, Y, ["PB4", "PB5"], junk, "junk", ss[b2], "ss%d" % b2, rstd[b2], "rstd%d" % b2, G4, "G4",
                                   tt[b2], "tt%d" % b2, xs, kx, tt[b2], "tt%d" % b2)
                DMA("sync", x_dst[t * 128:(t + 1) * 128, :], tt[b2][:], ["tt%d" % b2], [], "xst%d" % b2)
        x_src = x_dst

    final = [k for k in S.dma_counts.keys()]
    nsem = S.emit(final_wait_semkeys=final)
    return nc


def post_norm_residual(E, rstd_ops, Y, kY, junk, kjunk, ss, kss, rstd, krstd, G, kG, tt, ktt, xs, kx, xo, kxo):
    ssb = ss
    E("scalar", "activation", [kY[0]], [kjunk, kss, "lk" + kY[0]], out=junk[:, 0:512], in_=Y[0][:], func=AF.Square, accum_out=ssb[:])
    E("vector", "tensor_copy", [kY[0], "lk" + kY[0]], [ktt], out=tt[:, 0:512], in_=Y[0][:])
    E("scalar", "activation", [kY[1]], [kjunk, krstd, "lk" + kY[1]], out=junk[:, 512:1024], in_=Y[1][:], func=AF.Square, accum_out=rstd[:])
    E("vector", "tensor_copy", [kY[1], "lk" + kY[1]], [ktt], out=tt[:, 512:1024], in_=Y[1][:])
    E("vector", "tensor_tensor", [kss, krstd], [kss], out=ssb[:], in0=ssb[:], in1=rstd[:], op=ALU.add)
    rstd_ops(ssb[:], rstd[:], 1, [kss], [krstd], D)
    E("vector", "scalar_tensor_tensor", [ktt, krstd, kG], [ktt], out=tt[:], in0=tt[:], scalar=rstd[:, 0:1],
      in1=G[:], op0=ALU.mult, op1=ALU.mult)
    E("gpsimd", "tensor_tensor", [ktt, kx], [kxo], out=xo[:], in0=tt[:], in1=xs[:], op=ALU.add)


_CONSTS = None


def make_in_maps(inputs):
    global _CONSTS
    if _CONSTS is None:
        _CONSTS = _const_tables()
    x = np.asarray(inputs["x"], dtype=np.float32)
    c = np.asarray(inputs["c"], dtype=np.float32)
    shared = {k: np.ascontiguousarray(np.asarray(inputs[k], dtype=np.float32)) for k in WEIGHT_SPECS if k != "c"}
    maps = []
    for b in range(8):
        m = {"x": np.ascontiguousarray(x[b]), "c": np.ascontiguousarray(c[b:b + 1])}
        m.update(shared)
        m.update(_CONSTS)
        maps.append(m)
    return maps


def kernel(**inputs):
    nc = build()
    maps = make_in_maps(inputs)
    res = run_bass_kernel_spmd(nc, maps, core_ids=list(range(8)))
    return np.stack([np.asarray(r["out"]) for r in res.results], axis=0).astype(np.float32)
```

```python
import contextlib
import os
import numpy as np
import ml_dtypes
import concourse.bass as bass
import concourse.mybir as mybir
from concourse.bass_utils import run_bass_kernel_spmd

F32 = mybir.dt.float32
BF16 = mybir.dt.bfloat16
AF = mybir.ActivationFunctionType
ALU = mybir.AluOpType
AX = mybir.AxisListType

S_LEN = 4096
D = 1024
NT = S_LEN // 128
NH = 8
HD = 64
DFF = 2816
NFC = 2 * DFF // 128
NPAIR = DFF // 128
EPS = 1e-6
BIG = 30000.0
POOL_W = (2, 4, 8, 16)
VW = 68

ENGINES = ("tensor", "vector", "scalar", "gpsimd", "sync")
SEM_LIMIT = 30000


class Op:
    __slots__ = ("eng", "fn", "deps", "is_dma", "ticket", "signal", "idx")

    def __init__(self, eng, fn, is_dma):
        self.idx = 0
        self.eng = eng
        self.fn = fn
        self.is_dma = is_dma
        self.deps = []
        self.ticket = None
        self.signal = False


class Sched:
    def __init__(self, nc):
        self.nc = nc
        self.q = {e: [] for e in ENGINES}
        self.last_w = {}
        self.readers = {}
        self.dma_counts = {}
        self.dma_last = {}
        self.bar = {e: [] for e in ENGINES}

    def barrier(self):
        deps = []
        for e in ENGINES:
            for o in reversed(self.q[e]):
                if not o.is_dma:
                    deps.append(o)
                    break
        deps.extend(self.dma_last.values())
        for e in ENGINES:
            self.bar[e] = list(deps)
        self.last_w = {}
        self.readers = {}

    def op(self, eng, fn, reads=(), writes=(), dma=False, semkey=None):
        o = Op(eng, fn, dma)
        deps = set()
        for k in reads:
            w = self.last_w.get(k)
            if w is not None:
                deps.add(w)
        for k in writes:
            w = self.last_w.get(k)
            if w is not None:
                deps.add(w)
            for r in self.readers.get(k, ()):
                deps.add(r)
        for d in deps:
            if d is o:
                continue
            if (not d.is_dma) and (not dma) and d.eng == eng:
                if eng == "tensor":
                    continue
                raw = any(self.last_w.get(k) is d for k in reads)
                if not raw:
                    continue
            o.deps.append(d)
        if dma and semkey in self.dma_last:
            o.deps.append(self.dma_last[semkey])
        if self.bar[eng]:
            for d in self.bar[eng]:
                if d.is_dma or d.eng != eng:
                    o.deps.append(d)
            self.bar[eng] = []
        best = {}
        pruned = []
        for d in o.deps:
            if d.is_dma:
                pruned.append(d)
            else:
                b = best.get(d.eng)
                if b is None or d.idx > b.idx:
                    best[d.eng] = d
        o.deps = pruned + list(best.values())
        o.idx = len(self.q[eng])
        for k in writes:
            self.last_w[k] = o
            self.readers[k] = []
        for k in reads:
            self.readers.setdefault(k, []).append(o)
        if dma:
            c = self.dma_counts.get(semkey, 0) + 16
            self.dma_counts[semkey] = c
            o.ticket = (("dma", semkey), c)
            self.dma_last[semkey] = o
        self.q[eng].append(o)
        return o

    def emit(self, final_wait_semkeys=()):
        nc = self.nc
        for e in ENGINES:
            for o in self.q[e]:
                for d in o.deps:
                    if not d.is_dma:
                        d.signal = True
        semnames = set()
        for e in ENGINES:
            cnt = 0
            seg = 0
            for o in self.q[e]:
                if o.is_dma:
                    semnames.add(o.ticket[0])
                    continue
                if o.signal:
                    cnt += 1
                    if cnt > SEM_LIMIT:
                        seg += 1
                        cnt = 1
                    o.ticket = (("eng", e, seg), cnt)
                    semnames.add(o.ticket[0])
        semnames = sorted(semnames, key=str)
        with contextlib.ExitStack() as st:
            sems = {}
            for i, n in enumerate(semnames):
                sems[n] = st.enter_context(nc.semaphore("s%d" % i))
            block = st.enter_context(nc.Block())

            def run(engname):
                def body(eng):
                    waited = {}
                    for o in self.q[engname]:
                        need = {}
                        for d in o.deps:
                            s, v = d.ticket
                            if need.get(s, 0) < v:
                                need[s] = v
                        for s, v in need.items():
                            if waited.get(s, 0) < v:
                                eng.wait_ge(sems[s], v)
                                waited[s] = v
                        ins = o.fn(eng)
                        if o.is_dma:
                            ins.then_inc(sems[o.ticket[0]], 16)
                        elif o.signal:
                            ins.then_inc(sems[o.ticket[0]], 1)
                    if engname == "sync":
                        for k in final_wait_semkeys:
                            s = ("dma", k)
                            v = self.dma_counts[k]
                            if waited.get(s, 0) < v:
                                eng.wait_ge(sems[s], v)
                                waited[s] = v
                return body

            block.tensor(run("tensor"))
            block.vector(run("vector"))
            block.scalar(run("scalar"))
            block.gpsimd(run("gpsimd"))
            block.sync(run("sync"))
        return len(semnames)


def _const_tables():
    bf = ml_dtypes.bfloat16
    pos = np.arange(S_LEN, dtype=np.float32)
    inv_freq = np.power(np.float32(500000.0), -np.arange(0, 16, 2, dtype=np.float32) / np.float32(16))
    ang = (pos[:, None] * inv_freq[None, :]).astype(np.float32)
    cos = np.cos(ang).astype(np.float32)
    sin = np.sin(ang).astype(np.float32)
    def tm(a):
        return np.ascontiguousarray(a.reshape(NT, 128, -1).transpose(1, 0, 2))
    ropek = tm(np.concatenate([cos, sin], axis=1)).astype(np.float32)
    ropeq = (ropek * np.float32(0.125)).astype(np.float32)
    qt = np.arange(NT)
    j = qt // 2
    n = np.arange(16)
    past = (n[None, :] < j[:, None])
    pastmask = np.where(past, 0.0, -1e30).astype(np.float32).reshape(1, NT * 16)
    pastind = past.astype(np.float32).reshape(1, NT * 16)
    ownfut = np.where(n[None, :] > j[:, None], -BIG, 0.0).astype(np.float32).reshape(1, NT * 16)
    onehot = (np.arange(S_LEN)[None, :] // 256 == n[:, None]).astype(np.float32).astype(bf)
    k = np.arange(128)
    tri = (k[None, :] >= k[:, None]).astype(np.float32).astype(bf)
    identb = np.eye(128, dtype=np.float32).astype(bf)
    identf = np.eye(128, dtype=np.float32)
    onesb = np.ones((128, 128), dtype=np.float32).astype(bf)
    rc = np.zeros((1, 4, 16), dtype=np.float32)
    for g, w in enumerate(POOL_W):
        rc[0, g, :] = 1.0 / np.minimum(np.arange(16) + 1, w)
    return {
        "ropeq": ropeq, "ropek": ropek,
        "pastmask": np.ascontiguousarray(np.broadcast_to(pastmask, (128, NT * 16))),
        "pastind": np.ascontiguousarray(np.broadcast_to(pastind, (128, NT * 16))),
        "ownfut": np.ascontiguousarray(np.broadcast_to(ownfut, (128, NT * 16))),
        "onehot": onehot, "tri": tri, "identb": identb, "identf": identf, "onesb": onesb,
        "rc": np.ascontiguousarray(np.broadcast_to(rc, (128, 4, 16))),
    }


CONST_SPECS = {
    "ropeq": ([128, NT, 16], F32), "ropek": ([128, NT, 16], F32),
    "pastmask": ([128, NT * 16], F32), "pastind": ([128, NT * 16], F32), "ownfut": ([128, NT * 16], F32),
    "onehot": ([16, S_LEN], BF16), "tri": ([128, 128], BF16), "identb": ([128, 128], BF16),
    "identf": ([128, 128], F32), "onesb": ([128, 128], BF16), "rc": ([128, 4, 16], F32),
}

WEIGHT_SPECS = {
    "c": [1, D], "w_ada": [2, D, 6 * D], "b_ada": [2, 6 * D], "g_pre_mix": [2, D], "w_in": [2, D, 2048],
    "w_pool": [2, 4, 128, 128], "pool_scale": [2, 512], "attn_out_gain": [2, 512], "pool_out_gain": [2, 512],
    "w_out": [2, D, D], "g_post_mix": [2, D], "g_pre_ffn": [2, D], "w_up": [2, D, 2 * DFF],
    "conv_w": [2, 3, 2 * DFF], "conv_b": [2, 2 * DFF], "w_down": [2, DFF, D], "g_post_ffn": [2, D],
}


class Alloc:
    def __init__(self, nc):
        self.nc = nc
        self.base = (nc.sbuf_base + 63) // 64 * 64
        self.top = nc.sbuf_top
        self.cur = self.base
        self.n = 0

    def mark(self):
        return self.cur

    def reset(self, m):
        self.cur = m

    def __call__(self, shape, dt):
        sz = 1
        for s in shape[1:]:
            sz *= s
        nbytes = sz * (4 if dt == F32 else 2)
        nbytes = (nbytes + 63) // 64 * 64
        off = self.cur
        assert off + nbytes <= self.top, ("SBUF overflow", off, nbytes, self.top)
        self.cur += nbytes
        self.n += 1
        return self.nc.alloc_sbuf_tensor_at("sb%d" % self.n, list(shape), dt, offset=off)


def build(n_layers=2, stop_after=None, debug=False):
    nc = bass.Bass("TRN2", target_bir_lowering=False)
    I = {}
    I["x"] = nc.dram_tensor("x", [S_LEN, D], F32, kind="ExternalInput").ap()
    for k, shp in WEIGHT_SPECS.items():
        I[k] = nc.dram_tensor(k, shp, F32, kind="ExternalInput").ap()
    for k, (shp, dt) in CONST_SPECS.items():
        I[k] = nc.dram_tensor(k, shp, dt, kind="ExternalInput").ap()
    out = nc.dram_tensor("out", [S_LEN, D], F32, kind="ExternalOutput").ap()
    sk = "ExternalOutput" if debug else "Internal"
    qT_s = nc.dram_tensor("qT_s", [NH, HD, S_LEN], BF16, kind=sk).ap()
    kT_s = nc.dram_tensor("kT_s", [NH, HD, S_LEN], BF16, kind=sk).ap()
    v_s = nc.dram_tensor("v_s", [S_LEN, NH * VW], BF16, kind=sk).ap()
    pm_s = nc.dram_tensor("pm_s", [NT, 128, 512], BF16, kind=sk).ap()
    g_s = nc.dram_tensor("g_s", [2, 128, D], F32, kind=sk).ap()
    xa = nc.dram_tensor("xa", [S_LEN, D], F32, kind=sk).ap()
    xb = nc.dram_tensor("xb", [S_LEN, D], F32, kind=sk).ap()
    dbg = {}
    if debug:
        dbg["ao"] = nc.dram_tensor("dbg_ao", [S_LEN, 512], F32, kind="ExternalOutput").ap()

    S = Sched(nc)
    A = Alloc(nc)
    PB = [nc.alloc_psum_tensor("pb%d" % i, [128, 512], F32) for i in range(6)]
    PT = [nc.alloc_psum_tensor("pt%d" % i, [128, 1024], BF16) for i in range(2)]

    sec = [None]
    enabled = os.environ.get("KSEC")
    nt_run = int(os.environ.get("KNT", NT))

    def skip():
        return enabled is not None and sec[0] is not None and sec[0] not in enabled

    def E(eng, meth, reads, writes, **kw):
        if skip():
            return None
        return S.op(eng, lambda e: getattr(e, meth)(**kw), reads, writes)

    otc = [0]

    def OT():
        otc[0] += 1
        return "ot%d" % (otc[0] % 6)

    def DMA(eng, out_, in_, reads, writes, semkey, slow=False):
        if skip():
            return None
        if semkey is None:
            semkey = OT()
        if slow:
            return S.op(eng, lambda e: e.dma_start(out=out_, in_=in_, allow_slow_non_contiguous=True),
                        reads, writes, dma=True, semkey=semkey)
        return S.op(eng, lambda e: e.dma_start(out=out_, in_=in_), reads, writes, dma=True, semkey=semkey)

    identb = A([128, 128], BF16)
    identf = A([128, 128], F32)
    onesb = A([128, 128], BF16)
    tri = A([128, 128], BF16)
    for nm, t in (("identb", identb), ("identf", identf), ("onesb", onesb), ("tri", tri)):
        DMA("sync", t[:], I[nm], [], [nm], None)
    cols = A([128, 4, 8], F32)
    aog_col = A([128, 4], F32)
    ps_col = A([128, 4], F32)
    pg_col = A([128, 4], F32)
    cv_col = A([128, 4, NFC], F32)
    rstd_eps = EPS
    persist_mark = A.mark()

    def rstd_ops(ss, rstd, n, keys_r, keys_w, dim):
        E("scalar", "activation", keys_r, keys_w, out=rstd, in_=ss, func=AF.Ln, scale=1.0 / dim, bias=rstd_eps)
        E("scalar", "activation", keys_w, keys_w, out=rstd, in_=rstd, func=AF.Exp, scale=-0.5)

    x_src = I["x"]
    for l in range(n_layers):
        last_layer = (l == n_layers - 1)
        x_mid = xa
        x_dst = out if last_layer else xb
        S.barrier()
        A.reset(persist_mark)
        c_col = A([128, 8], F32)
        cact = A([128, 8], F32)
        cbc = A([128, 8, 128], F32)
        bada = A([128, 6 * D], F32)
        modbc = A([128, 6 * D], F32)
        wblk = [A([128, 8, 512], F32) for _ in range(2)]
        gbc = A([128, D], F32)
        gtmp = A([128, D], F32)
        dtmp = A([128, 8, 128], F32)
        stg = [A([64, 128], F32) for _ in range(2)]
        stc = [0]

        def load_col(vec1d, n, dst, wkey):
            i_ = stc[0] % 2
            stc[0] += 1
            sg = stg[i_]
            DMA("sync", sg[0:n, :], vec1d.rearrange("(c p) -> c p", p=128), [], ["stg%d" % i_], None)
            E("tensor", "matmul", ["stg%d" % i_, "identf"], ["PB2"], out=PB[2][:, 0:n], lhsT=sg[0:n, :], rhs=identf[0:n, 0:n],
              start=True, stop=True)
            E("vector", "tensor_copy", ["PB2"], [wkey], out=dst, in_=PB[2][:, 0:n])

        load_col(I["c"][0], 8, c_col[:], "c_col")
        DMA("sync", bada[:], I["b_ada"][l].partition_broadcast(128), [], ["bada"], None)
        load_col(I["attn_out_gain"][l], 4, aog_col[:], "aog")
        load_col(I["pool_scale"][l], 4, ps_col[:], "psc")
        load_col(I["pool_out_gain"][l], 4, pg_col[:], "pgc")
        for j3 in range(3):
            load_col(I["conv_w"][l, j3], NFC, cv_col[:, j3, :], "cv%d" % j3)
        load_col(I["conv_b"][l], NFC, cv_col[:, 3, :], "cv3")
        E("scalar", "activation", ["c_col"], ["cact"], out=cact[:], in_=c_col[:], func=AF.Silu)
        E("vector", "tensor_copy", ["cact"], ["cbc"], out=cbc[:], in_=cact[:].unsqueeze(2).broadcast_to([128, 8, 128]))
        for blk in range(12):
            wb = wblk[blk % 2]
            kb = "wblk%d" % (blk % 2)
            pm_ = PB[blk % 2]
            kp = "PB%d" % (blk % 2)
            DMA("sync", wb[:], I["w_ada"][l][:, blk * 512:(blk + 1) * 512].rearrange("(kc p) n -> p kc n", p=128),
                [], [kb], "wada%d" % (blk % 2))
            for kc in range(8):
                E("tensor", "matmul", [kb, "cbc"], [kp], out=pm_[:], lhsT=cbc[:, kc, :], rhs=wb[:, kc, :],
                  start=(kc == 0), stop=(kc == 7))
            E("vector", "tensor_tensor", [kp, "bada"], ["modbc"], out=modbc[:, blk * 512:(blk + 1) * 512], in0=pm_[:],
              in1=bada[:, blk * 512:(blk + 1) * 512], op=ALU.add)

        def diag_extract(src_ap, dst_ap, rkeys, wkey):
            E("vector", "tensor_tensor", rkeys + ["identf"], ["dtmp"], out=dtmp[:],
              in0=src_ap.rearrange("p (k q) -> p k q", k=8),
              in1=identf[:].unsqueeze(1).broadcast_to([128, 8, 128]), op=ALU.mult)
            E("vector", "tensor_reduce", ["dtmp"], [wkey], out=dst_ap, in_=dtmp[:], axis=AX.X, op=ALU.add)

        for gi, (gname, moff) in enumerate((("g_post_mix", 2 * D), ("g_post_ffn", 5 * D))):
            DMA("sync", gbc[:], I[gname][l].partition_broadcast(128), [], ["gbc"], None)
            E("vector", "tensor_tensor", ["gbc", "modbc"], ["gtmp"], out=gtmp[:], in0=modbc[:, moff:moff + D], in1=gbc[:], op=ALU.mult)
            DMA("sync", g_s[gi], gtmp[:], ["gtmp"], [], None)
        for ci, (gname, scoff, shoff) in enumerate((("g_pre_mix", 1 * D, 0), ("g_pre_ffn", 4 * D, 3 * D))):
            DMA("sync", gbc[:], I[gname][l].partition_broadcast(128), [], ["gbc"], None)
            E("vector", "scalar_tensor_tensor", ["gbc", "modbc"], ["gtmp"], out=gtmp[:], in0=modbc[:, scoff:scoff + D],
              scalar=1.0, in1=gbc[:], op0=ALU.add, op1=ALU.mult)
            diag_extract(gtmp[:], cols[:, 2 * ci, :], ["gtmp"], "cols%d" % (2 * ci))
            diag_extract(modbc[:, shoff:shoff + D], cols[:, 2 * ci + 1, :], ["modbc"], "cols%d" % (2 * ci + 1))
        if stop_after == "P":
            break

        S.barrier()
        A.reset(persist_mark)
        w_in = A([128, 8, 2048], BF16)
        w_pool = A([128, 4, 128], BF16)
        ropeq = A([128, NT, 16], F32)
        ropek = A([128, NT, 16], F32)
        rc = A([128, 4, 16], F32)
        xt = [A([128, D], F32) for _ in range(3)]
        junk = A([128, D], BF16)
        ss = [A([128, 1], F32) for _ in range(3)]
        rstd = [A([128, 1], F32) for _ in range(3)]
        xn = [A([128, D], BF16) for _ in range(3)]
        hT = [A([128, 8, 128], BF16) for _ in range(2)]
        q_tm = [A([128, 512], BF16) for _ in range(2)]
        k_tm = [A([128, 512], BF16) for _ in range(2)]
        rt = [A([128, 8, 8], F32) for _ in range(4)]
        zq = A([128, 8, 16], F32)
        qst = [A([64, 2, 4, 512], BF16) for _ in range(2)]
        kst = [A([64, 2, 4, 512], BF16) for _ in range(2)]
        vst = [A([128, 8, VW], BF16) for _ in range(2)]
        pmst = [A([128, 4, 128], BF16) for _ in range(2)]
        pz = A([128, 4, 144], F32)
        s2 = A([128, 4, 144], F32)
        s4 = A([128, 4, 144], F32)
        s8 = A([128, 4, 144], F32)
        s16 = A([128, 4, 144], F32)
        pooledT = [A([128, 4, 128], BF16) for _ in range(2)]
        po_sb = A([128, 4, 128], F32)
        sq = A([128, 4, 128], BF16)
        prs = A([128, 128], F32)
        ptmp = A([128, 4, 128], F32)
        etmp = A([128, 4, 16], F32)

        sec[0] = "L"
        for kc in range(8):
            DMA("gpsimd", w_in[:, kc, :], I["w_in"][l][kc * 128:(kc + 1) * 128, :], [], ["w_in"], "wl%d" % (kc % 4))
        DMA("gpsimd", w_pool[:], I["w_pool"][l].rearrange("g c e -> c g e"), [], ["w_pool"], "wl0")
        DMA("sync", ropeq[:], I["ropeq"], [], ["ropeq"], None)
        DMA("sync", ropek[:], I["ropek"], [], ["ropek"], None)
        DMA("sync", rc[:], I["rc"], [], ["rc"], None)
        for b_ in range(2):
            E("gpsimd", "memset", [], ["vst%d" % b_], ap=vst[b_][:], constant=1.0)
        E("gpsimd", "memset", [], ["pz"], ap=pz[:], constant=0.0)
        Pq, Pk, Pv, Pp, Py, Ps = PB
        T0, T1 = PT[0], PT[1]
        def m1_L(t):
            DMA("sync", xt[t % 3][:], x_src[t * 128:(t + 1) * 128, :], [], ["xt%d" % (t % 3)], "xl%d" % (t % 3))

        def m1_N(t):
            b3 = t % 3
            xs = xt[b3]
            kx = "xt%d" % b3
            E("scalar", "activation", [kx], ["junk", "ss%d" % b3], out=junk[:], in_=xs[:], func=AF.Square, accum_out=ss[b3][:])
            rstd_ops(ss[b3][:], rstd[b3][:], 1, ["ss%d" % b3], ["rstd%d" % b3], D)
            E("vector", "tensor_scalar", [kx, "rstd%d" % b3], ["xn%d" % b3], out=xn[b3][:], in0=xs[:], scalar1=rstd[b3][:, 0:1],
              scalar2=None, op0=ALU.mult)

        def m1_T(t):
            b3 = t % 3
            b2 = t % 2
            for kc in range(8):
                E("tensor", "transpose", ["xn%d" % b3, "identb"], ["T0"], out=T0[:, kc * 128:(kc + 1) * 128],
                  in_=xn[b3][:, kc * 128:(kc + 1) * 128], identity=identb[:])
            for kc in range(8):
                E("scalar", "activation", ["T0", "cols0", "cols1"], ["hT%d" % b2], out=hT[b2][:, kc, :], in_=T0[:, kc * 128:(kc + 1) * 128],
                  func=AF.Identity, scale=cols[:, 0, kc:kc + 1], bias=cols[:, 1, kc:kc + 1])

        def m1_B1(t):
            b2 = t % 2
            for P_, kP, c0 in ((Pq, "Pq", 0), (Pk, "Pk", 512), (Pv, "Pv", 1024)):
                for kc in range(8):
                    E("tensor", "matmul", ["hT%d" % b2, "w_in"], [kP], out=P_[:], lhsT=hT[b2][:, kc, :], rhs=w_in[:, kc, c0:c0 + 512],
                      start=(kc == 0), stop=(kc == 7))
            for g in range(4):
                for kc in range(8):
                    E("tensor", "matmul", ["hT%d" % b2, "w_in"], ["Pp"], out=Pp[:, g * 128:(g + 1) * 128],
                      lhsT=w_in[:, kc, 1536 + g * 128:1536 + (g + 1) * 128], rhs=hT[b2][:, kc, :], start=(kc == 0), stop=(kc == 7))
            for P_, kP, tm_, ktm, rope, krope, sc_ in ((Pq, "Pq", q_tm[b2], "q_tm%d" % b2, ropeq, "ropeq", 0.125),
                                                       (Pk, "Pk", k_tm[b2], "k_tm%d" % b2, ropek, "ropek", 1.0)):
                E("scalar", "activation", [kP], [ktm], out=tm_[:], in_=P_[:], func=AF.Identity, scale=sc_)
                pv = P_[:].rearrange("p (h d) -> p h d", h=8)
                ov = tm_[:].rearrange("p (h d) -> p h d", h=8)
                E("scalar", "activation", [kP], ["zq"], out=zq[:], in_=pv[:, :, 0:16], func=AF.Copy)
                cosb = rope[:, t, 0:8].unsqueeze(1).broadcast_to([128, 8, 8])
                sinb = rope[:, t, 8:16].unsqueeze(1).broadcast_to([128, 8, 8])
                x1 = zq[:, :, 0:8]
                x2 = zq[:, :, 8:16]
                E("vector", "tensor_tensor", ["zq", krope], ["rt0"], out=rt[0][:], in0=x1, in1=cosb, op=ALU.mult)
                E("vector", "tensor_tensor", ["zq", krope], ["rt1"], out=rt[1][:], in0=x2, in1=sinb, op=ALU.mult)
                E("vector", "tensor_tensor", ["zq", krope], ["rt2"], out=rt[2][:], in0=x2, in1=cosb, op=ALU.mult)
                E("vector", "tensor_tensor", ["zq", krope], ["rt3"], out=rt[3][:], in0=x1, in1=sinb, op=ALU.mult)
                E("vector", "tensor_tensor", ["rt0", "rt1"], [ktm], out=ov[:, :, 0:8], in0=rt[0][:], in1=rt[1][:], op=ALU.subtract)
                E("vector", "tensor_tensor", ["rt2", "rt3"], [ktm], out=ov[:, :, 8:16], in0=rt[2][:], in1=rt[3][:], op=ALU.add)
            vb = t % 2
            E("scalar", "activation", ["Pv"], ["vst%d" % vb], out=vst[vb][:, :, 0:64], in_=Pv[:].rearrange("p (h d) -> p h d", h=8),
              func=AF.Copy)
            DMA("sync", v_s[t * 128:(t + 1) * 128, :], vst[vb][:].rearrange("p h d -> p (h d)"), ["vst%d" % vb], [], "vs%d" % vb)
            pT = pooledT[b2]
            kpT = "pooledT%d" % b2
            E("scalar", "activation", ["Pp"], ["pz"], out=pz[:, :, 16:144], in_=Pp[:].rearrange("p (g t) -> p g t", g=4), func=AF.Copy)
            E("gpsimd", "tensor_tensor", ["pz"], ["s2"], out=s2[:, :, 1:144], in0=pz[:, :, 1:144], in1=pz[:, :, 0:143], op=ALU.add)
            E("gpsimd", "tensor_tensor", ["s2"], ["s4"], out=s4[:, 1:4, 3:144], in0=s2[:, 1:4, 3:144], in1=s2[:, 1:4, 1:142], op=ALU.add)
            E("gpsimd", "tensor_tensor", ["s4"], ["s8"], out=s8[:, 2:4, 7:144], in0=s4[:, 2:4, 7:144], in1=s4[:, 2:4, 3:140], op=ALU.add)
            E("gpsimd", "tensor_tensor", ["s8"], ["s16"], out=s16[:, 3:4, 15:144], in0=s8[:, 3:4, 15:144], in1=s8[:, 3:4, 7:136], op=ALU.add)
            sums = (s2, s4, s8, s16)
            for g in range(4):
                E("vector", "scalar_tensor_tensor", ["s%d" % (2 << g), "pz"], [kpT], out=pT[:, g, :], in0=sums[g][:, g, 16:144],
                  scalar=1.0 / POOL_W[g], in1=pz[:, g, 16:144], op0=ALU.mult, op1=ALU.subtract)
            if t == 0:
                for g in range(4):
                    E("gpsimd", "tensor_tensor", ["s%d" % (2 << g), "rc"], ["etmp"], out=etmp[:, g, :], in0=sums[g][:, g, 16:32], in1=rc[:, g, :], op=ALU.mult)
                    E("gpsimd", "tensor_tensor", ["etmp", "pz"], [kpT], out=pT[:, g, 0:16], in0=etmp[:, g, :], in1=pz[:, g, 16:32], op=ALU.subtract)
            E("gpsimd", "tensor_copy", ["pz", "s2", "s4", "s8", "s16", kpT], ["pz"], out=pz[:, :, 0:16], in_=pz[:, :, 128:144])

        def m1_B2(t):
            b2 = t % 2
            g4 = t // 4
            ti = t % 4
            sb_ = g4 % 2
            pT = pooledT[b2]
            kpT = "pooledT%d" % b2
            for g in range(4):
                E("tensor", "matmul", [kpT, "w_pool"], ["Py"], out=Py[:, g * 128:(g + 1) * 128], lhsT=w_pool[:, g, :], rhs=pT[:, g, :],
                  start=True, stop=True)
            for hi, (tm_, ktm, st_, kst_) in enumerate(((q_tm[b2], "q_tm%d" % b2, qst[sb_], "qst%d" % sb_), (k_tm[b2], "k_tm%d" % b2, kst[sb_], "kst%d" % sb_))):
                kT1 = "T1"
                for pr in range(4):
                    E("tensor", "transpose", [ktm, "identb"], [kT1], out=T1[:, hi * 512 + pr * 128:hi * 512 + (pr + 1) * 128],
                      in_=tm_[:, pr * 128:(pr + 1) * 128], identity=identb[:])
                for pa in range(2):
                    E("vector", "tensor_copy", [kT1], [kst_], out=st_[:, pa, :, ti * 128:(ti + 1) * 128],
                      in_=T1[pa * 64:(pa + 1) * 64, hi * 512:(hi + 1) * 512].rearrange("p (r t) -> p r t", r=4))
            if ti == 3:
                for pa in range(2):
                    DMA("sync", qT_s.rearrange("(pr pa) d t -> pa d pr t", pa=2)[pa][:, :, g4 * 512:(g4 + 1) * 512], qst[sb_][:, pa, :, :],
                        ["qst%d" % sb_], [], "qs%d%d" % (sb_, pa))
                    DMA("sync", kT_s.rearrange("(pr pa) d t -> pa d pr t", pa=2)[pa][:, :, g4 * 512:(g4 + 1) * 512], kst[sb_][:, pa, :, :],
                        ["kst%d" % sb_], [], "ks%d%d" % (sb_, pa))
            E("vector", "tensor_tensor", ["Py", "psc"], ["po_sb"], out=po_sb[:], in0=Py[:].rearrange("p (g t) -> p g t", g=4),
              in1=ps_col[:].unsqueeze(2).broadcast_to([128, 4, 128]), op=ALU.mult)
            E("scalar", "activation", ["po_sb"], ["sq"], out=sq[:], in_=po_sb[:], func=AF.Square)
            for g in range(4):
                E("tensor", "matmul", ["sq", "onesb"], ["Ps"], out=Ps[:, 0:128], lhsT=onesb[:], rhs=sq[:, g, :], start=(g == 0), stop=(g == 3))
            rstd_ops(Ps[:, 0:128], prs[:], 128, ["Ps"], ["prs"], 512)
            E("vector", "tensor_tensor", ["po_sb", "pgc"], ["ptmp"], out=ptmp[:], in0=po_sb[:],
              in1=pg_col[:].unsqueeze(2).broadcast_to([128, 4, 128]), op=ALU.mult)
            pb_ = t % 2
            E("vector", "tensor_tensor", ["ptmp", "prs"], ["pmst%d" % pb_], out=pmst[pb_][:], in0=ptmp[:],
              in1=prs[:].unsqueeze(1).broadcast_to([128, 4, 128]), op=ALU.mult)
            DMA("sync", pm_s[t], pmst[pb_][:].rearrange("p g t -> p (g t)"), ["pmst%d" % pb_], [], "pms%d" % pb_)

        sec[0] = None
        for t0_ in range(min(3, NT)):
            m1_L(t0_)
        m1_N(0)
        m1_N(1)
        m1_T(0)
        for t in range(NT):
            if t + 3 < NT:
                m1_L(t + 3)
            if t + 2 < NT:
                m1_N(t + 2)
            if t + 1 < NT:
                m1_T(t + 1)
            m1_B1(t)
            if t >= 1:
                m1_B2(t - 1)
        m1_B2(NT - 1)
        sec[0] = None
        if stop_after == "M1":
            break

        S.barrier()
        A.reset(persist_mark)
        ao_all = A([128, NT, 512], F32)
        w_out = A([128, 8, D], BF16)
        G2 = A([128, D], F32)
        m3_mark = A.mark()
        vaug = A([128, NT, NH * VW], BF16)
        kaug = [A([128, S_LEN], BF16) for _ in range(2)]
        qaug = [A([128, S_LEN], BF16) for _ in range(2)]
        kmT = A([64, 16], F32)
        kmTb = A([64, 16], BF16)
        pastmask = A([128, NT * 16], F32)
        pastind = A([128, NT * 16], F32)
        ownfut = A([128, NT * 16], F32)
        Gm = A([128, NT * 16], F32)
        m8 = A([128, NT, 8], F32)
        sel = A([128, NT * 16], F32)
        biasW = A([128, NT, 80], BF16)
        PTb = [A([128, 512], BF16) for _ in range(3)]
        rl = A([128, 4], F32)
        osb = A([128, 512], F32)
        for v4 in range(4):
            DMA("sync", vaug[:, v4 * 8:(v4 + 1) * 8, :], v_s[v4 * 1024:(v4 + 1) * 1024, :].rearrange("(t p) f -> p t f", p=128), [], ["vaug"], None)
        DMA("sync", pastmask[:], I["pastmask"], [], ["pastmask"], None)
        DMA("sync", pastind[:], I["pastind"], [], ["pastind"], None)
        DMA("sync", ownfut[:], I["ownfut"], [], ["ownfut"], None)
        for b_ in range(2):
            DMA("sync", kaug[b_][64:80, :], I["onehot"], [], ["kaug%d" % b_], None)
        E("gpsimd", "memset", [], ["biasW"], ap=biasW[:], constant=0.0)
        for kc in range(8):
            DMA("gpsimd", w_out[:, kc, :], I["w_out"][l][kc * 128:(kc + 1) * 128, :], [], ["w_out"], "wl%d" % (kc % 4))
        DMA("sync", G2[:], g_s[0], [], ["G2"], None)
        SB = PB[0:3]
        OB = PB[3:5]
        GP = PB[5]
        BT = PT[0]
        LA = 2

        def prologue_A(h):
            hb = h % 2
            kk = "kaug%d" % hb
            kq = "qaug%d" % hb
            DMA("sync", kaug[hb][0:64, :], kT_s[h], [], [kk], "kl%d" % hb)
            DMA("sync", qaug[hb][0:64, :], qT_s[h], [], [kq], "ql%d" % hb)
            E("vector", "tensor_reduce", [kk], ["kmT"], out=kmT[:], in_=kaug[hb][0:64, :].rearrange("p (n k) -> p n k", n=16), axis=AX.X, op=ALU.add)
            E("vector", "tensor_scalar", ["kmT"], ["kmTb"], out=kmTb[:], in0=kmT[:], scalar1=1.0 / 256, scalar2=None, op0=ALU.mult)
            for qt_ in range(NT):
                E("tensor", "matmul", [kq, "kmTb"], ["GP"], out=GP[:, qt_ * 16:(qt_ + 1) * 16], lhsT=qaug[hb][0:64, qt_ * 128:(qt_ + 1) * 128],
                  rhs=kmTb[:], start=True, stop=True)
            E("vector", "tensor_tensor", ["GP", "pastmask"], ["Gm"], out=Gm[:], in0=GP[:], in1=pastmask[:], op=ALU.add)
            for qt_ in range(NT):
                E("vector", "max", ["Gm"], ["m8"], out=m8[:, qt_, :], in_=Gm[:, qt_ * 16:(qt_ + 1) * 16])
            E("vector", "tensor_tensor", ["Gm", "m8"], ["sel"], out=sel[:].rearrange("p (q n) -> p q n", n=16),
              in0=Gm[:].rearrange("p (q n) -> p q n", n=16), in1=m8[:, :, 2:3].broadcast_to([128, NT, 16]), op=ALU.is_ge)
            E("vector", "tensor_scalar", ["sel"], ["sel"], out=sel[:], in0=sel[:], scalar1=-1.0, scalar2=BIG, op0=ALU.add, op1=ALU.mult)
            E("vector", "tensor_tensor", ["sel", "pastind"], ["sel"], out=sel[:], in0=sel[:], in1=pastind[:], op=ALU.mult)
            E("vector", "tensor_tensor", ["sel", "ownfut"], ["biasW"], out=biasW[:, :, 64:80], in0=sel[:].rearrange("p (q n) -> p q n", n=16),
              in1=ownfut[:].rearrange("p (q n) -> p q n", n=16), op=ALU.add)

        def prologue_B(h):
            hb = h % 2
            kq = "qaug%d" % hb
            for r4 in range(4):
                for q8 in range(8):
                    qt_ = r4 * 8 + q8
                    E("tensor", "transpose", ["biasW", "identb"], ["BT"], out=BT[0:80, q8 * 128:(q8 + 1) * 128], in_=biasW[:, qt_, :], identity=identb[:])
                E("vector", "tensor_copy", ["BT"], [kq], out=qaug[hb][64:80, r4 * 1024:(r4 + 1) * 1024], in_=BT[64:80, :])

        steps = [(h, g, kt) for h in range(NH) for g in range(8) for kt in range(4 * g + 4)]
        NPH = len(steps) // NH

        def emit_score(i):
            h, g, kt = steps[i]
            hb = h % 2
            kk = "kaug%d" % hb
            kq = "qaug%d" % hb
            sb_ = SB[i % 3]
            ks = "SB%d" % (i % 3)
            pt_ = PTb[i % 3]
            kpt = "PTb%d" % (i % 3)
            E("tensor", "matmul", [kk, kq], [ks], out=sb_[:], lhsT=kaug[hb][0:80, kt * 128:(kt + 1) * 128],
              rhs=qaug[hb][0:80, g * 512:(g + 1) * 512], start=True, stop=True)
            E("scalar", "activation", [ks], [kpt], out=pt_[:], in_=sb_[:], func=AF.Exp)
            r = kt - 4 * g
            if r >= 0:
                E("gpsimd", "tensor_tensor", [kpt, "tri"], [kpt], out=pt_[:, r * 128:(r + 1) * 128], in0=pt_[:, r * 128:(r + 1) * 128],
                  in1=tri[:], op=ALU.mult)

        def emit_pv(i):
            h, g, kt = steps[i]
            gg = h * 8 + g
            ob = OB[gg % 2]
            ko = "OB%d" % (gg % 2)
            pt_ = PTb[i % 3]
            kpt = "PTb%d" % (i % 3)
            for qi in range(4):
                if kt <= 4 * g + qi:
                    E("tensor", "matmul", [kpt, "vaug"], [ko], out=ob[:, qi * 128:qi * 128 + 65], lhsT=pt_[:, qi * 128:(qi + 1) * 128],
                      rhs=vaug[:, kt, h * VW:h * VW + 65], start=(kt == 0 and qi == 0), stop=(kt == 4 * g + qi))
            if kt == 4 * g + 3:
                E("vector", "tensor_copy", [ko], ["osb"], out=osb[:], in_=ob[:])
                obv = osb[:].rearrange("p (q c) -> p q c", q=4)
                E("vector", "reciprocal", ["osb"], ["rl"], out=rl[:].unsqueeze(2), in_=obv[:, :, 64:65])
                E("vector", "tensor_tensor", ["osb", "rl"], ["ao_all"], out=ao_all[:, 4 * g:4 * g + 4, h * 64:(h + 1) * 64], in0=obv[:, :, 0:64],
                  in1=rl[:].unsqueeze(2).broadcast_to([128, 4, 64]), op=ALU.mult)

        prologue_A(0)
        prologue_B(0)
        for i in range(len(steps) + LA):
            if i < len(steps):
                emit_score(i)
            j = i - LA
            if j >= 0:
                emit_pv(j)
                h = steps[j][0]
                pos = j - h * NPH
                if h + 1 < NH:
                    if pos == NPH // 2:
                        prologue_A(h + 1)
                    if pos == NPH - 16:
                        prologue_B(h + 1)
        if debug:
            for v4 in range(4):
                DMA("sync", dbg["ao"][v4 * 1024:(v4 + 1) * 1024, :].rearrange("(t p) f -> p t f", p=128), ao_all[:, v4 * 8:(v4 + 1) * 8, :], ["ao_all"], [], None)
        S.barrier()
        A.reset(m3_mark)
        xt = [A([128, D], F32) for _ in range(3)]
        junk = A([128, D], BF16)
        junk2 = A([128, 512], BF16)
        ssa = [A([128, 1], F32) for _ in range(2)]
        rstda = [A([128, 1], F32) for _ in range(2)]
        ss = [A([128, 1], F32) for _ in range(2)]
        rstd = [A([128, 1], F32) for _ in range(2)]
        an = [A([128, 512], BF16) for _ in range(2)]
        mTa = [A([128, 4, 128], BF16) for _ in range(2)]
        pmt = [A([128, 4, 128], BF16) for _ in range(3)]
        tt = [A([128, D], F32) for _ in range(2)]
        xo = [A([128, D], F32) for _ in range(2)]
        YY = [[PB[0], PB[1]], [PB[2], PB[3]]]
        TA = PT[1]

        def m3_L(t):
            DMA("sync", xt[t % 3][:], x_src[t * 128:(t + 1) * 128, :], [], ["xt%d" % (t % 3)], "xl%d" % (t % 3))
            DMA("sync", pmt[t % 3][:].rearrange("p g t -> p (g t)"), pm_s[t], [], ["pmt%d" % (t % 3)], "pml%d" % (t % 3))

        def m3_A(t):
            b2 = t % 2
            E("scalar", "activation", ["ao_all"], ["junk2", "ssa%d" % b2], out=junk2[:], in_=ao_all[:, t, :], func=AF.Square, accum_out=ssa[b2][:])
            rstd_ops(ssa[b2][:], rstda[b2][:], 1, ["ssa%d" % b2], ["rstda%d" % b2], 512)
            E("vector", "tensor_scalar", ["ao_all", "rstda%d" % b2], ["an%d" % b2], out=an[b2][:], in0=ao_all[:, t, :], scalar1=rstda[b2][:, 0:1],
              scalar2=None, op0=ALU.mult)
            for c4 in range(4):
                E("tensor", "transpose", ["an%d" % b2, "identb"], ["TA"], out=TA[:, c4 * 128:(c4 + 1) * 128], in_=an[b2][:, c4 * 128:(c4 + 1) * 128],
                  identity=identb[:])
            for c4 in range(4):
                E("scalar", "activation", ["TA", "aog"], ["mTa%d" % b2], out=mTa[b2][:, c4, :], in_=TA[:, c4 * 128:(c4 + 1) * 128], func=AF.Identity,
                  scale=aog_col[:, c4:c4 + 1])

        def m3_B(t):
            b2 = t % 2
            xs = xt[t % 3]
            kx = "xt%d" % (t % 3)
            Y = YY[b2]
            kY = ["PB%d" % (2 * b2), "PB%d" % (2 * b2 + 1)]
            for nb in range(2):
                for c8 in range(8):
                    lhs = mTa[b2][:, c8, :] if c8 < 4 else pmt[t % 3][:, c8 - 4, :]
                    E("tensor", "matmul", ["mTa%d" % b2, "pmt%d" % (t % 3), "w_out"], [kY[nb]], out=Y[nb][:], lhsT=lhs,
                      rhs=w_out[:, c8, nb * 512:(nb + 1) * 512], start=(c8 == 0), stop=(c8 == 7))
            post_norm_residual(E, rstd_ops, Y, kY, junk, "junk", ss[b2], "ss%d" % b2, rstd[b2], "rstd%d" % b2, G2, "G2",
                               tt[b2], "tt%d" % b2, xs, kx, xo[b2], "xo%d" % b2)
            DMA("sync", x_mid[t * 128:(t + 1) * 128, :], xo[b2][:], ["xo%d" % b2], [], "xst%d" % b2)

        m3_L(0)
        m3_L(1)
        m3_A(0)
        for t in range(NT):
            if t + 2 < NT:
                m3_L(t + 2)
            if t + 1 < NT:
                m3_A(t + 1)
            m3_B(t)
        if stop_after == "M3":
            break

        S.barrier()
        A.reset(persist_mark)
        w_up = A([128, 8, 2 * DFF], BF16)
        w_dn = A([128, NPAIR, D], BF16)
        G4 = A([128, D], F32)
        xt = [A([128, D], F32) for _ in range(2)]
        junk = A([128, D], BF16)
        ssa = [A([128, 1], F32) for _ in range(2)]
        rstda = [A([128, 1], F32) for _ in range(2)]
        ss = [A([128, 1], F32) for _ in range(2)]
        rstd = [A([128, 1], F32) for _ in range(2)]
        xn = [A([128, D], BF16) for _ in range(2)]
        hTg = A([128, 8, 512], BF16)
        mT = A([128, NPAIR, 512], BF16)
        acc = [[A([128, 512], F32) for _ in range(2)] for _ in range(2)]
        usb = [[A([128, 514], F32) for _ in range(2)] for _ in range(2)]
        hist = A([128, NFC, 2], F32)
        tt = [A([128, D], F32) for _ in range(2)]
        for kc in range(8):
            for hf in range(2):
                DMA("gpsimd", w_up[:, kc, hf * DFF:(hf + 1) * DFF], I["w_up"][l][kc * 128:(kc + 1) * 128, hf * DFF:(hf + 1) * DFF], [], ["w_up"], "wl%d" % ((2 * kc + hf) % 4))
        for c4 in range(0, NPAIR, 2):
            DMA("gpsimd", w_dn[:, c4:c4 + 2, :], I["w_down"][l][c4 * 128:(c4 + 2) * 128, :].rearrange("(c p) n -> p c n", p=128), [], ["w_dn"], "wl%d" % ((c4 // 2) % 4))
        DMA("sync", G4[:], g_s[1], [], ["G4"], None)
        E("gpsimd", "memset", [], ["hist"], ap=hist[:], constant=0.0)
        TF = PT[0]
        UB = [[PB[0], PB[1]], [PB[2], PB[3]]]
        Y = [PB[4], PB[5]]
        xcnt = [0]

        def xload(t):
            sl = xcnt[0] % 2
            xcnt[0] += 1
            DMA("sync", xt[sl][:], x_mid[t * 128:(t + 1) * 128, :], [], ["xt%d" % sl], "xl%d" % sl)
            return xt[sl], "xt%d" % sl

        def f_A(g, ti):
            t = g * 4 + ti
            b2 = t % 2
            xs, kx = xload(t)
            E("scalar", "activation", [kx], ["junk", "ssa%d" % b2], out=junk[:], in_=xs[:], func=AF.Square, accum_out=ssa[b2][:])
            rstd_ops(ssa[b2][:], rstda[b2][:], 1, ["ssa%d" % b2], ["rstda%d" % b2], D)
            E("vector", "tensor_scalar", [kx, "rstda%d" % b2], ["xn%d" % b2], out=xn[b2][:], in0=xs[:], scalar1=rstda[b2][:, 0:1],
              scalar2=None, op0=ALU.mult)
            for kc in range(8):
                E("tensor", "transpose", ["xn%d" % b2, "identb"], ["TF"], out=TF[:, kc * 128:(kc + 1) * 128],
                  in_=xn[b2][:, kc * 128:(kc + 1) * 128], identity=identb[:])
            for kc in range(8):
                E("scalar", "activation", ["TF", "cols2", "cols3"], ["hTg"], out=hTg[:, kc, ti * 128:(ti + 1) * 128],
                  in_=TF[:, kc * 128:(kc + 1) * 128], func=AF.Identity, scale=cols[:, 2, kc:kc + 1], bias=cols[:, 3, kc:kc + 1])

        pidx = 0
        pend = []

        def silu_flush():
            while pend:
                pb_, i_ = pend.pop(0)
                ka0 = "acc%d0" % pb_
                ka1 = "acc%d1" % pb_
                E("scalar", "activation", [ka0], [ka0], out=acc[pb_][0][:], in_=acc[pb_][0][:], func=AF.Silu)
                E("gpsimd", "tensor_tensor", [ka0, ka1], ["mT"], out=mT[:, i_, :], in0=acc[pb_][0][:], in1=acc[pb_][1][:], op=ALU.mult)

        for ti in range(4):
            f_A(0, ti)
        for g in range(8):
            for i in range(NPAIR):
                pb = pidx % 2
                pidx += 1
                halves = ((0, i), (1, NPAIR + i))
                for half, ch in halves:
                    U = UB[pb][half]
                    kU = "PB%d" % (2 * pb + half)
                    for kc in range(8):
                        E("tensor", "matmul", ["hTg", "w_up"], [kU], out=U[:], lhsT=w_up[:, kc, ch * 128:(ch + 1) * 128], rhs=hTg[:, kc, :],
                          start=(kc == 0), stop=(kc == 7))
                for half, ch in halves:
                    U = UB[pb][half]
                    kU = "PB%d" % (2 * pb + half)
                    ub = usb[pb][half]
                    ku = "usb%d%d" % (pb, half)
                    E("gpsimd", "tensor_copy", ["hist%d" % ch], [ku + "h"], out=ub[:, 0:2], in_=hist[:, ch, :])
                    E("scalar", "activation", [kU], [ku], out=ub[:, 2:514], in_=U[:], func=AF.Copy)
                    E("scalar", "activation", [kU, "cv2", "cv3"], ["acc%d%d" % (pb, half)], out=acc[pb][half][:], in_=U[:], func=AF.Identity,
                      scale=cv_col[:, 2, ch:ch + 1], bias=cv_col[:, 3, ch:ch + 1])
                    if g < 7:
                        E("gpsimd", "tensor_copy", [ku], ["hist%d" % ch], out=hist[:, ch, :], in_=ub[:, 512:514])
                silu_flush()
                for half, ch in halves:
                    ub = usb[pb][half]
                    ku = "usb%d%d" % (pb, half)
                    ac = acc[pb][half]
                    ka = "acc%d%d" % (pb, half)
                    E("vector", "scalar_tensor_tensor", [ku, ku + "h", ka, "cv1"], [ka], out=ac[:], in0=ub[:, 1:513], scalar=cv_col[:, 1, ch:ch + 1],
                      in1=ac[:], op0=ALU.mult, op1=ALU.add)
                for half, ch in halves:
                    ub = usb[pb][half]
                    ku = "usb%d%d" % (pb, half)
                    ac = acc[pb][half]
                    ka = "acc%d%d" % (pb, half)
                    E("vector", "scalar_tensor_tensor", [ku, ku + "h", ka, "cv0"], [ka], out=ac[:], in0=ub[:, 0:512], scalar=cv_col[:, 0, ch:ch + 1],
                      in1=ac[:], op0=ALU.mult, op1=ALU.add)
                pend.append((pb, i))
            silu_flush()
            for ti in range(4):
                t = g * 4 + ti
                b2 = t % 2
                for nb in range(2):
                    for i in range(NPAIR):
                        E("tensor", "matmul", ["mT", "w_dn"], ["PB%d" % (4 + nb)], out=Y[nb][:], lhsT=mT[:, i, ti * 128:(ti + 1) * 128],
                          rhs=w_dn[:, i, nb * 512:(nb + 1) * 512], start=(i == 0), stop=(i == NPAIR - 1))
                if g + 1 < 8:
                    f_A(g + 1, ti)
                xs, kx = xload(t)
                post_norm_residual(E, rstd_ops, Y, ["PB4", "PB5"], junk, "junk", ss[b2], "ss%d" % b2, rstd[b2], "rstd%d" % b2, G4, "G4",
                                   tt[b2], "tt%d" % b2, xs, kx, tt[b2], "tt%d" % b2)
                DMA("sync", x_dst[t * 128:(t + 1) * 128, :], tt[b2][:], ["tt%d" % b2], [], "xst%d" % b2)
        x_src = x_dst

    final = [k for k in S.dma_counts.keys()]
    nsem = S.emit(final_wait_semkeys=final)
    return nc


def post_norm_residual(E, rstd_ops, Y, kY, junk, kjunk, ss, kss, rstd, krstd, G, kG, tt, ktt, xs, kx, xo, kxo):
    ssb = ss
    E("scalar", "activation", [kY[0]], [kjunk, kss, "lk" + kY[0]], out=junk[:, 0:512], in_=Y[0][:], func=AF.Square, accum_out=ssb[:])
    E("vector", "tensor_copy", [kY[0], "lk" + kY[0]], [ktt], out=tt[:, 0:512], in_=Y[0][:])
    E("scalar", "activation", [kY[1]], [kjunk, krstd, "lk" + kY[1]], out=junk[:, 512:1024], in_=Y[1][:], func=AF.Square, accum_out=rstd[:])
    E("vector", "tensor_copy", [kY[1], "lk" + kY[1]], [ktt], out=tt[:, 512:1024], in_=Y[1][:])
    E("vector", "tensor_tensor", [kss, krstd], [kss], out=ssb[:], in0=ssb[:], in1=rstd[:], op=ALU.add)
    rstd_ops(ssb[:], rstd[:], 1, [kss], [krstd], D)
    E("vector", "scalar_tensor_tensor", [ktt, krstd, kG], [ktt], out=tt[:], in0=tt[:], scalar=rstd[:, 0:1],
      in1=G[:], op0=ALU.mult, op1=ALU.mult)
    E("gpsimd", "tensor_tensor", [ktt, kx], [kxo], out=xo[:], in0=tt[:], in1=xs[:], op=ALU.add)


_CONSTS = None


def make_in_maps(inputs):
    global _CONSTS
    if _CONSTS is None:
        _CONSTS = _const_tables()
    x = np.asarray(inputs["x"], dtype=np.float32)
    c = np.asarray(inputs["c"], dtype=np.float32)
    shared = {k: np.ascontiguousarray(np.asarray(inputs[k], dtype=np.float32)) for k in WEIGHT_SPECS if k != "c"}
    maps = []
    for b in range(8):
        m = {"x": np.ascontiguousarray(x[b]), "c": np.ascontiguousarray(c[b:b + 1])}
        m.update(shared)
        m.update(_CONSTS)
        maps.append(m)
    return maps


def kernel(**inputs):
    nc = build()
    maps = make_in_maps(inputs)
    res = run_bass_kernel_spmd(nc, maps, core_ids=list(range(8)))
    return np.stack([np.asarray(r["out"]) for r in res.results], axis=0).astype(np.float32)
```

```python
import contextlib
import os
import numpy as np
import ml_dtypes
import concourse.bass as bass
import concourse.mybir as mybir
from concourse.bass_utils import run_bass_kernel_spmd

F32 = mybir.dt.float32
BF16 = mybir.dt.bfloat16
AF = mybir.ActivationFunctionType
ALU = mybir.AluOpType
AX = mybir.AxisListType

S_LEN = 4096
D = 1024
NT = S_LEN // 128
NH = 8
HD = 64
DFF = 2816
NFC = 2 * DFF // 128
NPAIR = DFF // 128
EPS = 1e-6
BIG = 30000.0
POOL_W = (2, 4, 8, 16)
VW = 68

ENGINES = ("tensor", "vector", "scalar", "gpsimd", "sync")
SEM_LIMIT = 30000


class Op:
    __slots__ = ("eng", "fn", "deps", "is_dma", "ticket", "signal", "idx")

    def __init__(self, eng, fn, is_dma):
        self.idx = 0
        self.eng = eng
        self.fn = fn
        self.is_dma = is_dma
        self.deps = []
        self.ticket = None
        self.signal = False


class Sched:
    def __init__(self, nc):
        self.nc = nc
        self.q = {e: [] for e in ENGINES}
        self.last_w = {}
        self.readers = {}
        self.dma_counts = {}
        self.dma_last = {}
        self.bar = {e: [] for e in ENGINES}

    def barrier(self):
        deps = []
        for e in ENGINES:
            for o in reversed(self.q[e]):
                if not o.is_dma:
                    deps.append(o)
                    break
        deps.extend(self.dma_last.values())
        for e in ENGINES:
            self.bar[e] = list(deps)
        self.last_w = {}
        self.readers = {}

    def op(self, eng, fn, reads=(), writes=(), dma=False, semkey=None):
        o = Op(eng, fn, dma)
        deps = set()
        for k in reads:
            w = self.last_w.get(k)
            if w is not None:
                deps.add(w)
        for k in writes:
            w = self.last_w.get(k)
            if w is not None:
                deps.add(w)
            for r in self.readers.get(k, ()):
                deps.add(r)
        for d in deps:
            if d is o:
                continue
            if (not d.is_dma) and (not dma) and d.eng == eng:
                if eng == "tensor":
                    continue
                raw = any(self.last_w.get(k) is d for k in reads)
                if not raw:
                    continue
            o.deps.append(d)
        if dma and semkey in self.dma_last:
            o.deps.append(self.dma_last[semkey])
        if self.bar[eng]:
            for d in self.bar[eng]:
                if d.is_dma or d.eng != eng:
                    o.deps.append(d)
            self.bar[eng] = []
        best = {}
        pruned = []
        for d in o.deps:
            if d.is_dma:
                pruned.append(d)
            else:
                b = best.get(d.eng)
                if b is None or d.idx > b.idx:
                    best[d.eng] = d
        o.deps = pruned + list(best.values())
        o.idx = len(self.q[eng])
        for k in writes:
            self.last_w[k] = o
            self.readers[k] = []
        for k in reads:
            self.readers.setdefault(k, []).append(o)
        if dma:
            c = self.dma_counts.get(semkey, 0) + 16
            self.dma_counts[semkey] = c
            o.ticket = (("dma", semkey), c)
            self.dma_last[semkey] = o
        self.q[eng].append(o)
        return o

    def emit(self, final_wait_semkeys=()):
        nc = self.nc
        for e in ENGINES:
            for o in self.q[e]:
                for d in o.deps:
                    if not d.is_dma:
                        d.signal = True
        semnames = set()
        for e in ENGINES:
            cnt = 0
            seg = 0
            for o in self.q[e]:
                if o.is_dma:
                    semnames.add(o.ticket[0])
                    continue
                if o.signal:
                    cnt += 1
                    if cnt > SEM_LIMIT:
                        seg += 1
                        cnt = 1
                    o.ticket = (("eng", e, seg), cnt)
                    semnames.add(o.ticket[0])
        semnames = sorted(semnames, key=str)
        with contextlib.ExitStack() as st:
            sems = {}
            for i, n in enumerate(semnames):
                sems[n] = st.enter_context(nc.semaphore("s%d" % i))
            block = st.enter_context(nc.Block())

            def run(engname):
                def body(eng):
                    waited = {}
                    for o in self.q[engname]:
                        need = {}
                        for d in o.deps:
                            s, v = d.ticket
                            if need.get(s, 0) < v:
                                need[s] = v
                        for s, v in need.items():
                            if waited.get(s, 0) < v:
                                eng.wait_ge(sems[s], v)
                                waited[s] = v
                        ins = o.fn(eng)
                        if o.is_dma:
                            ins.then_inc(sems[o.ticket[0]], 16)
                        elif o.signal:
                            ins.then_inc(sems[o.ticket[0]], 1)
                    if engname == "sync":
                        for k in final_wait_semkeys:
                            s = ("dma", k)
                            v = self.dma_counts[k]
                            if waited.get(s, 0) < v:
                                eng.wait_ge(sems[s], v)
                                waited[s] = v
                return body

            block.tensor(run("tensor"))
            block.vector(run("vector"))
            block.scalar(run("scalar"))
            block.gpsimd(run("gpsimd"))
            block.sync(run("sync"))
        return len(semnames)


def _const_tables():
    bf = ml_dtypes.bfloat16
    pos = np.arange(S_LEN, dtype=np.float32)
    inv_freq = np.power(np.float32(500000.0), -np.arange(0, 16, 2, dtype=np.float32) / np.float32(16))
    ang = (pos[:, None] * inv_freq[None, :]).astype(np.float32)
    cos = np.cos(ang).astype(np.float32)
    sin = np.sin(ang).astype(np.float32)
    def tm(a):
        return np.ascontiguousarray(a.reshape(NT, 128, -1).transpose(1, 0, 2))
    ropek = tm(np.concatenate([cos, sin], axis=1)).astype(np.float32)
    ropeq = (ropek * np.float32(0.125)).astype(np.float32)
    qt = np.arange(NT)
    j = qt // 2
    n = np.arange(16)
    past = (n[None, :] < j[:, None])
    pastmask = np.where(past, 0.0, -1e30).astype(np.float32).reshape(1, NT * 16)
    pastind = past.astype(np.float32).reshape(1, NT * 16)
    ownfut = np.where(n[None, :] > j[:, None], -BIG, 0.0).astype(np.float32).reshape(1, NT * 16)
    onehot = (np.arange(S_LEN)[None, :] // 256 == n[:, None]).astype(np.float32).astype(bf)
    k = np.arange(128)
    tri = (k[None, :] >= k[:, None]).astype(np.float32).astype(bf)
    identb = np.eye(128, dtype=np.float32).astype(bf)
    identf = np.eye(128, dtype=np.float32)
    onesb = np.ones((128, 128), dtype=np.float32).astype(bf)
    rc = np.zeros((1, 4, 16), dtype=np.float32)
    for g, w in enumerate(POOL_W):
        rc[0, g, :] = 1.0 / np.minimum(np.arange(16) + 1, w)
    return {
        "ropeq": ropeq, "ropek": ropek,
        "pastmask": np.ascontiguousarray(np.broadcast_to(pastmask, (128, NT * 16))),
        "pastind": np.ascontiguousarray(np.broadcast_to(pastind, (128, NT * 16))),
        "ownfut": np.ascontiguousarray(np.broadcast_to(ownfut, (128, NT * 16))),
        "onehot": onehot, "tri": tri, "identb": identb, "identf": identf, "onesb": onesb,
        "rc": np.ascontiguousarray(np.broadcast_to(rc, (128, 4, 16))),
    }


CONST_SPECS = {
    "ropeq": ([128, NT, 16], F32), "ropek": ([128, NT, 16], F32),
    "pastmask": ([128, NT * 16], F32), "pastind": ([128, NT * 16], F32), "ownfut": ([128, NT * 16], F32),
    "onehot": ([16, S_LEN], BF16), "tri": ([128, 128], BF16), "identb": ([128, 128], BF16),
    "identf": ([128, 128], F32), "onesb": ([128, 128], BF16), "rc": ([128, 4, 16], F32),
}

WEIGHT_SPECS = {
    "c": [1, D], "w_ada": [2, D, 6 * D], "b_ada": [2, 6 * D], "g_pre_mix": [2, D], "w_in": [2, D, 2048],
    "w_pool": [2, 4, 128, 128], "pool_scale": [2, 512], "attn_out_gain": [2, 512], "pool_out_gain": [2, 512],
    "w_out": [2, D, D], "g_post_mix": [2, D], "g_pre_ffn": [2, D], "w_up": [2, D, 2 * DFF],
    "conv_w": [2, 3, 2 * DFF], "conv_b": [2, 2 * DFF], "w_down": [2, DFF, D], "g_post_ffn": [2, D],
}


class Alloc:
    def __init__(self, nc):
        self.nc = nc
        self.base = (nc.sbuf_base + 63) // 64 * 64
        self.top = nc.sbuf_top
        self.cur = self.base
        self.limit = self.top
        self.n = 0

    def mark(self):
        return self.cur

    def reset(self, m):
        self.cur = m

    def __call__(self, shape, dt):
        sz = 1
        for s in shape[1:]:
            sz *= s
        nbytes = sz * (4 if dt == F32 else 2)
        nbytes = (nbytes + 63) // 64 * 64
        off = self.cur
        assert off + nbytes <= self.limit, ("SBUF overflow", off, nbytes, self.limit)
        self.cur += nbytes
        self.n += 1
        return self.nc.alloc_sbuf_tensor_at("sb%d" % self.n, list(shape), dt, offset=off)


def build(n_layers=2, stop_after=None, debug=False):
    nc = bass.Bass("TRN2", target_bir_lowering=False)
    I = {}
    I["x"] = nc.dram_tensor("x", [S_LEN, D], F32, kind="ExternalInput").ap()
    for k, shp in WEIGHT_SPECS.items():
        I[k] = nc.dram_tensor(k, shp, F32, kind="ExternalInput").ap()
    for k, (shp, dt) in CONST_SPECS.items():
        I[k] = nc.dram_tensor(k, shp, dt, kind="ExternalInput").ap()
    out = nc.dram_tensor("out", [S_LEN, D], F32, kind="ExternalOutput").ap()
    sk = "ExternalOutput" if debug else "Internal"
    qT_s = nc.dram_tensor("qT_s", [NH, HD, S_LEN], BF16, kind=sk).ap()
    kT_s = nc.dram_tensor("kT_s", [NH, HD, S_LEN], BF16, kind=sk).ap()
    v_s = nc.dram_tensor("v_s", [S_LEN, NH * VW], BF16, kind=sk).ap()
    pm_s = nc.dram_tensor("pm_s", [NT, 128, 512], BF16, kind=sk).ap()
    g_s = nc.dram_tensor("g_s", [2, 128, D], F32, kind=sk).ap()
    xa = nc.dram_tensor("xa", [S_LEN, D], F32, kind=sk).ap()
    xb = nc.dram_tensor("xb", [S_LEN, D], F32, kind=sk).ap()
    dbg = {}
    if debug:
        dbg["ao"] = nc.dram_tensor("dbg_ao", [S_LEN, 512], F32, kind="ExternalOutput").ap()

    S = Sched(nc)
    A = Alloc(nc)
    PB = [nc.alloc_psum_tensor("pb%d" % i, [128, 512], F32) for i in range(6)]
    PT = [nc.alloc_psum_tensor("pt%d" % i, [128, 1024], BF16) for i in range(2)]

    sec = [None]
    enabled = os.environ.get("KSEC")
    nt_run = int(os.environ.get("KNT", NT))

    def skip():
        return enabled is not None and sec[0] is not None and sec[0] not in enabled

    def E(eng, meth, reads, writes, **kw):
        if skip():
            return None
        return S.op(eng, lambda e: getattr(e, meth)(**kw), reads, writes)

    otc = [0]

    def OT():
        otc[0] += 1
        return "ot%d" % (otc[0] % 6)

    def DMA(eng, out_, in_, reads, writes, semkey, slow=False):
        if skip():
            return None
        if semkey is None:
            semkey = OT()
        if slow:
            return S.op(eng, lambda e: e.dma_start(out=out_, in_=in_, allow_slow_non_contiguous=True),
                        reads, writes, dma=True, semkey=semkey)
        return S.op(eng, lambda e: e.dma_start(out=out_, in_=in_), reads, writes, dma=True, semkey=semkey)

    identb = A([128, 128], BF16)
    identf = A([128, 128], F32)
    onesb = A([128, 128], BF16)
    tri = A([128, 128], BF16)
    for nm, t in (("identb", identb), ("identf", identf), ("onesb", onesb), ("tri", tri)):
        DMA("sync", t[:], I[nm], [], [nm], None)
    cols = A([128, 4, 8], F32)
    aog_col = A([128, 4], F32)
    ps_col = A([128, 4], F32)
    pg_col = A([128, 4], F32)
    cv_col = A([128, 4, NFC], F32)
    rstd_eps = EPS
    persist_mark = A.mark()

    def rstd_ops(ss, rstd, n, keys_r, keys_w, dim):
        E("scalar", "activation", keys_r, keys_w, out=rstd, in_=ss, func=AF.Ln, scale=1.0 / dim, bias=rstd_eps)
        E("scalar", "activation", keys_w, keys_w, out=rstd, in_=rstd, func=AF.Exp, scale=-0.5)

    x_src = I["x"]
    for l in range(n_layers):
        last_layer = (l == n_layers - 1)
        x_mid = xa
        x_dst = out if last_layer else xb
        S.barrier()
        A.reset(persist_mark)
        A.limit = A.top
        c_col = A([128, 8], F32)
        cact = A([128, 8], F32)
        cbc = A([128, 8, 128], F32)
        bada = A([128, 6 * D], F32)
        modbc = A([128, 6 * D], F32)
        wblk = [A([128, 8, 512], F32) for _ in range(2)]
        gbc = A([128, D], F32)
        gtmp = A([128, D], F32)
        dtmp = A([128, 8, 128], F32)
        stg = [A([64, 128], F32) for _ in range(2)]
        stc = [0]

        def load_col(vec1d, n, dst, wkey):
            i_ = stc[0] % 2
            stc[0] += 1
            sg = stg[i_]
            DMA("sync", sg[0:n, :], vec1d.rearrange("(c p) -> c p", p=128), [], ["stg%d" % i_], None)
            E("tensor", "matmul", ["stg%d" % i_, "identf"], ["PB2"], out=PB[2][:, 0:n], lhsT=sg[0:n, :], rhs=identf[0:n, 0:n],
              start=True, stop=True)
            E("vector", "tensor_copy", ["PB2"], [wkey], out=dst, in_=PB[2][:, 0:n])

        load_col(I["c"][0], 8, c_col[:], "c_col")
        DMA("sync", bada[:], I["b_ada"][l].partition_broadcast(128), [], ["bada"], None)
        load_col(I["attn_out_gain"][l], 4, aog_col[:], "aog")
        load_col(I["pool_scale"][l], 4, ps_col[:], "psc")
        load_col(I["pool_out_gain"][l], 4, pg_col[:], "pgc")
        for j3 in range(3):
            load_col(I["conv_w"][l, j3], NFC, cv_col[:, j3, :], "cv%d" % j3)
        load_col(I["conv_b"][l], NFC, cv_col[:, 3, :], "cv3")
        E("scalar", "activation", ["c_col"], ["cact"], out=cact[:], in_=c_col[:], func=AF.Silu)
        E("vector", "tensor_copy", ["cact"], ["cbc"], out=cbc[:], in_=cact[:].unsqueeze(2).broadcast_to([128, 8, 128]))
        for blk in range(12):
            wb = wblk[blk % 2]
            kb = "wblk%d" % (blk % 2)
            pm_ = PB[blk % 2]
            kp = "PB%d" % (blk % 2)
            DMA("sync", wb[:], I["w_ada"][l][:, blk * 512:(blk + 1) * 512].rearrange("(kc p) n -> p kc n", p=128),
                [], [kb], "wada%d" % (blk % 2))
            for kc in range(8):
                E("tensor", "matmul", [kb, "cbc"], [kp], out=pm_[:], lhsT=cbc[:, kc, :], rhs=wb[:, kc, :],
                  start=(kc == 0), stop=(kc == 7))
            E("vector", "tensor_tensor", [kp, "bada"], ["modbc"], out=modbc[:, blk * 512:(blk + 1) * 512], in0=pm_[:],
              in1=bada[:, blk * 512:(blk + 1) * 512], op=ALU.add)

        def diag_extract(src_ap, dst_ap, rkeys, wkey):
            E("vector", "tensor_tensor", rkeys + ["identf"], ["dtmp"], out=dtmp[:],
              in0=src_ap.rearrange("p (k q) -> p k q", k=8),
              in1=identf[:].unsqueeze(1).broadcast_to([128, 8, 128]), op=ALU.mult)
            E("vector", "tensor_reduce", ["dtmp"], [wkey], out=dst_ap, in_=dtmp[:], axis=AX.X, op=ALU.add)

        for gi, (gname, moff) in enumerate((("g_post_mix", 2 * D), ("g_post_ffn", 5 * D))):
            DMA("sync", gbc[:], I[gname][l].partition_broadcast(128), [], ["gbc"], None)
            E("vector", "tensor_tensor", ["gbc", "modbc"], ["gtmp"], out=gtmp[:], in0=modbc[:, moff:moff + D], in1=gbc[:], op=ALU.mult)
            DMA("sync", g_s[gi], gtmp[:], ["gtmp"], [], None)
        for ci, (gname, scoff, shoff) in enumerate((("g_pre_mix", 1 * D, 0), ("g_pre_ffn", 4 * D, 3 * D))):
            DMA("sync", gbc[:], I[gname][l].partition_broadcast(128), [], ["gbc"], None)
            E("vector", "scalar_tensor_tensor", ["gbc", "modbc"], ["gtmp"], out=gtmp[:], in0=modbc[:, scoff:scoff + D],
              scalar=1.0, in1=gbc[:], op0=ALU.add, op1=ALU.mult)
            diag_extract(gtmp[:], cols[:, 2 * ci, :], ["gtmp"], "cols%d" % (2 * ci))
            diag_extract(modbc[:, shoff:shoff + D], cols[:, 2 * ci + 1, :], ["modbc"], "cols%d" % (2 * ci + 1))
        if stop_after == "P":
            break

        S.barrier()
        A.reset(persist_mark)
        w_in = A([128, 8, 2048], BF16)
        w_pool = A([128, 4, 128], BF16)
        ropeq = A([128, NT, 16], F32)
        ropek = A([128, NT, 16], F32)
        rc = A([128, 4, 16], F32)
        xt = [A([128, D], F32) for _ in range(3)]
        junk = A([128, D], BF16)
        ss = [A([128, 1], F32) for _ in range(3)]
        rstd = [A([128, 1], F32) for _ in range(3)]
        xn = [A([128, D], BF16) for _ in range(3)]
        hT = [A([128, 8, 128], BF16) for _ in range(2)]
        q_tm = [A([128, 512], BF16) for _ in range(2)]
        k_tm = [A([128, 512], BF16) for _ in range(2)]
        rt = [A([128, 8, 8], F32) for _ in range(4)]
        zq = A([128, 8, 16], F32)
        qst = [A([64, 2, 4, 512], BF16) for _ in range(2)]
        kst = [A([64, 2, 4, 512], BF16) for _ in range(2)]
        vst = [A([128, 8, VW], BF16) for _ in range(2)]
        pmst = [A([128, 4, 128], BF16) for _ in range(2)]
        pz = A([128, 4, 144], F32)
        s2 = A([128, 4, 144], F32)
        s4 = A([128, 4, 144], F32)
        s8 = A([128, 4, 144], F32)
        s16 = A([128, 4, 144], F32)
        pooledT = [A([128, 4, 128], BF16) for _ in range(2)]
        po_sb = A([128, 4, 128], F32)
        sq = A([128, 4, 128], BF16)
        prs = A([128, 128], F32)
        ptmp = A([128, 4, 128], F32)
        etmp = A([128, 4, 16], F32)

        sec[0] = "L"
        for kc in range(8):
            DMA("gpsimd", w_in[:, kc, :], I["w_in"][l][kc * 128:(kc + 1) * 128, :], [], ["w_in"], "wl%d" % (kc % 4))
        DMA("gpsimd", w_pool[:], I["w_pool"][l].rearrange("g c e -> c g e"), [], ["w_pool"], "wl0")
        DMA("sync", ropeq[:], I["ropeq"], [], ["ropeq"], None)
        DMA("sync", ropek[:], I["ropek"], [], ["ropek"], None)
        DMA("sync", rc[:], I["rc"], [], ["rc"], None)
        for b_ in range(2):
            E("gpsimd", "memset", [], ["vst%d" % b_], ap=vst[b_][:], constant=1.0)
        E("gpsimd", "memset", [], ["pz"], ap=pz[:], constant=0.0)
        Pq, Pk, Pv, Pp, Py, Ps = PB
        T0, T1 = PT[0], PT[1]
        def m1_L(t):
            DMA("sync", xt[t % 3][:], x_src[t * 128:(t + 1) * 128, :], [], ["xt%d" % (t % 3)], "xl%d" % (t % 3))

        def m1_N(t):
            b3 = t % 3
            xs = xt[b3]
            kx = "xt%d" % b3
            E("scalar", "activation", [kx], ["junk", "ss%d" % b3], out=junk[:], in_=xs[:], func=AF.Square, accum_out=ss[b3][:])
            rstd_ops(ss[b3][:], rstd[b3][:], 1, ["ss%d" % b3], ["rstd%d" % b3], D)
            E("vector", "tensor_scalar", [kx, "rstd%d" % b3], ["xn%d" % b3], out=xn[b3][:], in0=xs[:], scalar1=rstd[b3][:, 0:1],
              scalar2=None, op0=ALU.mult)

        def m1_T(t):
            b3 = t % 3
            b2 = t % 2
            for kc in range(8):
                E("tensor", "transpose", ["xn%d" % b3, "identb"], ["T0"], out=T0[:, kc * 128:(kc + 1) * 128],
                  in_=xn[b3][:, kc * 128:(kc + 1) * 128], identity=identb[:])
            for kc in range(8):
                E("scalar", "activation", ["T0", "cols0", "cols1"], ["hT%d" % b2], out=hT[b2][:, kc, :], in_=T0[:, kc * 128:(kc + 1) * 128],
                  func=AF.Identity, scale=cols[:, 0, kc:kc + 1], bias=cols[:, 1, kc:kc + 1])

        def m1_B1(t):
            b2 = t % 2
            for P_, kP, c0 in ((Pq, "Pq", 0), (Pk, "Pk", 512), (Pv, "Pv", 1024)):
                for kc in range(8):
                    E("tensor", "matmul", ["hT%d" % b2, "w_in"], [kP], out=P_[:], lhsT=hT[b2][:, kc, :], rhs=w_in[:, kc, c0:c0 + 512],
                      start=(kc == 0), stop=(kc == 7))
            for g in range(4):
                for kc in range(8):
                    E("tensor", "matmul", ["hT%d" % b2, "w_in"], ["Pp"], out=Pp[:, g * 128:(g + 1) * 128],
                      lhsT=w_in[:, kc, 1536 + g * 128:1536 + (g + 1) * 128], rhs=hT[b2][:, kc, :], start=(kc == 0), stop=(kc == 7))
            for P_, kP, tm_, ktm, rope, krope, sc_ in ((Pq, "Pq", q_tm[b2], "q_tm%d" % b2, ropeq, "ropeq", 0.125),
                                                       (Pk, "Pk", k_tm[b2], "k_tm%d" % b2, ropek, "ropek", 1.0)):
                E("scalar", "activation", [kP], [ktm], out=tm_[:], in_=P_[:], func=AF.Identity, scale=sc_)
                pv = P_[:].rearrange("p (h d) -> p h d", h=8)
                ov = tm_[:].rearrange("p (h d) -> p h d", h=8)
                E("scalar", "activation", [kP], ["zq"], out=zq[:], in_=pv[:, :, 0:16], func=AF.Copy)
                cosb = rope[:, t, 0:8].unsqueeze(1).broadcast_to([128, 8, 8])
                sinb = rope[:, t, 8:16].unsqueeze(1).broadcast_to([128, 8, 8])
                x1 = zq[:, :, 0:8]
                x2 = zq[:, :, 8:16]
                E("vector", "tensor_tensor", ["zq", krope], ["rt0"], out=rt[0][:], in0=x1, in1=cosb, op=ALU.mult)
                E("vector", "tensor_tensor", ["zq", krope], ["rt1"], out=rt[1][:], in0=x2, in1=sinb, op=ALU.mult)
                E("vector", "tensor_tensor", ["zq", krope], ["rt2"], out=rt[2][:], in0=x2, in1=cosb, op=ALU.mult)
                E("vector", "tensor_tensor", ["zq", krope], ["rt3"], out=rt[3][:], in0=x1, in1=sinb, op=ALU.mult)
                E("vector", "tensor_tensor", ["rt0", "rt1"], [ktm], out=ov[:, :, 0:8], in0=rt[0][:], in1=rt[1][:], op=ALU.subtract)
                E("vector", "tensor_tensor", ["rt2", "rt3"], [ktm], out=ov[:, :, 8:16], in0=rt[2][:], in1=rt[3][:], op=ALU.add)
            vb = t % 2
            E("scalar", "activation", ["Pv"], ["vst%d" % vb], out=vst[vb][:, :, 0:64], in_=Pv[:].rearrange("p (h d) -> p h d", h=8),
              func=AF.Copy)
            DMA("sync", v_s[t * 128:(t + 1) * 128, :], vst[vb][:].rearrange("p h d -> p (h d)"), ["vst%d" % vb], [], "vs%d" % vb)
            pT = pooledT[b2]
            kpT = "pooledT%d" % b2
            E("scalar", "activation", ["Pp"], ["pz"], out=pz[:, :, 16:144], in_=Pp[:].rearrange("p (g t) -> p g t", g=4), func=AF.Copy)
            E("gpsimd", "tensor_tensor", ["pz"], ["s2"], out=s2[:, :, 1:144], in0=pz[:, :, 1:144], in1=pz[:, :, 0:143], op=ALU.add)
            E("gpsimd", "tensor_tensor", ["s2"], ["s4"], out=s4[:, 1:4, 3:144], in0=s2[:, 1:4, 3:144], in1=s2[:, 1:4, 1:142], op=ALU.add)
            E("gpsimd", "tensor_tensor", ["s4"], ["s8"], out=s8[:, 2:4, 7:144], in0=s4[:, 2:4, 7:144], in1=s4[:, 2:4, 3:140], op=ALU.add)
            E("gpsimd", "tensor_tensor", ["s8"], ["s16"], out=s16[:, 3:4, 15:144], in0=s8[:, 3:4, 15:144], in1=s8[:, 3:4, 7:136], op=ALU.add)
            sums = (s2, s4, s8, s16)
            for g in range(4):
                E("vector", "scalar_tensor_tensor", ["s%d" % (2 << g), "pz"], [kpT], out=pT[:, g, :], in0=sums[g][:, g, 16:144],
                  scalar=1.0 / POOL_W[g], in1=pz[:, g, 16:144], op0=ALU.mult, op1=ALU.subtract)
            if t == 0:
                for g in range(4):
                    E("gpsimd", "tensor_tensor", ["s%d" % (2 << g), "rc"], ["etmp"], out=etmp[:, g, :], in0=sums[g][:, g, 16:32], in1=rc[:, g, :], op=ALU.mult)
                    E("gpsimd", "tensor_tensor", ["etmp", "pz"], [kpT], out=pT[:, g, 0:16], in0=etmp[:, g, :], in1=pz[:, g, 16:32], op=ALU.subtract)
            E("gpsimd", "tensor_copy", ["pz", "s2", "s4", "s8", "s16", kpT], ["pz"], out=pz[:, :, 0:16], in_=pz[:, :, 128:144])

        def m1_B2(t):
            b2 = t % 2
            g4 = t // 4
            ti = t % 4
            sb_ = g4 % 2
            pT = pooledT[b2]
            kpT = "pooledT%d" % b2
            for g in range(4):
                E("tensor", "matmul", [kpT, "w_pool"], ["Py"], out=Py[:, g * 128:(g + 1) * 128], lhsT=w_pool[:, g, :], rhs=pT[:, g, :],
                  start=True, stop=True)
            for hi, (tm_, ktm, st_, kst_) in enumerate(((q_tm[b2], "q_tm%d" % b2, qst[sb_], "qst%d" % sb_), (k_tm[b2], "k_tm%d" % b2, kst[sb_], "kst%d" % sb_))):
                kT1 = "T1"
                for pr in range(4):
                    E("tensor", "transpose", [ktm, "identb"], [kT1], out=T1[:, hi * 512 + pr * 128:hi * 512 + (pr + 1) * 128],
                      in_=tm_[:, pr * 128:(pr + 1) * 128], identity=identb[:])
                for pa in range(2):
                    E("vector", "tensor_copy", [kT1], [kst_], out=st_[:, pa, :, ti * 128:(ti + 1) * 128],
                      in_=T1[pa * 64:(pa + 1) * 64, hi * 512:(hi + 1) * 512].rearrange("p (r t) -> p r t", r=4))
            if ti == 3:
                for pa in range(2):
                    DMA("sync", qT_s.rearrange("(pr pa) d t -> pa d pr t", pa=2)[pa][:, :, g4 * 512:(g4 + 1) * 512], qst[sb_][:, pa, :, :],
                        ["qst%d" % sb_], [], "qs%d%d" % (sb_, pa))
                    DMA("sync", kT_s.rearrange("(pr pa) d t -> pa d pr t", pa=2)[pa][:, :, g4 * 512:(g4 + 1) * 512], kst[sb_][:, pa, :, :],
                        ["kst%d" % sb_], [], "ks%d%d" % (sb_, pa))
            E("vector", "tensor_tensor", ["Py", "psc"], ["po_sb"], out=po_sb[:], in0=Py[:].rearrange("p (g t) -> p g t", g=4),
              in1=ps_col[:].unsqueeze(2).broadcast_to([128, 4, 128]), op=ALU.mult)
            E("scalar", "activation", ["po_sb"], ["sq"], out=sq[:], in_=po_sb[:], func=AF.Square)
            for g in range(4):
                E("tensor", "matmul", ["sq", "onesb"], ["Ps"], out=Ps[:, 0:128], lhsT=onesb[:], rhs=sq[:, g, :], start=(g == 0), stop=(g == 3))
            rstd_ops(Ps[:, 0:128], prs[:], 128, ["Ps"], ["prs"], 512)
            E("vector", "tensor_tensor", ["po_sb", "pgc"], ["ptmp"], out=ptmp[:], in0=po_sb[:],
              in1=pg_col[:].unsqueeze(2).broadcast_to([128, 4, 128]), op=ALU.mult)
            pb_ = t % 2
            E("vector", "tensor_tensor", ["ptmp", "prs"], ["pmst%d" % pb_], out=pmst[pb_][:], in0=ptmp[:],
              in1=prs[:].unsqueeze(1).broadcast_to([128, 4, 128]), op=ALU.mult)
            DMA("sync", pm_s[t], pmst[pb_][:].rearrange("p g t -> p (g t)"), ["pmst%d" % pb_], [], "pms%d" % pb_)

        sec[0] = None
        for t0_ in range(min(3, NT)):
            m1_L(t0_)
        m1_N(0)
        m1_N(1)
        m1_T(0)
        for t in range(NT):
            if t + 3 < NT:
                m1_L(t + 3)
            if t + 2 < NT:
                m1_N(t + 2)
            if t + 1 < NT:
                m1_T(t + 1)
            m1_B1(t)
            if t >= 1:
                m1_B2(t - 1)
        m1_B2(NT - 1)
        sec[0] = None
        if stop_after == "M1":
            break

        S.barrier()
        A.reset(persist_mark)
        ao_all = A([128, NT, 512], F32)
        w_out = A([128, 8, D], BF16)
        G2 = A([128, D], F32)
        m3_mark = A.mark()
        vaug = A([128, NT, NH * VW], BF16)
        kaug = [A([128, S_LEN], BF16) for _ in range(2)]
        qaug = [A([128, S_LEN], BF16) for _ in range(2)]
        kmT = A([64, 16], F32)
        kmTb = A([64, 16], BF16)
        pastmask = A([128, NT * 16], F32)
        pastind = A([128, NT * 16], F32)
        ownfut = A([128, NT * 16], F32)
        Gm = A([128, NT * 16], F32)
        m8 = A([128, NT, 8], F32)
        sel = A([128, NT * 16], F32)
        biasW = A([128, NT, 80], BF16)
        PTb = [A([128, 512], BF16) for _ in range(3)]
        rl = A([128, 4], F32)
        osb = A([128, 512], F32)
        for v4 in range(4):
            DMA("sync", vaug[:, v4 * 8:(v4 + 1) * 8, :], v_s[v4 * 1024:(v4 + 1) * 1024, :].rearrange("(t p) f -> p t f", p=128), [], ["vaug"], None)
        DMA("sync", pastmask[:], I["pastmask"], [], ["pastmask"], None)
        DMA("sync", pastind[:], I["pastind"], [], ["pastind"], None)
        DMA("sync", ownfut[:], I["ownfut"], [], ["ownfut"], None)
        for b_ in range(2):
            DMA("sync", kaug[b_][64:80, :], I["onehot"], [], ["kaug%d" % b_], None)
        E("gpsimd", "memset", [], ["biasW"], ap=biasW[:], constant=0.0)
        for kc in range(8):
            DMA("gpsimd", w_out[:, kc, :], I["w_out"][l][kc * 128:(kc + 1) * 128, :], [], ["w_out"], "wl%d" % (kc % 4))
        DMA("sync", G2[:], g_s[0], [], ["G2"], None)
        SB = PB[0:3]
        OB = PB[3:5]
        GP = PB[5]
        BT = PT[0]
        LA = 2

        def prologue_A(h):
            hb = h % 2
            kk = "kaug%d" % hb
            kq = "qaug%d" % hb
            DMA("sync", kaug[hb][0:64, :], kT_s[h], [], [kk], "kl%d" % hb)
            DMA("sync", qaug[hb][0:64, :], qT_s[h], [], [kq], "ql%d" % hb)
            E("vector", "tensor_reduce", [kk], ["kmT"], out=kmT[:], in_=kaug[hb][0:64, :].rearrange("p (n k) -> p n k", n=16), axis=AX.X, op=ALU.add)
            E("vector", "tensor_scalar", ["kmT"], ["kmTb"], out=kmTb[:], in0=kmT[:], scalar1=1.0 / 256, scalar2=None, op0=ALU.mult)
            for qt_ in range(NT):
                E("tensor", "matmul", [kq, "kmTb"], ["GP"], out=GP[:, qt_ * 16:(qt_ + 1) * 16], lhsT=qaug[hb][0:64, qt_ * 128:(qt_ + 1) * 128],
                  rhs=kmTb[:], start=True, stop=True)
            E("vector", "tensor_tensor", ["GP", "pastmask"], ["Gm"], out=Gm[:], in0=GP[:], in1=pastmask[:], op=ALU.add)
            for qt_ in range(NT):
                E("vector", "max", ["Gm"], ["m8"], out=m8[:, qt_, :], in_=Gm[:, qt_ * 16:(qt_ + 1) * 16])
            E("vector", "tensor_tensor", ["Gm", "m8"], ["sel"], out=sel[:].rearrange("p (q n) -> p q n", n=16),
              in0=Gm[:].rearrange("p (q n) -> p q n", n=16), in1=m8[:, :, 2:3].broadcast_to([128, NT, 16]), op=ALU.is_ge)
            E("vector", "tensor_scalar", ["sel"], ["sel"], out=sel[:], in0=sel[:], scalar1=-1.0, scalar2=BIG, op0=ALU.add, op1=ALU.mult)
            E("vector", "tensor_tensor", ["sel", "pastind"], ["sel"], out=sel[:], in0=sel[:], in1=pastind[:], op=ALU.mult)
            E("vector", "tensor_tensor", ["sel", "ownfut"], ["biasW"], out=biasW[:, :, 64:80], in0=sel[:].rearrange("p (q n) -> p q n", n=16),
              in1=ownfut[:].rearrange("p (q n) -> p q n", n=16), op=ALU.add)

        def prologue_B(h):
            hb = h % 2
            kq = "qaug%d" % hb
            for r4 in range(4):
                for q8 in range(8):
                    qt_ = r4 * 8 + q8
                    E("tensor", "transpose", ["biasW", "identb"], ["BT"], out=BT[0:80, q8 * 128:(q8 + 1) * 128], in_=biasW[:, qt_, :], identity=identb[:])
                E("vector", "tensor_copy", ["BT"], [kq], out=qaug[hb][64:80, r4 * 1024:(r4 + 1) * 1024], in_=BT[64:80, :])

        steps = [(h, g, kt) for h in range(NH) for g in range(8) for kt in range(4 * g + 4)]
        NPH = len(steps) // NH

        def emit_score(i):
            h, g, kt = steps[i]
            hb = h % 2
            kk = "kaug%d" % hb
            kq = "qaug%d" % hb
            sb_ = SB[i % 3]
            ks = "SB%d" % (i % 3)
            pt_ = PTb[i % 3]
            kpt = "PTb%d" % (i % 3)
            r = kt - 4 * g
            c0 = 128 * r if r > 0 else 0
            E("tensor", "matmul", [kk, kq], [ks], out=sb_[:, c0:512], lhsT=kaug[hb][0:80, kt * 128:(kt + 1) * 128],
              rhs=qaug[hb][0:80, g * 512 + c0:(g + 1) * 512], start=True, stop=True)
            E("scalar", "activation", [ks], [kpt], out=pt_[:, c0:512], in_=sb_[:, c0:512], func=AF.Exp)
            if r >= 0:
                E("gpsimd", "tensor_tensor", [kpt, "tri"], [kpt], out=pt_[:, r * 128:(r + 1) * 128], in0=pt_[:, r * 128:(r + 1) * 128],
                  in1=tri[:], op=ALU.mult)

        def emit_pv(i):
            h, g, kt = steps[i]
            gg = h * 8 + g
            ob = OB[gg % 2]
            ko = "OB%d" % (gg % 2)
            pt_ = PTb[i % 3]
            kpt = "PTb%d" % (i % 3)
            for qi in range(4):
                if kt <= 4 * g + qi:
                    E("tensor", "matmul", [kpt, "vaug"], [ko], out=ob[:, qi * 128:qi * 128 + 65], lhsT=pt_[:, qi * 128:(qi + 1) * 128],
                      rhs=vaug[:, kt, h * VW:h * VW + 65], start=(kt == 0 and qi == 0), stop=(kt == 4 * g + qi))
            if kt == 4 * g + 3:
                E("vector", "tensor_copy", [ko], ["osb"], out=osb[:], in_=ob[:])
                obv = osb[:].rearrange("p (q c) -> p q c", q=4)
                E("vector", "reciprocal", ["osb"], ["rl"], out=rl[:].unsqueeze(2), in_=obv[:, :, 64:65])
                E("vector", "tensor_tensor", ["osb", "rl"], ["ao_all"], out=ao_all[:, 4 * g:4 * g + 4, h * 64:(h + 1) * 64], in0=obv[:, :, 0:64],
                  in1=rl[:].unsqueeze(2).broadcast_to([128, 4, 64]), op=ALU.mult)

        prologue_A(0)
        prologue_B(0)
        for i in range(len(steps) + LA):
            if i < len(steps):
                emit_score(i)
            j = i - LA
            if j >= 0:
                emit_pv(j)
                h = steps[j][0]
                pos = j - h * NPH
                if h + 1 < NH:
                    if pos == NPH // 2:
                        prologue_A(h + 1)
                    if pos == NPH - 16:
                        prologue_B(h + 1)
        if debug:
            for v4 in range(4):
                DMA("sync", dbg["ao"][v4 * 1024:(v4 + 1) * 1024, :].rearrange("(t p) f -> p t f", p=128), ao_all[:, v4 * 8:(v4 + 1) * 8, :], ["ao_all"], [], None)
        S.barrier()
        A.reset(m3_mark)
        WUP_BYTES = 8 * 2 * DFF * 2
        WUP_OFF = (A.top - WUP_BYTES) // 64 * 64
        A.limit = WUP_OFF
        w_up = nc.alloc_sbuf_tensor_at("w_up_l%d" % l, [128, 8, 2 * DFF], BF16, offset=WUP_OFF)
        for kc in range(8):
            for hf in range(2):
                DMA("gpsimd", w_up[:, kc, hf * DFF:(hf + 1) * DFF], I["w_up"][l][kc * 128:(kc + 1) * 128, hf * DFF:(hf + 1) * DFF], [], ["w_up"], "wl%d" % ((2 * kc + hf) % 4))
        xt = [A([128, D], F32) for _ in range(3)]
        junk = A([128, D], BF16)
        junk2 = A([128, 512], BF16)
        ssa = [A([128, 1], F32) for _ in range(2)]
        rstda = [A([128, 1], F32) for _ in range(2)]
        ss = [A([128, 1], F32) for _ in range(2)]
        rstd = [A([128, 1], F32) for _ in range(2)]
        an = [A([128, 512], BF16) for _ in range(2)]
        mTa = [A([128, 4, 128], BF16) for _ in range(2)]
        pmt = [A([128, 4, 128], BF16) for _ in range(3)]
        tt = [A([128, D], F32) for _ in range(2)]
        YY = [[PB[0], PB[1]], [PB[2], PB[3]]]
        TA = PT[1]

        def m3_L(t):
            DMA("sync", xt[t % 3][:], x_src[t * 128:(t + 1) * 128, :], [], ["xt%d" % (t % 3)], "xl%d" % (t % 3))
            DMA("sync", pmt[t % 3][:].rearrange("p g t -> p (g t)"), pm_s[t], [], ["pmt%d" % (t % 3)], "pml%d" % (t % 3))

        def m3_A(t):
            b2 = t % 2
            E("scalar", "activation", ["ao_all"], ["junk2", "ssa%d" % b2], out=junk2[:], in_=ao_all[:, t, :], func=AF.Square, accum_out=ssa[b2][:])
            rstd_ops(ssa[b2][:], rstda[b2][:], 1, ["ssa%d" % b2], ["rstda%d" % b2], 512)
            E("vector", "tensor_scalar", ["ao_all", "rstda%d" % b2], ["an%d" % b2], out=an[b2][:], in0=ao_all[:, t, :], scalar1=rstda[b2][:, 0:1],
              scalar2=None, op0=ALU.mult)
            for c4 in range(4):
                E("tensor", "transpose", ["an%d" % b2, "identb"], ["TA"], out=TA[:, c4 * 128:(c4 + 1) * 128], in_=an[b2][:, c4 * 128:(c4 + 1) * 128],
                  identity=identb[:])
            for c4 in range(4):
                E("scalar", "activation", ["TA", "aog"], ["mTa%d" % b2], out=mTa[b2][:, c4, :], in_=TA[:, c4 * 128:(c4 + 1) * 128], func=AF.Identity,
                  scale=aog_col[:, c4:c4 + 1])

        def m3_B(t):
            b2 = t % 2
            xs = xt[t % 3]
            kx = "xt%d" % (t % 3)
            Y = YY[b2]
            kY = ["PB%d" % (2 * b2), "PB%d" % (2 * b2 + 1)]
            for nb in range(2):
                for c8 in range(8):
                    lhs = mTa[b2][:, c8, :] if c8 < 4 else pmt[t % 3][:, c8 - 4, :]
                    E("tensor", "matmul", ["mTa%d" % b2, "pmt%d" % (t % 3), "w_out"], [kY[nb]], out=Y[nb][:], lhsT=lhs,
                      rhs=w_out[:, c8, nb * 512:(nb + 1) * 512], start=(c8 == 0), stop=(c8 == 7))
            post_norm_residual(E, rstd_ops, Y, kY, junk, "junk", ss[b2], "ss%d" % b2, rstd[b2], "rstd%d" % b2, G2, "G2",
                               tt[b2], "tt%d" % b2, xs, kx, tt[b2], "tt%d" % b2)
            DMA("sync", x_mid[t * 128:(t + 1) * 128, :], tt[b2][:], ["tt%d" % b2], [], "xst%d" % b2)

        m3_L(0)
        m3_L(1)
        m3_A(0)
        for t in range(NT):
            if t + 2 < NT:
                m3_L(t + 2)
            if t + 1 < NT:
                m3_A(t + 1)
            m3_B(t)
        if stop_after == "M3":
            break

        S.barrier()
        A.reset(persist_mark)
        A.limit = WUP_OFF
        w_dn = A([128, NPAIR, D], BF16)
        G4 = A([128, D], F32)
        xt = [A([128, D], F32) for _ in range(2)]
        junk = A([128, D], BF16)
        ssa = [A([128, 1], F32) for _ in range(2)]
        rstda = [A([128, 1], F32) for _ in range(2)]
        ss = [A([128, 1], F32) for _ in range(2)]
        rstd = [A([128, 1], F32) for _ in range(2)]
        xn = [A([128, D], BF16) for _ in range(2)]
        hTg = A([128, 8, 512], BF16)
        mT = A([128, NPAIR, 512], BF16)
        acc = [[A([128, 512], F32) for _ in range(2)] for _ in range(2)]
        usb = [[A([128, 514], F32) for _ in range(2)] for _ in range(2)]
        hist = A([128, NFC, 2], F32)
        tt = [A([128, D], F32) for _ in range(2)]
        for c4 in range(0, NPAIR, 2):
            DMA("gpsimd", w_dn[:, c4:c4 + 2, :], I["w_down"][l][c4 * 128:(c4 + 2) * 128, :].rearrange("(c p) n -> p c n", p=128), [], ["w_dn"], "wl%d" % ((c4 // 2) % 4))
        DMA("sync", G4[:], g_s[1], [], ["G4"], None)
        E("gpsimd", "memset", [], ["hist"], ap=hist[:], constant=0.0)
        TF = PT[0]
        UB = [[PB[0], PB[1]], [PB[2], PB[3]]]
        Y = [PB[4], PB[5]]
        xcnt = [0]

        def xload(t):
            sl = xcnt[0] % 2
            xcnt[0] += 1
            DMA("sync", xt[sl][:], x_mid[t * 128:(t + 1) * 128, :], [], ["xt%d" % sl], "xl%d" % sl)
            return xt[sl], "xt%d" % sl

        def f_A(g, ti):
            t = g * 4 + ti
            b2 = t % 2
            xs, kx = xload(t)
            E("scalar", "activation", [kx], ["junk", "ssa%d" % b2], out=junk[:], in_=xs[:], func=AF.Square, accum_out=ssa[b2][:])
            rstd_ops(ssa[b2][:], rstda[b2][:], 1, ["ssa%d" % b2], ["rstda%d" % b2], D)
            E("vector", "tensor_scalar", [kx, "rstda%d" % b2], ["xn%d" % b2], out=xn[b2][:], in0=xs[:], scalar1=rstda[b2][:, 0:1],
              scalar2=None, op0=ALU.mult)
            for kc in range(8):
                E("tensor", "transpose", ["xn%d" % b2, "identb"], ["TF"], out=TF[:, kc * 128:(kc + 1) * 128],
                  in_=xn[b2][:, kc * 128:(kc + 1) * 128], identity=identb[:])
            for kc in range(8):
                E("scalar", "activation", ["TF", "cols2", "cols3"], ["hTg"], out=hTg[:, kc, ti * 128:(ti + 1) * 128],
                  in_=TF[:, kc * 128:(kc + 1) * 128], func=AF.Identity, scale=cols[:, 2, kc:kc + 1], bias=cols[:, 3, kc:kc + 1])

        pidx = 0
        pend = []

        def silu_flush():
            while pend:
                pb_, i_ = pend.pop(0)
                ka0 = "acc%d0" % pb_
                ka1 = "acc%d1" % pb_
                E("scalar", "activation", [ka0], [ka0], out=acc[pb_][0][:], in_=acc[pb_][0][:], func=AF.Silu)
                E("vector", "tensor_tensor", [ka0, ka1], ["mT"], out=mT[:, i_, :], in0=acc[pb_][0][:], in1=acc[pb_][1][:], op=ALU.mult)

        for ti in range(4):
            f_A(0, ti)
        for g in range(8):
            for i in range(NPAIR):
                pb = pidx % 2
                pidx += 1
                halves = ((0, i), (1, NPAIR + i))
                for half, ch in halves:
                    U = UB[pb][half]
                    kU = "PB%d" % (2 * pb + half)
                    for kc in range(8):
                        E("tensor", "matmul", ["hTg", "w_up"], [kU], out=U[:], lhsT=w_up[:, kc, ch * 128:(ch + 1) * 128], rhs=hTg[:, kc, :],
                          start=(kc == 0), stop=(kc == 7))
                for half, ch in halves:
                    U = UB[pb][half]
                    kU = "PB%d" % (2 * pb + half)
                    ub = usb[pb][half]
                    ku = "usb%d%d" % (pb, half)
                    E("gpsimd", "tensor_copy", ["hist%d" % ch], [ku + "h"], out=ub[:, 0:2], in_=hist[:, ch, :])
                    E("scalar", "activation", [kU], [ku], out=ub[:, 2:514], in_=U[:], func=AF.Copy)
                    E("scalar", "activation", [kU, "cv2", "cv3"], ["acc%d%d" % (pb, half)], out=acc[pb][half][:], in_=U[:], func=AF.Identity,
                      scale=cv_col[:, 2, ch:ch + 1], bias=cv_col[:, 3, ch:ch + 1])
                    if g < 7:
                        E("gpsimd", "tensor_copy", [ku], ["hist%d" % ch], out=hist[:, ch, :], in_=ub[:, 512:514])
                silu_flush()
                for half, ch in halves:
                    ub = usb[pb][half]
                    ku = "usb%d%d" % (pb, half)
                    ac = acc[pb][half]
                    ka = "acc%d%d" % (pb, half)
                    E("vector", "scalar_tensor_tensor", [ku, ku + "h", ka, "cv1"], [ka], out=ac[:], in0=ub[:, 1:513], scalar=cv_col[:, 1, ch:ch + 1],
                      in1=ac[:], op0=ALU.mult, op1=ALU.add)
                for half, ch in halves:
                    ub = usb[pb][half]
                    ku = "usb%d%d" % (pb, half)
                    ac = acc[pb][half]
                    ka = "acc%d%d" % (pb, half)
                    E("vector", "scalar_tensor_tensor", [ku, ku + "h", ka, "cv0"], [ka], out=ac[:], in0=ub[:, 0:512], scalar=cv_col[:, 0, ch:ch + 1],
                      in1=ac[:], op0=ALU.mult, op1=ALU.add)
                pend.append((pb, i))
            silu_flush()
            for ti in range(4):
                t = g * 4 + ti
                b2 = t % 2
                for nb in range(2):
                    for i in range(NPAIR):
                        E("tensor", "matmul", ["mT", "w_dn"], ["PB%d" % (4 + nb)], out=Y[nb][:], lhsT=mT[:, i, ti * 128:(ti + 1) * 128],
                          rhs=w_dn[:, i, nb * 512:(nb + 1) * 512], start=(i == 0), stop=(i == NPAIR - 1))
                if g + 1 < 8:
                    f_A(g + 1, ti)
                xs, kx = xload(t)
                post_norm_residual(E, rstd_ops, Y, ["PB4", "PB5"], junk, "junk", ss[b2], "ss%d" % b2, rstd[b2], "rstd%d" % b2, G4, "G4",
                                   tt[b2], "tt%d" % b2, xs, kx, tt[b2], "tt%d" % b2)
                DMA("sync", x_dst[t * 128:(t + 1) * 128, :], tt[b2][:], ["tt%d" % b2], [], "xst%d" % b2)
        x_src = x_dst

    final = [k for k in S.dma_counts.keys()]
    nsem = S.emit(final_wait_semkeys=final)
    return nc


def post_norm_residual(E, rstd_ops, Y, kY, junk, kjunk, ss, kss, rstd, krstd, G, kG, tt, ktt, xs, kx, xo, kxo):
    ssb = ss
    E("scalar", "activation", [kY[0]], [kjunk, kss, "lk" + kY[0]], out=junk[:, 0:512], in_=Y[0][:], func=AF.Square, accum_out=ssb[:])
    E("vector", "tensor_copy", [kY[0], "lk" + kY[0]], [ktt], out=tt[:, 0:512], in_=Y[0][:])
    E("scalar", "activation", [kY[1]], [kjunk, krstd, "lk" + kY[1]], out=junk[:, 512:1024], in_=Y[1][:], func=AF.Square, accum_out=rstd[:])
    E("vector", "tensor_copy", [kY[1], "lk" + kY[1]], [ktt], out=tt[:, 512:1024], in_=Y[1][:])
    E("vector", "tensor_tensor", [kss, krstd], [kss], out=ssb[:], in0=ssb[:], in1=rstd[:], op=ALU.add)
    rstd_ops(ssb[:], rstd[:], 1, [kss], [krstd], D)
    E("vector", "scalar_tensor_tensor", [ktt, krstd, kG], [ktt], out=tt[:], in0=tt[:], scalar=rstd[:, 0:1],
      in1=G[:], op0=ALU.mult, op1=ALU.mult)
    E("gpsimd", "tensor_tensor", [ktt, kx], [kxo], out=xo[:], in0=tt[:], in1=xs[:], op=ALU.add)


_CONSTS = None


def make_in_maps(inputs):
    global _CONSTS
    if _CONSTS is None:
        _CONSTS = _const_tables()
    x = np.asarray(inputs["x"], dtype=np.float32)
    c = np.asarray(inputs["c"], dtype=np.float32)
    shared = {k: np.ascontiguousarray(np.asarray(inputs[k], dtype=np.float32)) for k in WEIGHT_SPECS if k != "c"}
    maps = []
    for b in range(8):
        m = {"x": np.ascontiguousarray(x[b]), "c": np.ascontiguousarray(c[b:b + 1])}
        m.update(shared)
        m.update(_CONSTS)
        maps.append(m)
    return maps


def kernel(**inputs):
    nc = build()
    maps = make_in_maps(inputs)
    res = run_bass_kernel_spmd(nc, maps, core_ids=list(range(8)))
    return np.stack([np.asarray(r["out"]) for r in res.results], axis=0).astype(np.float32)
```

```python
import contextlib
import os
import numpy as np
import ml_dtypes
import concourse.bass as bass
import concourse.mybir as mybir
from concourse.bass_utils import run_bass_kernel_spmd

F32 = mybir.dt.float32
BF16 = mybir.dt.bfloat16
AF = mybir.ActivationFunctionType
ALU = mybir.AluOpType
AX = mybir.AxisListType

S_LEN = 4096
D = 1024
NT = S_LEN // 128
NH = 8
HD = 64
DFF = 2816
NFC = 2 * DFF // 128
NPAIR = DFF // 128
EPS = 1e-6
BIG = 30000.0
POOL_W = (2, 4, 8, 16)
VW = 68

ENGINES = ("tensor", "vector", "scalar", "gpsimd", "sync")
SEM_LIMIT = 30000


class Op:
    __slots__ = ("eng", "fn", "deps", "is_dma", "ticket", "signal", "idx")

    def __init__(self, eng, fn, is_dma):
        self.idx = 0
        self.eng = eng
        self.fn = fn
        self.is_dma = is_dma
        self.deps = []
        self.ticket = None
        self.signal = False


class Sched:
    def __init__(self, nc):
        self.nc = nc
        self.q = {e: [] for e in ENGINES}
        self.last_w = {}
        self.readers = {}
        self.dma_counts = {}
        self.dma_last = {}
        self.bar = {e: [] for e in ENGINES}

    def barrier(self):
        deps = []
        for e in ENGINES:
            for o in reversed(self.q[e]):
                if not o.is_dma:
                    deps.append(o)
                    break
        deps.extend(self.dma_last.values())
        for e in ENGINES:
            self.bar[e] = list(deps)
        self.last_w = {}
        self.readers = {}

    def op(self, eng, fn, reads=(), writes=(), dma=False, semkey=None):
        o = Op(eng, fn, dma)
        deps = set()
        for k in reads:
            w = self.last_w.get(k)
            if w is not None:
                deps.add(w)
        for k in writes:
            w = self.last_w.get(k)
            if w is not None:
                deps.add(w)
            for r in self.readers.get(k, ()):
                deps.add(r)
        for d in deps:
            if d is o:
                continue
            if (not d.is_dma) and (not dma) and d.eng == eng:
                if eng == "tensor":
                    continue
                raw = any(self.last_w.get(k) is d for k in reads)
                if not raw:
                    continue
            o.deps.append(d)
        if dma and semkey in self.dma_last:
            o.deps.append(self.dma_last[semkey])
        if self.bar[eng]:
            for d in self.bar[eng]:
                if d.is_dma or d.eng != eng:
                    o.deps.append(d)
            self.bar[eng] = []
        best = {}
        pruned = []
        for d in o.deps:
            if d.is_dma:
                pruned.append(d)
            else:
                b = best.get(d.eng)
                if b is None or d.idx > b.idx:
                    best[d.eng] = d
        o.deps = pruned + list(best.values())
        o.idx = len(self.q[eng])
        for k in writes:
            self.last_w[k] = o
            self.readers[k] = []
        for k in reads:
            self.readers.setdefault(k, []).append(o)
        if dma:
            c = self.dma_counts.get(semkey, 0) + 16
            self.dma_counts[semkey] = c
            o.ticket = (("dma", semkey), c)
            self.dma_last[semkey] = o
        self.q[eng].append(o)
        return o

    def emit(self, final_wait_semkeys=()):
        nc = self.nc
        for e in ENGINES:
            for o in self.q[e]:
                for d in o.deps:
                    if not d.is_dma:
                        d.signal = True
        semnames = set()
        for e in ENGINES:
            cnt = 0
            seg = 0
            for o in self.q[e]:
                if o.is_dma:
                    semnames.add(o.ticket[0])
                    continue
                if o.signal:
                    cnt += 1
                    if cnt > SEM_LIMIT:
                        seg += 1
                        cnt = 1
                    o.ticket = (("eng", e, seg), cnt)
                    semnames.add(o.ticket[0])
        semnames = sorted(semnames, key=str)
        with contextlib.ExitStack() as st:
            sems = {}
            for i, n in enumerate(semnames):
                sems[n] = st.enter_context(nc.semaphore("s%d" % i))
            block = st.enter_context(nc.Block())

            def run(engname):
                def body(eng):
                    waited = {}
                    for o in self.q[engname]:
                        need = {}
                        for d in o.deps:
                            s, v = d.ticket
                            if need.get(s, 0) < v:
                                need[s] = v
                        for s, v in need.items():
                            if waited.get(s, 0) < v:
                                eng.wait_ge(sems[s], v)
                                waited[s] = v
                        ins = o.fn(eng)
                        if o.is_dma:
                            ins.then_inc(sems[o.ticket[0]], 16)
                        elif o.signal:
                            ins.then_inc(sems[o.ticket[0]], 1)
                    if engname == "sync":
                        for k in final_wait_semkeys:
                            s = ("dma", k)
                            v = self.dma_counts[k]
                            if waited.get(s, 0) < v:
                                eng.wait_ge(sems[s], v)
                                waited[s] = v
                return body

            block.tensor(run("tensor"))
            block.vector(run("vector"))
            block.scalar(run("scalar"))
            block.gpsimd(run("gpsimd"))
            block.sync(run("sync"))
        return len(semnames)


def _const_tables():
    bf = ml_dtypes.bfloat16
    pos = np.arange(S_LEN, dtype=np.float32)
    inv_freq = np.power(np.float32(500000.0), -np.arange(0, 16, 2, dtype=np.float32) / np.float32(16))
    ang = (pos[:, None] * inv_freq[None, :]).astype(np.float32)
    cos = np.cos(ang).astype(np.float32)
    sin = np.sin(ang).astype(np.float32)
    def tm(a):
        return np.ascontiguousarray(a.reshape(NT, 128, -1).transpose(1, 0, 2))
    ropek = tm(np.concatenate([cos, sin], axis=1)).astype(np.float32)
    ropeq = (ropek * np.float32(0.125)).astype(np.float32)
    qt = np.arange(NT)
    j = qt // 2
    n = np.arange(16)
    past = (n[None, :] < j[:, None])
    pastmask = np.where(past, 0.0, -1e30).astype(np.float32).reshape(1, NT * 16)
    pastind = past.astype(np.float32).reshape(1, NT * 16)
    ownfut = np.where(n[None, :] > j[:, None], -BIG, 0.0).astype(np.float32).reshape(1, NT * 16)
    onehot = (np.arange(S_LEN)[None, :] // 256 == n[:, None]).astype(np.float32).astype(bf)
    k = np.arange(128)
    tri = (k[None, :] >= k[:, None]).astype(np.float32).astype(bf)
    identb = np.eye(128, dtype=np.float32).astype(bf)
    identf = np.eye(128, dtype=np.float32)
    onesb = np.ones((128, 128), dtype=np.float32).astype(bf)
    rc = np.zeros((1, 4, 16), dtype=np.float32)
    for g, w in enumerate(POOL_W):
        rc[0, g, :] = 1.0 / np.minimum(np.arange(16) + 1, w)
    return {
        "ropeq": ropeq, "ropek": ropek,
        "pastmask": np.ascontiguousarray(np.broadcast_to(pastmask, (128, NT * 16))),
        "pastind": np.ascontiguousarray(np.broadcast_to(pastind, (128, NT * 16))),
        "ownfut": np.ascontiguousarray(np.broadcast_to(ownfut, (128, NT * 16))),
        "onehot": onehot, "tri": tri, "identb": identb, "identf": identf, "onesb": onesb,
        "rc": np.ascontiguousarray(np.broadcast_to(rc, (128, 4, 16))),
    }


CONST_SPECS = {
    "ropeq": ([128, NT, 16], F32), "ropek": ([128, NT, 16], F32),
    "pastmask": ([128, NT * 16], F32), "pastind": ([128, NT * 16], F32), "ownfut": ([128, NT * 16], F32),
    "onehot": ([16, S_LEN], BF16), "tri": ([128, 128], BF16), "identb": ([128, 128], BF16),
    "identf": ([128, 128], F32), "onesb": ([128, 128], BF16), "rc": ([128, 4, 16], F32),
}

WEIGHT_SPECS = {
    "c": [1, D], "w_ada": [2, D, 6 * D], "b_ada": [2, 6 * D], "g_pre_mix": [2, D], "w_in": [2, D, 2048],
    "w_pool": [2, 4, 128, 128], "pool_scale": [2, 512], "attn_out_gain": [2, 512], "pool_out_gain": [2, 512],
    "w_out": [2, D, D], "g_post_mix": [2, D], "g_pre_ffn": [2, D], "w_up": [2, D, 2 * DFF],
    "conv_w": [2, 3, 2 * DFF], "conv_b": [2, 2 * DFF], "w_down": [2, DFF, D], "g_post_ffn": [2, D],
}


class Alloc:
    def __init__(self, nc):
        self.nc = nc
        self.base = (nc.sbuf_base + 63) // 64 * 64
        self.top = nc.sbuf_top
        self.cur = self.base
        self.limit = self.top
        self.n = 0

    def mark(self):
        return self.cur

    def reset(self, m):
        self.cur = m

    def __call__(self, shape, dt):
        sz = 1
        for s in shape[1:]:
            sz *= s
        nbytes = sz * (4 if dt == F32 else 2)
        nbytes = (nbytes + 63) // 64 * 64
        off = self.cur
        assert off + nbytes <= self.limit, ("SBUF overflow", off, nbytes, self.limit)
        self.cur += nbytes
        self.n += 1
        return self.nc.alloc_sbuf_tensor_at("sb%d" % self.n, list(shape), dt, offset=off)


def build(n_layers=2, stop_after=None, debug=False):
    nc = bass.Bass("TRN2", target_bir_lowering=False)
    I = {}
    I["x"] = nc.dram_tensor("x", [S_LEN, D], F32, kind="ExternalInput").ap()
    for k, shp in WEIGHT_SPECS.items():
        I[k] = nc.dram_tensor(k, shp, F32, kind="ExternalInput").ap()
    for k, (shp, dt) in CONST_SPECS.items():
        I[k] = nc.dram_tensor(k, shp, dt, kind="ExternalInput").ap()
    out = nc.dram_tensor("out", [S_LEN, D], F32, kind="ExternalOutput").ap()
    sk = "ExternalOutput" if debug else "Internal"
    qT_s = nc.dram_tensor("qT_s", [NH, HD, S_LEN], BF16, kind=sk).ap()
    kT_s = nc.dram_tensor("kT_s", [NH, HD, S_LEN], BF16, kind=sk).ap()
    v_s = nc.dram_tensor("v_s", [S_LEN, NH * VW], BF16, kind=sk).ap()
    pm_s = nc.dram_tensor("pm_s", [NT, 128, 512], BF16, kind=sk).ap()
    g_s = nc.dram_tensor("g_s", [2, 128, D], F32, kind=sk).ap()
    xa = nc.dram_tensor("xa", [S_LEN, D], F32, kind=sk).ap()
    xb = nc.dram_tensor("xb", [S_LEN, D], F32, kind=sk).ap()
    dbg = {}
    if debug:
        dbg["ao"] = nc.dram_tensor("dbg_ao", [S_LEN, 512], F32, kind="ExternalOutput").ap()

    S = Sched(nc)
    A = Alloc(nc)
    PB = [nc.alloc_psum_tensor("pb%d" % i, [128, 512], F32) for i in range(6)]
    PT = [nc.alloc_psum_tensor("pt%d" % i, [128, 1024], BF16) for i in range(2)]

    sec = [None]
    enabled = os.environ.get("KSEC")
    nt_run = int(os.environ.get("KNT", NT))

    def skip():
        return enabled is not None and sec[0] is not None and sec[0] not in enabled

    def E(eng, meth, reads, writes, **kw):
        if skip():
            return None
        return S.op(eng, lambda e: getattr(e, meth)(**kw), reads, writes)

    otc = [0]

    def OT():
        otc[0] += 1
        return "ot%d" % (otc[0] % 6)

    def DMA(eng, out_, in_, reads, writes, semkey, slow=False):
        if skip():
            return None
        if semkey is None:
            semkey = OT()
        if slow:
            return S.op(eng, lambda e: e.dma_start(out=out_, in_=in_, allow_slow_non_contiguous=True),
                        reads, writes, dma=True, semkey=semkey)
        return S.op(eng, lambda e: e.dma_start(out=out_, in_=in_), reads, writes, dma=True, semkey=semkey)

    identb = A([128, 128], BF16)
    identf = A([128, 128], F32)
    onesb = A([128, 128], BF16)
    tri = A([128, 128], BF16)
    for nm, t in (("identb", identb), ("identf", identf), ("onesb", onesb), ("tri", tri)):
        DMA("sync", t[:], I[nm], [], [nm], None)
    cols = A([128, 4, 8], F32)
    aog_col = A([128, 4], F32)
    ps_col = A([128, 4], F32)
    pg_col = A([128, 4], F32)
    cv_col = A([128, 4, NFC], F32)
    rstd_eps = EPS
    persist_mark = A.mark()

    def rstd_ops(ss, rstd, n, keys_r, keys_w, dim):
        E("scalar", "activation", keys_r, keys_w, out=rstd, in_=ss, func=AF.Ln, scale=1.0 / dim, bias=rstd_eps)
        E("scalar", "activation", keys_w, keys_w, out=rstd, in_=rstd, func=AF.Exp, scale=-0.5)

    x_src = I["x"]
    for l in range(n_layers):
        last_layer = (l == n_layers - 1)
        x_mid = xa
        x_dst = out if last_layer else xb
        S.barrier()
        A.reset(persist_mark)
        A.limit = A.top
        c_col = A([128, 8], F32)
        cact = A([128, 8], F32)
        cbc = A([128, 8, 128], F32)
        bada = A([128, 6 * D], F32)
        modbc = A([128, 6 * D], F32)
        wblk = [A([128, 8, 512], F32) for _ in range(2)]
        gbc = A([128, D], F32)
        gtmp = A([128, D], F32)
        dtmp = A([128, 8, 128], F32)
        stg = [A([64, 128], F32) for _ in range(2)]
        stc = [0]

        def load_col(vec1d, n, dst, wkey):
            i_ = stc[0] % 2
            stc[0] += 1
            sg = stg[i_]
            DMA("sync", sg[0:n, :], vec1d.rearrange("(c p) -> c p", p=128), [], ["stg%d" % i_], None)
            E("tensor", "matmul", ["stg%d" % i_, "identf"], ["PB2"], out=PB[2][:, 0:n], lhsT=sg[0:n, :], rhs=identf[0:n, 0:n],
              start=True, stop=True)
            E("vector", "tensor_copy", ["PB2"], [wkey], out=dst, in_=PB[2][:, 0:n])

        load_col(I["c"][0], 8, c_col[:], "c_col")
        DMA("sync", bada[:], I["b_ada"][l].partition_broadcast(128), [], ["bada"], None)
        load_col(I["attn_out_gain"][l], 4, aog_col[:], "aog")
        load_col(I["pool_scale"][l], 4, ps_col[:], "psc")
        load_col(I["pool_out_gain"][l], 4, pg_col[:], "pgc")
        for j3 in range(3):
            load_col(I["conv_w"][l, j3], NFC, cv_col[:, j3, :], "cv%d" % j3)
        load_col(I["conv_b"][l], NFC, cv_col[:, 3, :], "cv3")
        E("scalar", "activation", ["c_col"], ["cact"], out=cact[:], in_=c_col[:], func=AF.Silu)
        E("vector", "tensor_copy", ["cact"], ["cbc"], out=cbc[:], in_=cact[:].unsqueeze(2).broadcast_to([128, 8, 128]))
        for blk in range(12):
            wb = wblk[blk % 2]
            kb = "wblk%d" % (blk % 2)
            pm_ = PB[blk % 2]
            kp = "PB%d" % (blk % 2)
            DMA("sync", wb[:], I["w_ada"][l][:, blk * 512:(blk + 1) * 512].rearrange("(kc p) n -> p kc n", p=128),
                [], [kb], "wada%d" % (blk % 2))
            for kc in range(8):
                E("tensor", "matmul", [kb, "cbc"], [kp], out=pm_[:], lhsT=cbc[:, kc, :], rhs=wb[:, kc, :],
                  start=(kc == 0), stop=(kc == 7))
            E("vector", "tensor_tensor", [kp, "bada"], ["modbc"], out=modbc[:, blk * 512:(blk + 1) * 512], in0=pm_[:],
              in1=bada[:, blk * 512:(blk + 1) * 512], op=ALU.add)

        def diag_extract(src_ap, dst_ap, rkeys, wkey):
            E("vector", "tensor_tensor", rkeys + ["identf"], ["dtmp"], out=dtmp[:],
              in0=src_ap.rearrange("p (k q) -> p k q", k=8),
              in1=identf[:].unsqueeze(1).broadcast_to([128, 8, 128]), op=ALU.mult)
            E("vector", "tensor_reduce", ["dtmp"], [wkey], out=dst_ap, in_=dtmp[:], axis=AX.X, op=ALU.add)

        for gi, (gname, moff) in enumerate((("g_post_mix", 2 * D), ("g_post_ffn", 5 * D))):
            DMA("sync", gbc[:], I[gname][l].partition_broadcast(128), [], ["gbc"], None)
            E("vector", "tensor_tensor", ["gbc", "modbc"], ["gtmp"], out=gtmp[:], in0=modbc[:, moff:moff + D], in1=gbc[:], op=ALU.mult)
            DMA("sync", g_s[gi], gtmp[:], ["gtmp"], [], None)
        for ci, (gname, scoff, shoff) in enumerate((("g_pre_mix", 1 * D, 0), ("g_pre_ffn", 4 * D, 3 * D))):
            DMA("sync", gbc[:], I[gname][l].partition_broadcast(128), [], ["gbc"], None)
            E("vector", "scalar_tensor_tensor", ["gbc", "modbc"], ["gtmp"], out=gtmp[:], in0=modbc[:, scoff:scoff + D],
              scalar=1.0, in1=gbc[:], op0=ALU.add, op1=ALU.mult)
            diag_extract(gtmp[:], cols[:, 2 * ci, :], ["gtmp"], "cols%d" % (2 * ci))
            diag_extract(modbc[:, shoff:shoff + D], cols[:, 2 * ci + 1, :], ["modbc"], "cols%d" % (2 * ci + 1))
        if stop_after == "P":
            break

        S.barrier()
        A.reset(persist_mark)
        w_in = A([128, 8, 2048], BF16)
        w_pool = A([128, 4, 128], BF16)
        ropeq = A([128, NT, 16], F32)
        ropek = A([128, NT, 16], F32)
        rc = A([128, 4, 16], F32)
        xt = [A([128, D], F32) for _ in range(3)]
        junk = A([128, D], BF16)
        ss = [A([128, 1], F32) for _ in range(3)]
        rstd = [A([128, 1], F32) for _ in range(3)]
        xn = [A([128, D], BF16) for _ in range(3)]
        hT = [A([128, 8, 128], BF16) for _ in range(2)]
        q_tm = [A([128, 512], BF16) for _ in range(2)]
        k_tm = [A([128, 512], BF16) for _ in range(2)]
        rt = [A([128, 8, 8], F32) for _ in range(4)]
        zq = A([128, 8, 16], F32)
        qst = [A([64, 2, 4, 512], BF16) for _ in range(2)]
        kst = [A([64, 2, 4, 512], BF16) for _ in range(2)]
        vst = [A([128, 8, VW], BF16) for _ in range(2)]
        pmst = [A([128, 4, 128], BF16) for _ in range(2)]
        pz = A([128, 4, 144], F32)
        s2 = A([128, 4, 144], F32)
        s4 = A([128, 4, 144], F32)
        s8 = A([128, 4, 144], F32)
        s16 = A([128, 4, 144], F32)
        pooledT = [A([128, 4, 128], BF16) for _ in range(2)]
        po_sb = A([128, 4, 128], F32)
        sq = A([128, 4, 128], BF16)
        prs = A([128, 128], F32)
        ptmp = A([128, 4, 128], F32)
        etmp = A([128, 4, 16], F32)

        sec[0] = "L"
        for kc in range(8):
            DMA("gpsimd", w_in[:, kc, :], I["w_in"][l][kc * 128:(kc + 1) * 128, :], [], ["w_in"], "wl%d" % (kc % 4))
        DMA("gpsimd", w_pool[:], I["w_pool"][l].rearrange("g c e -> c g e"), [], ["w_pool"], "wl0")
        DMA("sync", ropeq[:], I["ropeq"], [], ["ropeq"], None)
        DMA("sync", ropek[:], I["ropek"], [], ["ropek"], None)
        DMA("sync", rc[:], I["rc"], [], ["rc"], None)
        for b_ in range(2):
            E("gpsimd", "memset", [], ["vst%d" % b_], ap=vst[b_][:], constant=1.0)
        E("gpsimd", "memset", [], ["pz"], ap=pz[:], constant=0.0)
        Pq, Pk, Pv, Pp, Py, Ps = PB
        T0, T1 = PT[0], PT[1]
        def m1_L(t):
            DMA("sync", xt[t % 3][:], x_src[t * 128:(t + 1) * 128, :], [], ["xt%d" % (t % 3)], "xl%d" % (t % 3))

        def m1_N(t):
            b3 = t % 3
            xs = xt[b3]
            kx = "xt%d" % b3
            E("scalar", "activation", [kx], ["junk", "ss%d" % b3], out=junk[:], in_=xs[:], func=AF.Square, accum_out=ss[b3][:])
            rstd_ops(ss[b3][:], rstd[b3][:], 1, ["ss%d" % b3], ["rstd%d" % b3], D)
            E("vector", "tensor_scalar", [kx, "rstd%d" % b3], ["xn%d" % b3], out=xn[b3][:], in0=xs[:], scalar1=rstd[b3][:, 0:1],
              scalar2=None, op0=ALU.mult)

        def m1_T(t):
            b3 = t % 3
            b2 = t % 2
            for kc in range(8):
                E("tensor", "transpose", ["xn%d" % b3, "identb"], ["T0"], out=T0[:, kc * 128:(kc + 1) * 128],
                  in_=xn[b3][:, kc * 128:(kc + 1) * 128], identity=identb[:])
            for kc in range(8):
                E("scalar", "activation", ["T0", "cols0", "cols1"], ["hT%d" % b2], out=hT[b2][:, kc, :], in_=T0[:, kc * 128:(kc + 1) * 128],
                  func=AF.Identity, scale=cols[:, 0, kc:kc + 1], bias=cols[:, 1, kc:kc + 1])

        def m1_B1(t):
            b2 = t % 2
            for P_, kP, c0 in ((Pq, "Pq", 0), (Pk, "Pk", 512), (Pv, "Pv", 1024)):
                for kc in range(8):
                    E("tensor", "matmul", ["hT%d" % b2, "w_in"], [kP], out=P_[:], lhsT=hT[b2][:, kc, :], rhs=w_in[:, kc, c0:c0 + 512],
                      start=(kc == 0), stop=(kc == 7))
            for g in range(4):
                for kc in range(8):
                    E("tensor", "matmul", ["hT%d" % b2, "w_in"], ["Pp"], out=Pp[:, g * 128:(g + 1) * 128],
                      lhsT=w_in[:, kc, 1536 + g * 128:1536 + (g + 1) * 128], rhs=hT[b2][:, kc, :], start=(kc == 0), stop=(kc == 7))
            for P_, kP, tm_, ktm, rope, krope, sc_ in ((Pq, "Pq", q_tm[b2], "q_tm%d" % b2, ropeq, "ropeq", 0.125),
                                                       (Pk, "Pk", k_tm[b2], "k_tm%d" % b2, ropek, "ropek", 1.0)):
                E("scalar", "activation", [kP], [ktm], out=tm_[:], in_=P_[:], func=AF.Identity, scale=sc_)
                pv = P_[:].rearrange("p (h d) -> p h d", h=8)
                ov = tm_[:].rearrange("p (h d) -> p h d", h=8)
                E("scalar", "activation", [kP], ["zq"], out=zq[:], in_=pv[:, :, 0:16], func=AF.Copy)
                cosb = rope[:, t, 0:8].unsqueeze(1).broadcast_to([128, 8, 8])
                sinb = rope[:, t, 8:16].unsqueeze(1).broadcast_to([128, 8, 8])
                x1 = zq[:, :, 0:8]
                x2 = zq[:, :, 8:16]
                E("vector", "tensor_tensor", ["zq", krope], ["rt0"], out=rt[0][:], in0=x1, in1=cosb, op=ALU.mult)
                E("vector", "tensor_tensor", ["zq", krope], ["rt1"], out=rt[1][:], in0=x2, in1=sinb, op=ALU.mult)
                E("vector", "tensor_tensor", ["zq", krope], ["rt2"], out=rt[2][:], in0=x2, in1=cosb, op=ALU.mult)
                E("vector", "tensor_tensor", ["zq", krope], ["rt3"], out=rt[3][:], in0=x1, in1=sinb, op=ALU.mult)
                E("vector", "tensor_tensor", ["rt0", "rt1"], [ktm], out=ov[:, :, 0:8], in0=rt[0][:], in1=rt[1][:], op=ALU.subtract)
                E("vector", "tensor_tensor", ["rt2", "rt3"], [ktm], out=ov[:, :, 8:16], in0=rt[2][:], in1=rt[3][:], op=ALU.add)
            vb = t % 2
            E("scalar", "activation", ["Pv"], ["vst%d" % vb], out=vst[vb][:, :, 0:64], in_=Pv[:].rearrange("p (h d) -> p h d", h=8),
              func=AF.Copy)
            DMA("sync", v_s[t * 128:(t + 1) * 128, :], vst[vb][:].rearrange("p h d -> p (h d)"), ["vst%d" % vb], [], "vs%d" % vb)
            pT = pooledT[b2]
            kpT = "pooledT%d" % b2
            E("scalar", "activation", ["Pp"], ["pz"], out=pz[:, :, 16:144], in_=Pp[:].rearrange("p (g t) -> p g t", g=4), func=AF.Copy)
            E("gpsimd", "tensor_tensor", ["pz"], ["s2"], out=s2[:, :, 1:144], in0=pz[:, :, 1:144], in1=pz[:, :, 0:143], op=ALU.add)
            E("gpsimd", "tensor_tensor", ["s2"], ["s4"], out=s4[:, 1:4, 3:144], in0=s2[:, 1:4, 3:144], in1=s2[:, 1:4, 1:142], op=ALU.add)
            E("gpsimd", "tensor_tensor", ["s4"], ["s8"], out=s8[:, 2:4, 7:144], in0=s4[:, 2:4, 7:144], in1=s4[:, 2:4, 3:140], op=ALU.add)
            E("gpsimd", "tensor_tensor", ["s8"], ["s16"], out=s16[:, 3:4, 15:144], in0=s8[:, 3:4, 15:144], in1=s8[:, 3:4, 7:136], op=ALU.add)
            sums = (s2, s4, s8, s16)
            for g in range(4):
                E("vector", "scalar_tensor_tensor", ["s%d" % (2 << g), "pz"], [kpT], out=pT[:, g, :], in0=sums[g][:, g, 16:144],
                  scalar=1.0 / POOL_W[g], in1=pz[:, g, 16:144], op0=ALU.mult, op1=ALU.subtract)
            if t == 0:
                for g in range(4):
                    E("gpsimd", "tensor_tensor", ["s%d" % (2 << g), "rc"], ["etmp"], out=etmp[:, g, :], in0=sums[g][:, g, 16:32], in1=rc[:, g, :], op=ALU.mult)
                    E("gpsimd", "tensor_tensor", ["etmp", "pz"], [kpT], out=pT[:, g, 0:16], in0=etmp[:, g, :], in1=pz[:, g, 16:32], op=ALU.subtract)
            E("gpsimd", "tensor_copy", ["pz", "s2", "s4", "s8", "s16", kpT], ["pz"], out=pz[:, :, 0:16], in_=pz[:, :, 128:144])

        def m1_B2(t):
            b2 = t % 2
            g4 = t // 4
            ti = t % 4
            sb_ = g4 % 2
            pT = pooledT[b2]
            kpT = "pooledT%d" % b2
            for g in range(4):
                E("tensor", "matmul", [kpT, "w_pool"], ["Py"], out=Py[:, g * 128:(g + 1) * 128], lhsT=w_pool[:, g, :], rhs=pT[:, g, :],
                  start=True, stop=True)
            for hi, (tm_, ktm, st_, kst_) in enumerate(((q_tm[b2], "q_tm%d" % b2, qst[sb_], "qst%d" % sb_), (k_tm[b2], "k_tm%d" % b2, kst[sb_], "kst%d" % sb_))):
                kT1 = "T1"
                for pr in range(4):
                    E("tensor", "transpose", [ktm, "identb"], [kT1], out=T1[:, hi * 512 + pr * 128:hi * 512 + (pr + 1) * 128],
                      in_=tm_[:, pr * 128:(pr + 1) * 128], identity=identb[:])
                for pa in range(2):
                    E("vector", "tensor_copy", [kT1], [kst_], out=st_[:, pa, :, ti * 128:(ti + 1) * 128],
                      in_=T1[pa * 64:(pa + 1) * 64, hi * 512:(hi + 1) * 512].rearrange("p (r t) -> p r t", r=4))
            if ti == 3:
                for pa in range(2):
                    DMA("sync", qT_s.rearrange("(pr pa) d t -> pa d pr t", pa=2)[pa][:, :, g4 * 512:(g4 + 1) * 512], qst[sb_][:, pa, :, :],
                        ["qst%d" % sb_], [], "qs%d%d" % (sb_, pa))
                    DMA("sync", kT_s.rearrange("(pr pa) d t -> pa d pr t", pa=2)[pa][:, :, g4 * 512:(g4 + 1) * 512], kst[sb_][:, pa, :, :],
                        ["kst%d" % sb_], [], "ks%d%d" % (sb_, pa))
            E("vector", "tensor_tensor", ["Py", "psc"], ["po_sb"], out=po_sb[:], in0=Py[:].rearrange("p (g t) -> p g t", g=4),
              in1=ps_col[:].unsqueeze(2).broadcast_to([128, 4, 128]), op=ALU.mult)
            E("scalar", "activation", ["po_sb"], ["sq"], out=sq[:], in_=po_sb[:], func=AF.Square)
            for g in range(4):
                E("tensor", "matmul", ["sq", "onesb"], ["Ps"], out=Ps[:, 0:128], lhsT=onesb[:], rhs=sq[:, g, :], start=(g == 0), stop=(g == 3))
            rstd_ops(Ps[:, 0:128], prs[:], 128, ["Ps"], ["prs"], 512)
            E("vector", "tensor_tensor", ["po_sb", "pgc"], ["ptmp"], out=ptmp[:], in0=po_sb[:],
              in1=pg_col[:].unsqueeze(2).broadcast_to([128, 4, 128]), op=ALU.mult)
            pb_ = t % 2
            E("vector", "tensor_tensor", ["ptmp", "prs"], ["pmst%d" % pb_], out=pmst[pb_][:], in0=ptmp[:],
              in1=prs[:].unsqueeze(1).broadcast_to([128, 4, 128]), op=ALU.mult)
            DMA("sync", pm_s[t], pmst[pb_][:].rearrange("p g t -> p (g t)"), ["pmst%d" % pb_], [], "pms%d" % pb_)

        sec[0] = None
        for t0_ in range(min(3, NT)):
            m1_L(t0_)
        m1_N(0)
        m1_N(1)
        m1_T(0)
        for t in range(NT):
            if t + 3 < NT:
                m1_L(t + 3)
            if t + 2 < NT:
                m1_N(t + 2)
            if t + 1 < NT:
                m1_T(t + 1)
            m1_B1(t)
            if t >= 1:
                m1_B2(t - 1)
        m1_B2(NT - 1)
        sec[0] = None
        if stop_after == "M1":
            break

        S.barrier()
        A.reset(persist_mark)
        ao_all = A([128, NT, 512], F32)
        w_out = A([128, 8, D], BF16)
        G2 = A([128, D], F32)
        m3_mark = A.mark()
        vaug = A([128, NT, NH * VW], BF16)
        kaug = [A([128, S_LEN], BF16) for _ in range(2)]
        qaug = [A([128, S_LEN], BF16) for _ in range(2)]
        kmT = A([64, 16], F32)
        kmTb = A([64, 16], BF16)
        pastmask = A([128, NT * 16], F32)
        pastind = A([128, NT * 16], F32)
        ownfut = A([128, NT * 16], F32)
        Gm = A([128, NT * 16], F32)
        m8 = A([128, NT, 8], F32)
        sel = A([128, NT * 16], F32)
        biasW = A([128, NT, 80], BF16)
        PTb = [A([128, 512], BF16) for _ in range(3)]
        rl = A([128, 4], F32)
        osb = A([128, 512], F32)
        for v4 in range(4):
            DMA("sync", vaug[:, v4 * 8:(v4 + 1) * 8, :], v_s[v4 * 1024:(v4 + 1) * 1024, :].rearrange("(t p) f -> p t f", p=128), [], ["vaug"], None)
        DMA("sync", pastmask[:], I["pastmask"], [], ["pastmask"], None)
        DMA("sync", pastind[:], I["pastind"], [], ["pastind"], None)
        DMA("sync", ownfut[:], I["ownfut"], [], ["ownfut"], None)
        for b_ in range(2):
            DMA("sync", kaug[b_][64:80, :], I["onehot"], [], ["kaug%d" % b_], None)
        E("gpsimd", "memset", [], ["biasW"], ap=biasW[:], constant=0.0)
        for kc in range(8):
            DMA("gpsimd", w_out[:, kc, :], I["w_out"][l][kc * 128:(kc + 1) * 128, :], [], ["w_out"], "wl%d" % (kc % 4))
        DMA("sync", G2[:], g_s[0], [], ["G2"], None)
        SB = PB[0:3]
        OB = PB[3:5]
        GP = PB[5]
        BT = PT[0]
        LA = 2

        def prologue_A(h):
            hb = h % 2
            kk = "kaug%d" % hb
            kq = "qaug%d" % hb
            DMA("sync", kaug[hb][0:64, :], kT_s[h], [], [kk], "kl%d" % hb)
            DMA("sync", qaug[hb][0:64, :], qT_s[h], [], [kq], "ql%d" % hb)
            E("vector", "tensor_reduce", [kk], ["kmT"], out=kmT[:], in_=kaug[hb][0:64, :].rearrange("p (n k) -> p n k", n=16), axis=AX.X, op=ALU.add)
            E("vector", "tensor_scalar", ["kmT"], ["kmTb"], out=kmTb[:], in0=kmT[:], scalar1=1.0 / 256, scalar2=None, op0=ALU.mult)
            for qt_ in range(NT):
                E("tensor", "matmul", [kq, "kmTb"], ["GP"], out=GP[:, qt_ * 16:(qt_ + 1) * 16], lhsT=qaug[hb][0:64, qt_ * 128:(qt_ + 1) * 128],
                  rhs=kmTb[:], start=True, stop=True)
            E("vector", "tensor_tensor", ["GP", "pastmask"], ["Gm"], out=Gm[:], in0=GP[:], in1=pastmask[:], op=ALU.add)
            for qt_ in range(NT):
                E("vector", "max", ["Gm"], ["m8"], out=m8[:, qt_, :], in_=Gm[:, qt_ * 16:(qt_ + 1) * 16])
            E("vector", "tensor_tensor", ["Gm", "m8"], ["sel"], out=sel[:].rearrange("p (q n) -> p q n", n=16),
              in0=Gm[:].rearrange("p (q n) -> p q n", n=16), in1=m8[:, :, 2:3].broadcast_to([128, NT, 16]), op=ALU.is_ge)
            E("vector", "tensor_scalar", ["sel"], ["sel"], out=sel[:], in0=sel[:], scalar1=-1.0, scalar2=BIG, op0=ALU.add, op1=ALU.mult)
            E("vector", "tensor_tensor", ["sel", "pastind"], ["sel"], out=sel[:], in0=sel[:], in1=pastind[:], op=ALU.mult)
            E("vector", "tensor_tensor", ["sel", "ownfut"], ["biasW"], out=biasW[:, :, 64:80], in0=sel[:].rearrange("p (q n) -> p q n", n=16),
              in1=ownfut[:].rearrange("p (q n) -> p q n", n=16), op=ALU.add)

        def prologue_B(h):
            hb = h % 2
            kq = "qaug%d" % hb
            for r4 in range(4):
                for q8 in range(8):
                    qt_ = r4 * 8 + q8
                    E("tensor", "transpose", ["biasW", "identb"], ["BT"], out=BT[0:80, q8 * 128:(q8 + 1) * 128], in_=biasW[:, qt_, :], identity=identb[:])
                E("vector", "tensor_copy", ["BT"], [kq], out=qaug[hb][64:80, r4 * 1024:(r4 + 1) * 1024], in_=BT[64:80, :])

        steps = [(h, g, kt) for h in range(NH) for g in range(8) for kt in range(4 * g + 4)]
        NPH = len(steps) // NH

        def emit_score(i):
            h, g, kt = steps[i]
            hb = h % 2
            kk = "kaug%d" % hb
            kq = "qaug%d" % hb
            sb_ = SB[i % 3]
            ks = "SB%d" % (i % 3)
            pt_ = PTb[i % 3]
            kpt = "PTb%d" % (i % 3)
            r = kt - 4 * g
            c0 = 128 * r if r > 0 else 0
            E("tensor", "matmul", [kk, kq], [ks], out=sb_[:, c0:512], lhsT=kaug[hb][0:80, kt * 128:(kt + 1) * 128],
              rhs=qaug[hb][0:80, g * 512 + c0:(g + 1) * 512], start=True, stop=True)
            E("scalar", "activation", [ks], [kpt], out=pt_[:, c0:512], in_=sb_[:, c0:512], func=AF.Exp)
            if r >= 0:
                E("gpsimd", "tensor_tensor", [kpt, "tri"], [kpt], out=pt_[:, r * 128:(r + 1) * 128], in0=pt_[:, r * 128:(r + 1) * 128],
                  in1=tri[:], op=ALU.mult)

        def emit_pv(i):
            h, g, kt = steps[i]
            gg = h * 8 + g
            ob = OB[gg % 2]
            ko = "OB%d" % (gg % 2)
            pt_ = PTb[i % 3]
            kpt = "PTb%d" % (i % 3)
            for qi in range(4):
                if kt <= 4 * g + qi:
                    E("tensor", "matmul", [kpt, "vaug"], [ko], out=ob[:, qi * 128:qi * 128 + 65], lhsT=pt_[:, qi * 128:(qi + 1) * 128],
                      rhs=vaug[:, kt, h * VW:h * VW + 65], start=(kt == 0 and qi == 0), stop=(kt == 4 * g + qi))
            if kt == 4 * g + 3:
                E("vector", "tensor_copy", [ko], ["osb"], out=osb[:], in_=ob[:])
                obv = osb[:].rearrange("p (q c) -> p q c", q=4)
                E("vector", "reciprocal", ["osb"], ["rl"], out=rl[:].unsqueeze(2), in_=obv[:, :, 64:65])
                E("vector", "tensor_tensor", ["osb", "rl"], ["ao_all"], out=ao_all[:, 4 * g:4 * g + 4, h * 64:(h + 1) * 64], in0=obv[:, :, 0:64],
                  in1=rl[:].unsqueeze(2).broadcast_to([128, 4, 64]), op=ALU.mult)

        prologue_A(0)
        prologue_B(0)
        for i in range(len(steps) + LA):
            if i < len(steps):
                emit_score(i)
            j = i - LA
            if j >= 0:
                emit_pv(j)
                h = steps[j][0]
                pos = j - h * NPH
                if h + 1 < NH:
                    if pos == NPH // 2:
                        prologue_A(h + 1)
                    if pos == NPH - 16:
                        prologue_B(h + 1)
        if debug:
            for v4 in range(4):
                DMA("sync", dbg["ao"][v4 * 1024:(v4 + 1) * 1024, :].rearrange("(t p) f -> p t f", p=128), ao_all[:, v4 * 8:(v4 + 1) * 8, :], ["ao_all"], [], None)
        S.barrier()
        A.reset(m3_mark)
        WUP_BYTES = 8 * 2 * DFF * 2
        WUP_OFF = (A.top - WUP_BYTES) // 64 * 64
        A.limit = WUP_OFF
        w_up = nc.alloc_sbuf_tensor_at("w_up_l%d" % l, [128, 8, 2 * DFF], BF16, offset=WUP_OFF)
        def wup_prefetch(j):
            kc, hf = j // 2, j % 2
            DMA("gpsimd", w_up[:, kc, hf * DFF:(hf + 1) * DFF], I["w_up"][l][kc * 128:(kc + 1) * 128, hf * DFF:(hf + 1) * DFF], [], ["w_up"], "wl%d" % (j % 4))
        xt = [A([128, D], F32) for _ in range(3)]
        junk = A([128, D], BF16)
        junk2 = A([128, 512], BF16)
        ssa = [A([128, 1], F32) for _ in range(2)]
        rstda = [A([128, 1], F32) for _ in range(2)]
        ss = [A([128, 1], F32) for _ in range(2)]
        rstd = [A([128, 1], F32) for _ in range(2)]
        an = [A([128, 512], BF16) for _ in range(2)]
        mTa = [A([128, 4, 128], BF16) for _ in range(2)]
        pmt = [A([128, 4, 128], BF16) for _ in range(3)]
        tt = [A([128, D], F32) for _ in range(2)]
        YY = [[PB[0], PB[1]], [PB[2], PB[3]]]
        TA = PT[1]

        def m3_L(t):
            DMA("sync", xt[t % 3][:], x_src[t * 128:(t + 1) * 128, :], [], ["xt%d" % (t % 3)], "xl%d" % (t % 3))
            DMA("sync", pmt[t % 3][:].rearrange("p g t -> p (g t)"), pm_s[t], [], ["pmt%d" % (t % 3)], "pml%d" % (t % 3))

        def m3_A(t):
            b2 = t % 2
            E("scalar", "activation", ["ao_all"], ["junk2", "ssa%d" % b2], out=junk2[:], in_=ao_all[:, t, :], func=AF.Square, accum_out=ssa[b2][:])
            rstd_ops(ssa[b2][:], rstda[b2][:], 1, ["ssa%d" % b2], ["rstda%d" % b2], 512)
            E("vector", "tensor_scalar", ["ao_all", "rstda%d" % b2], ["an%d" % b2], out=an[b2][:], in0=ao_all[:, t, :], scalar1=rstda[b2][:, 0:1],
              scalar2=None, op0=ALU.mult)
            for c4 in range(4):
                E("tensor", "transpose", ["an%d" % b2, "identb"], ["TA"], out=TA[:, c4 * 128:(c4 + 1) * 128], in_=an[b2][:, c4 * 128:(c4 + 1) * 128],
                  identity=identb[:])
            for c4 in range(4):
                E("scalar", "activation", ["TA", "aog"], ["mTa%d" % b2], out=mTa[b2][:, c4, :], in_=TA[:, c4 * 128:(c4 + 1) * 128], func=AF.Identity,
                  scale=aog_col[:, c4:c4 + 1])

        def m3_B(t):
            b2 = t % 2
            xs = xt[t % 3]
            kx = "xt%d" % (t % 3)
            Y = YY[b2]
            kY = ["PB%d" % (2 * b2), "PB%d" % (2 * b2 + 1)]
            for nb in range(2):
                for c8 in range(8):
                    lhs = mTa[b2][:, c8, :] if c8 < 4 else pmt[t % 3][:, c8 - 4, :]
                    E("tensor", "matmul", ["mTa%d" % b2, "pmt%d" % (t % 3), "w_out"], [kY[nb]], out=Y[nb][:], lhsT=lhs,
                      rhs=w_out[:, c8, nb * 512:(nb + 1) * 512], start=(c8 == 0), stop=(c8 == 7))
            post_norm_residual(E, rstd_ops, Y, kY, junk, "junk", ss[b2], "ss%d" % b2, rstd[b2], "rstd%d" % b2, G2, "G2",
                               tt[b2], "tt%d" % b2, xs, kx, tt[b2], "tt%d" % b2)
            DMA("sync", x_mid[t * 128:(t + 1) * 128, :], tt[b2][:], ["tt%d" % b2], [], "xst%d" % b2)

        m3_L(0)
        m3_L(1)
        m3_A(0)
        for t in range(NT):
            if t + 2 < NT:
                m3_L(t + 2)
            if t + 1 < NT:
                m3_A(t + 1)
            if t % 2 == 0:
                wup_prefetch(t // 2)
            m3_B(t)
        if stop_after == "M3":
            break

        S.barrier()
        A.reset(persist_mark)
        A.limit = WUP_OFF
        w_dn = A([128, NPAIR, D], BF16)
        G4 = A([128, D], F32)
        xt = [A([128, D], F32) for _ in range(2)]
        junk = A([128, D], BF16)
        ssa = [A([128, 1], F32) for _ in range(2)]
        rstda = [A([128, 1], F32) for _ in range(2)]
        ss = [A([128, 1], F32) for _ in range(2)]
        rstd = [A([128, 1], F32) for _ in range(2)]
        xn = [A([128, D], BF16) for _ in range(2)]
        hTg = A([128, 8, 512], BF16)
        mT = A([128, NPAIR, 512], BF16)
        acc = [[A([128, 512], F32) for _ in range(2)] for _ in range(2)]
        usb = [[A([128, 514], F32) for _ in range(2)] for _ in range(2)]
        hist = A([128, NFC, 2], F32)
        tt = [A([128, D], F32) for _ in range(2)]
        for c4 in range(0, NPAIR, 2):
            DMA("gpsimd", w_dn[:, c4:c4 + 2, :], I["w_down"][l][c4 * 128:(c4 + 2) * 128, :].rearrange("(c p) n -> p c n", p=128), [], ["w_dn"], "wl%d" % ((c4 // 2) % 4))
        DMA("sync", G4[:], g_s[1], [], ["G4"], None)
        E("gpsimd", "memset", [], ["hist"], ap=hist[:], constant=0.0)
        TF = PT[0]
        UB = [[PB[0], PB[1]], [PB[2], PB[3]]]
        Y = [PB[4], PB[5]]
        xcnt = [0]

        def xload(t):
            sl = xcnt[0] % 2
            xcnt[0] += 1
            DMA("sync", xt[sl][:], x_mid[t * 128:(t + 1) * 128, :], [], ["xt%d" % sl], "xl%d" % sl)
            return xt[sl], "xt%d" % sl

        def f_A(g, ti):
            t = g * 4 + ti
            b2 = t % 2
            xs, kx = xload(t)
            E("scalar", "activation", [kx], ["junk", "ssa%d" % b2], out=junk[:], in_=xs[:], func=AF.Square, accum_out=ssa[b2][:])
            rstd_ops(ssa[b2][:], rstda[b2][:], 1, ["ssa%d" % b2], ["rstda%d" % b2], D)
            E("vector", "tensor_scalar", [kx, "rstda%d" % b2], ["xn%d" % b2], out=xn[b2][:], in0=xs[:], scalar1=rstda[b2][:, 0:1],
              scalar2=None, op0=ALU.mult)
            for kc in range(8):
                E("tensor", "transpose", ["xn%d" % b2, "identb"], ["TF"], out=TF[:, kc * 128:(kc + 1) * 128],
                  in_=xn[b2][:, kc * 128:(kc + 1) * 128], identity=identb[:])
            for kc in range(8):
                E("scalar", "activation", ["TF", "cols2", "cols3"], ["hTg"], out=hTg[:, kc, ti * 128:(ti + 1) * 128],
                  in_=TF[:, kc * 128:(kc + 1) * 128], func=AF.Identity, scale=cols[:, 2, kc:kc + 1], bias=cols[:, 3, kc:kc + 1])

        pidx = 0
        pend = []

        def silu_flush():
            while pend:
                pb_, i_ = pend.pop(0)
                ka0 = "acc%d0" % pb_
                ka1 = "acc%d1" % pb_
                E("scalar", "activation", [ka0], [ka0], out=acc[pb_][0][:], in_=acc[pb_][0][:], func=AF.Silu)
                E("vector", "tensor_tensor", [ka0, ka1], ["mT"], out=mT[:, i_, :], in0=acc[pb_][0][:], in1=acc[pb_][1][:], op=ALU.mult)

        for ti in range(4):
            f_A(0, ti)
        for g in range(8):
            for i in range(NPAIR):
                pb = pidx % 2
                pidx += 1
                halves = ((0, i), (1, NPAIR + i))
                for half, ch in halves:
                    U = UB[pb][half]
                    kU = "PB%d" % (2 * pb + half)
                    for kc in range(8):
                        E("tensor", "matmul", ["hTg", "w_up"], [kU], out=U[:], lhsT=w_up[:, kc, ch * 128:(ch + 1) * 128], rhs=hTg[:, kc, :],
                          start=(kc == 0), stop=(kc == 7))
                for half, ch in halves:
                    U = UB[pb][half]
                    kU = "PB%d" % (2 * pb + half)
                    ub = usb[pb][half]
                    ku = "usb%d%d" % (pb, half)
                    E("gpsimd", "tensor_copy", ["hist%d" % ch], [ku + "h"], out=ub[:, 0:2], in_=hist[:, ch, :])
                    E("scalar", "activation", [kU], [ku], out=ub[:, 2:514], in_=U[:], func=AF.Copy)
                    E("scalar", "activation", [kU, "cv2", "cv3"], ["acc%d%d" % (pb, half)], out=acc[pb][half][:], in_=U[:], func=AF.Identity,
                      scale=cv_col[:, 2, ch:ch + 1], bias=cv_col[:, 3, ch:ch + 1])
                    if g < 7:
                        E("gpsimd", "tensor_copy", [ku], ["hist%d" % ch], out=hist[:, ch, :], in_=ub[:, 512:514])
                silu_flush()
                for half, ch in halves:
                    ub = usb[pb][half]
                    ku = "usb%d%d" % (pb, half)
                    ac = acc[pb][half]
                    ka = "acc%d%d" % (pb, half)
                    E("vector", "scalar_tensor_tensor", [ku, ku + "h", ka, "cv1"], [ka], out=ac[:], in0=ub[:, 1:513], scalar=cv_col[:, 1, ch:ch + 1],
                      in1=ac[:], op0=ALU.mult, op1=ALU.add)
                for half, ch in halves:
                    ub = usb[pb][half]
                    ku = "usb%d%d" % (pb, half)
                    ac = acc[pb][half]
                    ka = "acc%d%d" % (pb, half)
                    E("vector", "scalar_tensor_tensor", [ku, ku + "h", ka, "cv0"], [ka], out=ac[:], in0=ub[:, 0:512], scalar=cv_col[:, 0, ch:ch + 1],
                      in1=ac[:], op0=ALU.mult, op1=ALU.add)
                pend.append((pb, i))
            silu_flush()
            for ti in range(4):
                t = g * 4 + ti
                b2 = t % 2
                for nb in range(2):
                    for i in range(NPAIR):
                        E("tensor", "matmul", ["mT", "w_dn"], ["PB%d" % (4 + nb)], out=Y[nb][:], lhsT=mT[:, i, ti * 128:(ti + 1) * 128],
                          rhs=w_dn[:, i, nb * 512:(nb + 1) * 512], start=(i == 0), stop=(i == NPAIR - 1))
                if g + 1 < 8:
                    f_A(g + 1, ti)
                xs, kx = xload(t)
                post_norm_residual(E, rstd_ops, Y, ["PB4", "PB5"], junk, "junk", ss[b2], "ss%d" % b2, rstd[b2], "rstd%d" % b2, G4, "G4",
                                   tt[b2], "tt%d" % b2, xs, kx, tt[b2], "tt%d" % b2)
                DMA("sync", x_dst[t * 128:(t + 1) * 128, :], tt[b2][:], ["tt%d" % b2], [], "xst%d" % b2)
        x_src = x_dst

    final = [k for k in S.dma_counts.keys()]
    nsem = S.emit(final_wait_semkeys=final)
    return nc


def post_norm_residual(E, rstd_ops, Y, kY, junk, kjunk, ss, kss, rstd, krstd, G, kG, tt, ktt, xs, kx, xo, kxo):
    ssb = ss
    E("scalar", "activation", [kY[0]], [kjunk, kss, "lk" + kY[0]], out=junk[:, 0:512], in_=Y[0][:], func=AF.Square, accum_out=ssb[:])
    E("vector", "tensor_copy", [kY[0], "lk" + kY[0]], [ktt], out=tt[:, 0:512], in_=Y[0][:])
    E("scalar", "activation", [kY[1]], [kjunk, krstd, "lk" + kY[1]], out=junk[:, 512:1024], in_=Y[1][:], func=AF.Square, accum_out=rstd[:])
    E("vector", "tensor_copy", [kY[1], "lk" + kY[1]], [ktt], out=tt[:, 512:1024], in_=Y[1][:])
    E("vector", "tensor_tensor", [kss, krstd], [kss], out=ssb[:], in0=ssb[:], in1=rstd[:], op=ALU.add)
    rstd_ops(ssb[:], rstd[:], 1, [kss], [krstd], D)
    E("vector", "scalar_tensor_tensor", [ktt, krstd, kG], [ktt], out=tt[:], in0=tt[:], scalar=rstd[:, 0:1],
      in1=G[:], op0=ALU.mult, op1=ALU.mult)
    E("gpsimd", "tensor_tensor", [ktt, kx], [kxo], out=xo[:], in0=tt[:], in1=xs[:], op=ALU.add)


_CONSTS = None


def make_in_maps(inputs):
    global _CONSTS
    if _CONSTS is None:
        _CONSTS = _const_tables()
    x = np.asarray(inputs["x"], dtype=np.float32)
    c = np.asarray(inputs["c"], dtype=np.float32)
    shared = {k: np.ascontiguousarray(np.asarray(inputs[k], dtype=np.float32)) for k in WEIGHT_SPECS if k != "c"}
    maps = []
    for b in range(8):
        m = {"x": np.ascontiguousarray(x[b]), "c": np.ascontiguousarray(c[b:b + 1])}
        m.update(shared)
        m.update(_CONSTS)
        maps.append(m)
    return maps


def kernel(**inputs):
    nc = build()
    maps = make_in_maps(inputs)
    res = run_bass_kernel_spmd(nc, maps, core_ids=list(range(8)))
    return np.stack([np.asarray(r["out"]) for r in res.results], axis=0).astype(np.float32)
```

```python
import contextlib
import os
import numpy as np
import ml_dtypes
import concourse.bass as bass
import concourse.mybir as mybir
from concourse.bass_utils import run_bass_kernel_spmd

F32 = mybir.dt.float32
BF16 = mybir.dt.bfloat16
AF = mybir.ActivationFunctionType
ALU = mybir.AluOpType
AX = mybir.AxisListType

S_LEN = 4096
D = 1024
NT = S_LEN // 128
NH = 8
HD = 64
DFF = 2816
NFC = 2 * DFF // 128
NPAIR = DFF // 128
EPS = 1e-6
BIG = 30000.0
POOL_W = (2, 4, 8, 16)
VW = 68

ENGINES = ("tensor", "vector", "scalar", "gpsimd", "sync")
SEM_LIMIT = 30000


class Op:
    __slots__ = ("eng", "fn", "deps", "is_dma", "ticket", "signal", "idx")

    def __init__(self, eng, fn, is_dma):
        self.idx = 0
        self.eng = eng
        self.fn = fn
        self.is_dma = is_dma
        self.deps = []
        self.ticket = None
        self.signal = False


class Sched:
    def __init__(self, nc):
        self.nc = nc
        self.q = {e: [] for e in ENGINES}
        self.last_w = {}
        self.readers = {}
        self.dma_counts = {}
        self.dma_last = {}
        self.bar = {e: [] for e in ENGINES}

    def barrier(self):
        deps = []
        for e in ENGINES:
            for o in reversed(self.q[e]):
                if not o.is_dma:
                    deps.append(o)
                    break
        deps.extend(self.dma_last.values())
        for e in ENGINES:
            self.bar[e] = list(deps)
        self.last_w = {}
        self.readers = {}

    def op(self, eng, fn, reads=(), writes=(), dma=False, semkey=None):
        o = Op(eng, fn, dma)
        deps = set()
        for k in reads:
            w = self.last_w.get(k)
            if w is not None:
                deps.add(w)
        for k in writes:
            w = self.last_w.get(k)
            if w is not None:
                deps.add(w)
            for r in self.readers.get(k, ()):
                deps.add(r)
        for d in deps:
            if d is o:
                continue
            if (not d.is_dma) and (not dma) and d.eng == eng:
                if eng == "tensor":
                    continue
                raw = any(self.last_w.get(k) is d for k in reads)
                if not raw:
                    continue
            o.deps.append(d)
        if dma and semkey in self.dma_last:
            o.deps.append(self.dma_last[semkey])
        if self.bar[eng]:
            for d in self.bar[eng]:
                if d.is_dma or d.eng != eng:
                    o.deps.append(d)
            self.bar[eng] = []
        best = {}
        pruned = []
        for d in o.deps:
            if d.is_dma:
                pruned.append(d)
            else:
                b = best.get(d.eng)
                if b is None or d.idx > b.idx:
                    best[d.eng] = d
        o.deps = pruned + list(best.values())
        o.idx = len(self.q[eng])
        for k in writes:
            self.last_w[k] = o
            self.readers[k] = []
        for k in reads:
            self.readers.setdefault(k, []).append(o)
        if dma:
            c = self.dma_counts.get(semkey, 0) + 16
            self.dma_counts[semkey] = c
            o.ticket = (("dma", semkey), c)
            self.dma_last[semkey] = o
        self.q[eng].append(o)
        return o

    def emit(self, final_wait_semkeys=()):
        nc = self.nc
        for e in ENGINES:
            for o in self.q[e]:
                for d in o.deps:
                    if not d.is_dma:
                        d.signal = True
        semnames = set()
        for e in ENGINES:
            cnt = 0
            seg = 0
            for o in self.q[e]:
                if o.is_dma:
                    semnames.add(o.ticket[0])
                    continue
                if o.signal:
                    cnt += 1
                    if cnt > SEM_LIMIT:
                        seg += 1
                        cnt = 1
                    o.ticket = (("eng", e, seg), cnt)
                    semnames.add(o.ticket[0])
        semnames = sorted(semnames, key=str)
        with contextlib.ExitStack() as st:
            sems = {}
            for i, n in enumerate(semnames):
                sems[n] = st.enter_context(nc.semaphore("s%d" % i))
            block = st.enter_context(nc.Block())

            def run(engname):
                def body(eng):
                    waited = {}
                    for o in self.q[engname]:
                        need = {}
                        for d in o.deps:
                            s, v = d.ticket
                            if need.get(s, 0) < v:
                                need[s] = v
                        for s, v in need.items():
                            if waited.get(s, 0) < v:
                                eng.wait_ge(sems[s], v)
                                waited[s] = v
                        ins = o.fn(eng)
                        if o.is_dma:
                            ins.then_inc(sems[o.ticket[0]], 16)
                        elif o.signal:
                            ins.then_inc(sems[o.ticket[0]], 1)
                    if engname == "sync":
                        for k in final_wait_semkeys:
                            s = ("dma", k)
                            v = self.dma_counts[k]
                            if waited.get(s, 0) < v:
                                eng.wait_ge(sems[s], v)
                                waited[s] = v
                return body

            block.tensor(run("tensor"))
            block.vector(run("vector"))
            block.scalar(run("scalar"))
            block.gpsimd(run("gpsimd"))
            block.sync(run("sync"))
        return len(semnames)


def _const_tables():
    bf = ml_dtypes.bfloat16
    pos = np.arange(S_LEN, dtype=np.float32)
    inv_freq = np.power(np.float32(500000.0), -np.arange(0, 16, 2, dtype=np.float32) / np.float32(16))
    ang = (pos[:, None] * inv_freq[None, :]).astype(np.float32)
    cos = np.cos(ang).astype(np.float32)
    sin = np.sin(ang).astype(np.float32)
    def tm(a):
        return np.ascontiguousarray(a.reshape(NT, 128, -1).transpose(1, 0, 2))
    ropek = tm(np.concatenate([cos, sin], axis=1)).astype(np.float32)
    ropeq = (ropek * np.float32(0.125)).astype(np.float32)
    qt = np.arange(NT)
    j = qt // 2
    n = np.arange(16)
    past = (n[None, :] < j[:, None])
    pastmask = np.where(past, 0.0, -1e30).astype(np.float32).reshape(1, NT * 16)
    pastind = past.astype(np.float32).reshape(1, NT * 16)
    ownfut = np.where(n[None, :] > j[:, None], -BIG, 0.0).astype(np.float32).reshape(1, NT * 16)
    onehot = (np.arange(S_LEN)[None, :] // 256 == n[:, None]).astype(np.float32).astype(bf)
    k = np.arange(128)
    tri = (k[None, :] >= k[:, None]).astype(np.float32).astype(bf)
    identb = np.eye(128, dtype=np.float32).astype(bf)
    identf = np.eye(128, dtype=np.float32)
    onesb = np.ones((128, 128), dtype=np.float32).astype(bf)
    rc = np.zeros((1, 4, 16), dtype=np.float32)
    for g, w in enumerate(POOL_W):
        rc[0, g, :] = 1.0 / np.minimum(np.arange(16) + 1, w)
    return {
        "ropeq": ropeq, "ropek": ropek,
        "pastmask": np.ascontiguousarray(np.broadcast_to(pastmask, (128, NT * 16))),
        "pastind": np.ascontiguousarray(np.broadcast_to(pastind, (128, NT * 16))),
        "ownfut": np.ascontiguousarray(np.broadcast_to(ownfut, (128, NT * 16))),
        "onehot": onehot, "tri": tri, "identb": identb, "identf": identf, "onesb": onesb,
        "rc": np.ascontiguousarray(np.broadcast_to(rc, (128, 4, 16))),
    }


CONST_SPECS = {
    "ropeq": ([128, NT, 16], F32), "ropek": ([128, NT, 16], F32),
    "pastmask": ([128, NT * 16], F32), "pastind": ([128, NT * 16], F32), "ownfut": ([128, NT * 16], F32),
    "onehot": ([16, S_LEN], BF16), "tri": ([128, 128], BF16), "identb": ([128, 128], BF16),
    "identf": ([128, 128], F32), "onesb": ([128, 128], BF16), "rc": ([128, 4, 16], F32),
}

WEIGHT_SPECS = {
    "c": [1, D], "w_ada": [2, D, 6 * D], "b_ada": [2, 6 * D], "g_pre_mix": [2, D], "w_in": [2, D, 2048],
    "w_pool": [2, 4, 128, 128], "pool_scale": [2, 512], "attn_out_gain": [2, 512], "pool_out_gain": [2, 512],
    "w_out": [2, D, D], "g_post_mix": [2, D], "g_pre_ffn": [2, D], "w_up": [2, D, 2 * DFF],
    "conv_w": [2, 3, 2 * DFF], "conv_b": [2, 2 * DFF], "w_down": [2, DFF, D], "g_post_ffn": [2, D],
}


class Alloc:
    def __init__(self, nc):
        self.nc = nc
        self.base = (nc.sbuf_base + 63) // 64 * 64
        self.top = nc.sbuf_top
        self.cur = self.base
        self.limit = self.top
        self.n = 0

    def mark(self):
        return self.cur

    def reset(self, m):
        self.cur = m

    def __call__(self, shape, dt):
        sz = 1
        for s in shape[1:]:
            sz *= s
        nbytes = sz * (4 if dt == F32 else 2)
        nbytes = (nbytes + 63) // 64 * 64
        off = self.cur
        assert off + nbytes <= self.limit, ("SBUF overflow", off, nbytes, self.limit)
        self.cur += nbytes
        self.n += 1
        return self.nc.alloc_sbuf_tensor_at("sb%d" % self.n, list(shape), dt, offset=off)


def build(n_layers=2, stop_after=None, debug=False):
    nc = bass.Bass("TRN2", target_bir_lowering=False)
    I = {}
    I["x"] = nc.dram_tensor("x", [S_LEN, D], F32, kind="ExternalInput").ap()
    for k, shp in WEIGHT_SPECS.items():
        I[k] = nc.dram_tensor(k, shp, F32, kind="ExternalInput").ap()
    for k, (shp, dt) in CONST_SPECS.items():
        I[k] = nc.dram_tensor(k, shp, dt, kind="ExternalInput").ap()
    out = nc.dram_tensor("out", [S_LEN, D], F32, kind="ExternalOutput").ap()
    sk = "ExternalOutput" if debug else "Internal"
    qT_s = nc.dram_tensor("qT_s", [NH, HD, S_LEN], BF16, kind=sk).ap()
    kT_s = nc.dram_tensor("kT_s", [NH, HD, S_LEN], BF16, kind=sk).ap()
    v_s = nc.dram_tensor("v_s", [S_LEN, NH * VW], BF16, kind=sk).ap()
    pm_s = nc.dram_tensor("pm_s", [NT, 128, 512], BF16, kind=sk).ap()
    g_s = nc.dram_tensor("g_s", [2, 128, D], F32, kind=sk).ap()
    xa = nc.dram_tensor("xa", [S_LEN, D], F32, kind=sk).ap()
    xb = nc.dram_tensor("xb", [S_LEN, D], F32, kind=sk).ap()
    dbg = {}
    if debug:
        dbg["ao"] = nc.dram_tensor("dbg_ao", [S_LEN, 512], F32, kind="ExternalOutput").ap()

    S = Sched(nc)
    A = Alloc(nc)
    PB = [nc.alloc_psum_tensor("pb%d" % i, [128, 512], F32) for i in range(6)]
    PT = [nc.alloc_psum_tensor("pt%d" % i, [128, 1024], BF16) for i in range(2)]

    sec = [None]
    enabled = os.environ.get("KSEC")
    nt_run = int(os.environ.get("KNT", NT))

    def skip():
        return enabled is not None and sec[0] is not None and sec[0] not in enabled

    def E(eng, meth, reads, writes, **kw):
        if skip():
            return None
        return S.op(eng, lambda e: getattr(e, meth)(**kw), reads, writes)

    otc = [0]

    def OT():
        otc[0] += 1
        return "ot%d" % (otc[0] % 6)

    def DMA(eng, out_, in_, reads, writes, semkey, slow=False):
        if skip():
            return None
        if semkey is None:
            semkey = OT()
        if slow:
            return S.op(eng, lambda e: e.dma_start(out=out_, in_=in_, allow_slow_non_contiguous=True),
                        reads, writes, dma=True, semkey=semkey)
        return S.op(eng, lambda e: e.dma_start(out=out_, in_=in_), reads, writes, dma=True, semkey=semkey)

    identb = A([128, 128], BF16)
    identf = A([128, 128], F32)
    onesb = A([128, 128], BF16)
    tri = A([128, 128], BF16)
    for nm, t in (("identb", identb), ("identf", identf), ("onesb", onesb), ("tri", tri)):
        DMA("sync", t[:], I[nm], [], [nm], None)
    cols = A([128, 4, 8], F32)
    aog_col = A([128, 4], F32)
    ps_col = A([128, 4], F32)
    pg_col = A([128, 4], F32)
    cv_col = A([128, 4, NFC], F32)
    rstd_eps = EPS
    persist_mark = A.mark()

    def rstd_ops(ss, rstd, n, keys_r, keys_w, dim):
        E("scalar", "activation", keys_r, keys_w, out=rstd, in_=ss, func=AF.Ln, scale=1.0 / dim, bias=rstd_eps)
        E("scalar", "activation", keys_w, keys_w, out=rstd, in_=rstd, func=AF.Exp, scale=-0.5)

    x_src = I["x"]
    for l in range(n_layers):
        last_layer = (l == n_layers - 1)
        x_mid = xa
        x_dst = out if last_layer else xb
        S.barrier()
        A.reset(persist_mark)
        A.limit = A.top
        c_col = A([128, 8], F32)
        cact = A([128, 8], F32)
        cbc = A([128, 8, 128], F32)
        bada = A([128, 6 * D], F32)
        modbc = A([128, 6 * D], F32)
        wblk = [A([128, 8, 512], F32) for _ in range(2)]
        gbc = A([128, D], F32)
        gtmp = A([128, D], F32)
        dtmp = A([128, 8, 128], F32)
        stg = [A([64, 128], F32) for _ in range(2)]
        stc = [0]

        def load_col(vec1d, n, dst, wkey):
            i_ = stc[0] % 2
            stc[0] += 1
            sg = stg[i_]
            DMA("sync", sg[0:n, :], vec1d.rearrange("(c p) -> c p", p=128), [], ["stg%d" % i_], None)
            E("tensor", "matmul", ["stg%d" % i_, "identf"], ["PB2"], out=PB[2][:, 0:n], lhsT=sg[0:n, :], rhs=identf[0:n, 0:n],
              start=True, stop=True)
            E("vector", "tensor_copy", ["PB2"], [wkey], out=dst, in_=PB[2][:, 0:n])

        load_col(I["c"][0], 8, c_col[:], "c_col")
        DMA("sync", bada[:], I["b_ada"][l].partition_broadcast(128), [], ["bada"], None)
        load_col(I["attn_out_gain"][l], 4, aog_col[:], "aog")
        load_col(I["pool_scale"][l], 4, ps_col[:], "psc")
        load_col(I["pool_out_gain"][l], 4, pg_col[:], "pgc")
        for j3 in range(3):
            load_col(I["conv_w"][l, j3], NFC, cv_col[:, j3, :], "cv%d" % j3)
        load_col(I["conv_b"][l], NFC, cv_col[:, 3, :], "cv3")
        E("scalar", "activation", ["c_col"], ["cact"], out=cact[:], in_=c_col[:], func=AF.Silu)
        E("vector", "tensor_copy", ["cact"], ["cbc"], out=cbc[:], in_=cact[:].unsqueeze(2).broadcast_to([128, 8, 128]))
        for blk in range(12):
            wb = wblk[blk % 2]
            kb = "wblk%d" % (blk % 2)
            pm_ = PB[blk % 2]
            kp = "PB%d" % (blk % 2)
            DMA("sync", wb[:], I["w_ada"][l][:, blk * 512:(blk + 1) * 512].rearrange("(kc p) n -> p kc n", p=128),
                [], [kb], "wada%d" % (blk % 2))
            for kc in range(8):
                E("tensor", "matmul", [kb, "cbc"], [kp], out=pm_[:], lhsT=cbc[:, kc, :], rhs=wb[:, kc, :],
                  start=(kc == 0), stop=(kc == 7))
            E("vector", "tensor_tensor", [kp, "bada"], ["modbc"], out=modbc[:, blk * 512:(blk + 1) * 512], in0=pm_[:],
              in1=bada[:, blk * 512:(blk + 1) * 512], op=ALU.add)

        def diag_extract(src_ap, dst_ap, rkeys, wkey):
            E("vector", "tensor_tensor", rkeys + ["identf"], ["dtmp"], out=dtmp[:],
              in0=src_ap.rearrange("p (k q) -> p k q", k=8),
              in1=identf[:].unsqueeze(1).broadcast_to([128, 8, 128]), op=ALU.mult)
            E("vector", "tensor_reduce", ["dtmp"], [wkey], out=dst_ap, in_=dtmp[:], axis=AX.X, op=ALU.add)

        for gi, (gname, moff) in enumerate((("g_post_mix", 2 * D), ("g_post_ffn", 5 * D))):
            DMA("sync", gbc[:], I[gname][l].partition_broadcast(128), [], ["gbc"], None)
            E("vector", "tensor_tensor", ["gbc", "modbc"], ["gtmp"], out=gtmp[:], in0=modbc[:, moff:moff + D], in1=gbc[:], op=ALU.mult)
            DMA("sync", g_s[gi], gtmp[:], ["gtmp"], [], None)
        for ci, (gname, scoff, shoff) in enumerate((("g_pre_mix", 1 * D, 0), ("g_pre_ffn", 4 * D, 3 * D))):
            DMA("sync", gbc[:], I[gname][l].partition_broadcast(128), [], ["gbc"], None)
            E("vector", "scalar_tensor_tensor", ["gbc", "modbc"], ["gtmp"], out=gtmp[:], in0=modbc[:, scoff:scoff + D],
              scalar=1.0, in1=gbc[:], op0=ALU.add, op1=ALU.mult)
            diag_extract(gtmp[:], cols[:, 2 * ci, :], ["gtmp"], "cols%d" % (2 * ci))
            diag_extract(modbc[:, shoff:shoff + D], cols[:, 2 * ci + 1, :], ["modbc"], "cols%d" % (2 * ci + 1))
        if stop_after == "P":
            break

        S.barrier()
        A.reset(persist_mark)
        w_in = A([128, 8, 2048], BF16)
        w_pool = A([128, 4, 128], BF16)
        ropeq = A([128, NT, 16], F32)
        ropek = A([128, NT, 16], F32)
        rc = A([128, 4, 16], F32)
        xt = [A([128, D], F32) for _ in range(3)]
        junk = A([128, D], BF16)
        ss = [A([128, 1], F32) for _ in range(3)]
        rstd = [A([128, 1], F32) for _ in range(3)]
        xn = [A([128, D], BF16) for _ in range(3)]
        hT = [A([128, 8, 128], BF16) for _ in range(2)]
        q_tm = [A([128, 512], BF16) for _ in range(2)]
        k_tm = [A([128, 512], BF16) for _ in range(2)]
        rt = [A([128, 8, 8], F32) for _ in range(4)]
        zq = A([128, 8, 16], F32)
        qst = [A([64, 2, 4, 512], BF16) for _ in range(2)]
        kst = [A([64, 2, 4, 512], BF16) for _ in range(2)]
        vst = [A([128, 8, VW], BF16) for _ in range(2)]
        pmst = [A([128, 4, 128], BF16) for _ in range(2)]
        pz = A([128, 4, 144], F32)
        s2 = A([128, 4, 144], F32)
        s4 = A([128, 4, 144], F32)
        s8 = A([128, 4, 144], F32)
        s16 = A([128, 4, 144], F32)
        pooledT = [A([128, 4, 128], BF16) for _ in range(2)]
        po_sb = A([128, 4, 128], F32)
        sq = A([128, 4, 128], BF16)
        prs = A([128, 128], F32)
        ptmp = A([128, 4, 128], F32)
        etmp = A([128, 4, 16], F32)

        sec[0] = "L"
        for kc in range(8):
            DMA("gpsimd", w_in[:, kc, :], I["w_in"][l][kc * 128:(kc + 1) * 128, :], [], ["w_in"], "wl%d" % (kc % 4))
        DMA("gpsimd", w_pool[:], I["w_pool"][l].rearrange("g c e -> c g e"), [], ["w_pool"], "wl0")
        DMA("sync", ropeq[:], I["ropeq"], [], ["ropeq"], None)
        DMA("sync", ropek[:], I["ropek"], [], ["ropek"], None)
        DMA("sync", rc[:], I["rc"], [], ["rc"], None)
        for b_ in range(2):
            E("gpsimd", "memset", [], ["vst%d" % b_], ap=vst[b_][:], constant=1.0)
        E("gpsimd", "memset", [], ["pz"], ap=pz[:], constant=0.0)
        Pq, Pk, Pv, Pp, Py, Ps = PB
        T0, T1 = PT[0], PT[1]
        def m1_L(t):
            DMA("sync", xt[t % 3][:], x_src[t * 128:(t + 1) * 128, :], [], ["xt%d" % (t % 3)], "xl%d" % (t % 3))

        def m1_N(t):
            b3 = t % 3
            xs = xt[b3]
            kx = "xt%d" % b3
            E("scalar", "activation", [kx], ["junk", "ss%d" % b3], out=junk[:], in_=xs[:], func=AF.Square, accum_out=ss[b3][:])
            rstd_ops(ss[b3][:], rstd[b3][:], 1, ["ss%d" % b3], ["rstd%d" % b3], D)
            E("vector", "tensor_scalar", [kx, "rstd%d" % b3], ["xn%d" % b3], out=xn[b3][:], in0=xs[:], scalar1=rstd[b3][:, 0:1],
              scalar2=None, op0=ALU.mult)

        def m1_T(t):
            b3 = t % 3
            b2 = t % 2
            for kc in range(8):
                E("tensor", "transpose", ["xn%d" % b3, "identb"], ["T0"], out=T0[:, kc * 128:(kc + 1) * 128],
                  in_=xn[b3][:, kc * 128:(kc + 1) * 128], identity=identb[:])
            for kc in range(8):
                E("scalar", "activation", ["T0", "cols0", "cols1"], ["hT%d" % b2], out=hT[b2][:, kc, :], in_=T0[:, kc * 128:(kc + 1) * 128],
                  func=AF.Identity, scale=cols[:, 0, kc:kc + 1], bias=cols[:, 1, kc:kc + 1])

        def m1_B1(t):
            b2 = t % 2
            for P_, kP, c0 in ((Pq, "Pq", 0), (Pk, "Pk", 512), (Pv, "Pv", 1024)):
                for kc in range(8):
                    E("tensor", "matmul", ["hT%d" % b2, "w_in"], [kP], out=P_[:], lhsT=hT[b2][:, kc, :], rhs=w_in[:, kc, c0:c0 + 512],
                      start=(kc == 0), stop=(kc == 7))
            for g in range(4):
                for kc in range(8):
                    E("tensor", "matmul", ["hT%d" % b2, "w_in"], ["Pp"], out=Pp[:, g * 128:(g + 1) * 128],
                      lhsT=w_in[:, kc, 1536 + g * 128:1536 + (g + 1) * 128], rhs=hT[b2][:, kc, :], start=(kc == 0), stop=(kc == 7))
            for P_, kP, tm_, ktm, rope, krope, sc_ in ((Pq, "Pq", q_tm[b2], "q_tm%d" % b2, ropeq, "ropeq", 0.125),
                                                       (Pk, "Pk", k_tm[b2], "k_tm%d" % b2, ropek, "ropek", 1.0)):
                E("scalar", "activation", [kP], [ktm], out=tm_[:], in_=P_[:], func=AF.Identity, scale=sc_)
                pv = P_[:].rearrange("p (h d) -> p h d", h=8)
                ov = tm_[:].rearrange("p (h d) -> p h d", h=8)
                E("scalar", "activation", [kP], ["zq"], out=zq[:], in_=pv[:, :, 0:16], func=AF.Copy)
                cosb = rope[:, t, 0:8].unsqueeze(1).broadcast_to([128, 8, 8])
                sinb = rope[:, t, 8:16].unsqueeze(1).broadcast_to([128, 8, 8])
                x1 = zq[:, :, 0:8]
                x2 = zq[:, :, 8:16]
                E("vector", "tensor_tensor", ["zq", krope], ["rt0"], out=rt[0][:], in0=x1, in1=cosb, op=ALU.mult)
                E("vector", "tensor_tensor", ["zq", krope], ["rt1"], out=rt[1][:], in0=x2, in1=sinb, op=ALU.mult)
                E("vector", "tensor_tensor", ["zq", krope], ["rt2"], out=rt[2][:], in0=x2, in1=cosb, op=ALU.mult)
                E("vector", "tensor_tensor", ["zq", krope], ["rt3"], out=rt[3][:], in0=x1, in1=sinb, op=ALU.mult)
                E("vector", "tensor_tensor", ["rt0", "rt1"], [ktm], out=ov[:, :, 0:8], in0=rt[0][:], in1=rt[1][:], op=ALU.subtract)
                E("vector", "tensor_tensor", ["rt2", "rt3"], [ktm], out=ov[:, :, 8:16], in0=rt[2][:], in1=rt[3][:], op=ALU.add)
            vb = t % 2
            E("scalar", "activation", ["Pv"], ["vst%d" % vb], out=vst[vb][:, :, 0:64], in_=Pv[:].rearrange("p (h d) -> p h d", h=8),
              func=AF.Copy)
            DMA("sync", v_s[t * 128:(t + 1) * 128, :], vst[vb][:].rearrange("p h d -> p (h d)"), ["vst%d" % vb], [], "vs%d" % vb)
            pT = pooledT[b2]
            kpT = "pooledT%d" % b2
            E("scalar", "activation", ["Pp"], ["pz"], out=pz[:, :, 16:144], in_=Pp[:].rearrange("p (g t) -> p g t", g=4), func=AF.Copy)
            E("gpsimd", "tensor_tensor", ["pz"], ["s2"], out=s2[:, :, 1:144], in0=pz[:, :, 1:144], in1=pz[:, :, 0:143], op=ALU.add)
            E("gpsimd", "tensor_tensor", ["s2"], ["s4"], out=s4[:, 1:4, 3:144], in0=s2[:, 1:4, 3:144], in1=s2[:, 1:4, 1:142], op=ALU.add)
            E("gpsimd", "tensor_tensor", ["s4"], ["s8"], out=s8[:, 2:4, 7:144], in0=s4[:, 2:4, 7:144], in1=s4[:, 2:4, 3:140], op=ALU.add)
            E("gpsimd", "tensor_tensor", ["s8"], ["s16"], out=s16[:, 3:4, 15:144], in0=s8[:, 3:4, 15:144], in1=s8[:, 3:4, 7:136], op=ALU.add)
            sums = (s2, s4, s8, s16)
            for g in range(4):
                E("vector", "scalar_tensor_tensor", ["s%d" % (2 << g), "pz"], [kpT], out=pT[:, g, :], in0=sums[g][:, g, 16:144],
                  scalar=1.0 / POOL_W[g], in1=pz[:, g, 16:144], op0=ALU.mult, op1=ALU.subtract)
            if t == 0:
                for g in range(4):
                    E("gpsimd", "tensor_tensor", ["s%d" % (2 << g), "rc"], ["etmp"], out=etmp[:, g, :], in0=sums[g][:, g, 16:32], in1=rc[:, g, :], op=ALU.mult)
                    E("gpsimd", "tensor_tensor", ["etmp", "pz"], [kpT], out=pT[:, g, 0:16], in0=etmp[:, g, :], in1=pz[:, g, 16:32], op=ALU.subtract)
            E("gpsimd", "tensor_copy", ["pz", "s2", "s4", "s8", "s16", kpT], ["pz"], out=pz[:, :, 0:16], in_=pz[:, :, 128:144])

        def m1_B2(t):
            b2 = t % 2
            g4 = t // 4
            ti = t % 4
            sb_ = g4 % 2
            pT = pooledT[b2]
            kpT = "pooledT%d" % b2
            for g in range(4):
                E("tensor", "matmul", [kpT, "w_pool"], ["Py"], out=Py[:, g * 128:(g + 1) * 128], lhsT=w_pool[:, g, :], rhs=pT[:, g, :],
                  start=True, stop=True)
            for hi, (tm_, ktm, st_, kst_) in enumerate(((q_tm[b2], "q_tm%d" % b2, qst[sb_], "qst%d" % sb_), (k_tm[b2], "k_tm%d" % b2, kst[sb_], "kst%d" % sb_))):
                kT1 = "T1"
                for pr in range(4):
                    E("tensor", "transpose", [ktm, "identb"], [kT1], out=T1[:, hi * 512 + pr * 128:hi * 512 + (pr + 1) * 128],
                      in_=tm_[:, pr * 128:(pr + 1) * 128], identity=identb[:])
                for pa in range(2):
                    E("vector", "tensor_copy", [kT1], [kst_], out=st_[:, pa, :, ti * 128:(ti + 1) * 128],
                      in_=T1[pa * 64:(pa + 1) * 64, hi * 512:(hi + 1) * 512].rearrange("p (r t) -> p r t", r=4))
            if ti == 3:
                for pa in range(2):
                    DMA("sync", qT_s.rearrange("(pr pa) d t -> pa d pr t", pa=2)[pa][:, :, g4 * 512:(g4 + 1) * 512], qst[sb_][:, pa, :, :],
                        ["qst%d" % sb_], [], "qs%d%d" % (sb_, pa))
                    DMA("sync", kT_s.rearrange("(pr pa) d t -> pa d pr t", pa=2)[pa][:, :, g4 * 512:(g4 + 1) * 512], kst[sb_][:, pa, :, :],
                        ["kst%d" % sb_], [], "ks%d%d" % (sb_, pa))
            E("vector", "tensor_tensor", ["Py", "psc"], ["po_sb"], out=po_sb[:], in0=Py[:].rearrange("p (g t) -> p g t", g=4),
              in1=ps_col[:].unsqueeze(2).broadcast_to([128, 4, 128]), op=ALU.mult)
            E("scalar", "activation", ["po_sb"], ["sq"], out=sq[:], in_=po_sb[:], func=AF.Square)
            for g in range(4):
                E("tensor", "matmul", ["sq", "onesb"], ["Ps"], out=Ps[:, 0:128], lhsT=onesb[:], rhs=sq[:, g, :], start=(g == 0), stop=(g == 3))
            rstd_ops(Ps[:, 0:128], prs[:], 128, ["Ps"], ["prs"], 512)
            E("vector", "tensor_tensor", ["po_sb", "pgc"], ["ptmp"], out=ptmp[:], in0=po_sb[:],
              in1=pg_col[:].unsqueeze(2).broadcast_to([128, 4, 128]), op=ALU.mult)
            pb_ = t % 2
            E("vector", "tensor_tensor", ["ptmp", "prs"], ["pmst%d" % pb_], out=pmst[pb_][:], in0=ptmp[:],
              in1=prs[:].unsqueeze(1).broadcast_to([128, 4, 128]), op=ALU.mult)
            DMA("sync", pm_s[t], pmst[pb_][:].rearrange("p g t -> p (g t)"), ["pmst%d" % pb_], [], "pms%d" % pb_)

        sec[0] = None
        for t0_ in range(min(3, NT)):
            m1_L(t0_)
        m1_N(0)
        m1_N(1)
        m1_T(0)
        for t in range(NT):
            if t + 3 < NT:
                m1_L(t + 3)
            if t + 2 < NT:
                m1_N(t + 2)
            if t + 1 < NT:
                m1_T(t + 1)
            m1_B1(t)
            if t >= 1:
                m1_B2(t - 1)
        m1_B2(NT - 1)
        sec[0] = None
        if stop_after == "M1":
            break

        S.barrier()
        A.reset(persist_mark)
        ao_all = A([128, NT, 512], F32)
        w_out = A([128, 8, D], BF16)
        G2 = A([128, D], F32)
        m3_mark = A.mark()
        vaug = A([128, NT, NH * VW], BF16)
        kaug = [A([128, S_LEN], BF16) for _ in range(2)]
        qaug = [A([128, S_LEN], BF16) for _ in range(2)]
        kmT = A([64, 16], F32)
        kmTb = A([64, 16], BF16)
        pastmask = A([128, NT * 16], F32)
        pastind = A([128, NT * 16], F32)
        ownfut = A([128, NT * 16], F32)
        Gm = A([128, NT * 16], F32)
        m8 = A([128, NT, 8], F32)
        sel = A([128, NT * 16], F32)
        biasW = A([128, NT, 80], BF16)
        PTb = [A([128, 512], BF16) for _ in range(3)]
        rl = A([128, 4], F32)
        osb = A([128, 512], F32)
        for v4 in range(4):
            DMA("sync", vaug[:, v4 * 8:(v4 + 1) * 8, :], v_s[v4 * 1024:(v4 + 1) * 1024, :].rearrange("(t p) f -> p t f", p=128), [], ["vaug"], None)
        DMA("sync", pastmask[:], I["pastmask"], [], ["pastmask"], None)
        DMA("sync", pastind[:], I["pastind"], [], ["pastind"], None)
        DMA("sync", ownfut[:], I["ownfut"], [], ["ownfut"], None)
        for b_ in range(2):
            DMA("sync", kaug[b_][64:80, :], I["onehot"], [], ["kaug%d" % b_], None)
        E("gpsimd", "memset", [], ["biasW"], ap=biasW[:], constant=0.0)
        def wout_load(kc):
            DMA("gpsimd", w_out[:, kc, :], I["w_out"][l][kc * 128:(kc + 1) * 128, :], [], ["w_out"], "wl%d" % (kc % 4))
        DMA("sync", G2[:], g_s[0], [], ["G2"], None)
        SB = PB[0:3]
        OB = PB[3:5]
        GP = PB[5]
        BT = PT[0]
        LA = 2

        def prologue_A(h):
            hb = h % 2
            kk = "kaug%d" % hb
            kq = "qaug%d" % hb
            DMA("sync", kaug[hb][0:64, :], kT_s[h], [], [kk], "kl%d" % hb)
            DMA("sync", qaug[hb][0:64, :], qT_s[h], [], [kq], "ql%d" % hb)
            E("vector", "tensor_reduce", [kk], ["kmT"], out=kmT[:], in_=kaug[hb][0:64, :].rearrange("p (n k) -> p n k", n=16), axis=AX.X, op=ALU.add)
            E("vector", "tensor_scalar", ["kmT"], ["kmTb"], out=kmTb[:], in0=kmT[:], scalar1=1.0 / 256, scalar2=None, op0=ALU.mult)
            for qt_ in range(NT):
                E("tensor", "matmul", [kq, "kmTb"], ["GP"], out=GP[:, qt_ * 16:(qt_ + 1) * 16], lhsT=qaug[hb][0:64, qt_ * 128:(qt_ + 1) * 128],
                  rhs=kmTb[:], start=True, stop=True)
            E("vector", "tensor_tensor", ["GP", "pastmask"], ["Gm"], out=Gm[:], in0=GP[:], in1=pastmask[:], op=ALU.add)
            for qt_ in range(NT):
                E("vector", "max", ["Gm"], ["m8"], out=m8[:, qt_, :], in_=Gm[:, qt_ * 16:(qt_ + 1) * 16])
            E("vector", "tensor_tensor", ["Gm", "m8"], ["sel"], out=sel[:].rearrange("p (q n) -> p q n", n=16),
              in0=Gm[:].rearrange("p (q n) -> p q n", n=16), in1=m8[:, :, 2:3].broadcast_to([128, NT, 16]), op=ALU.is_ge)
            E("vector", "tensor_scalar", ["sel"], ["sel"], out=sel[:], in0=sel[:], scalar1=-1.0, scalar2=BIG, op0=ALU.add, op1=ALU.mult)
            E("vector", "tensor_tensor", ["sel", "pastind"], ["sel"], out=sel[:], in0=sel[:], in1=pastind[:], op=ALU.mult)
            E("vector", "tensor_tensor", ["sel", "ownfut"], ["biasW"], out=biasW[:, :, 64:80], in0=sel[:].rearrange("p (q n) -> p q n", n=16),
              in1=ownfut[:].rearrange("p (q n) -> p q n", n=16), op=ALU.add)

        def prologue_B(h):
            hb = h % 2
            kq = "qaug%d" % hb
            for r4 in range(4):
                for q8 in range(8):
                    qt_ = r4 * 8 + q8
                    E("tensor", "transpose", ["biasW", "identb"], ["BT"], out=BT[0:80, q8 * 128:(q8 + 1) * 128], in_=biasW[:, qt_, :], identity=identb[:])
                E("vector", "tensor_copy", ["BT"], [kq], out=qaug[hb][64:80, r4 * 1024:(r4 + 1) * 1024], in_=BT[64:80, :])

        steps = [(h, g, kt) for h in range(NH) for g in range(8) for kt in range(4 * g + 4)]
        NPH = len(steps) // NH

        def emit_score(i):
            h, g, kt = steps[i]
            hb = h % 2
            kk = "kaug%d" % hb
            kq = "qaug%d" % hb
            sb_ = SB[i % 3]
            ks = "SB%d" % (i % 3)
            pt_ = PTb[i % 3]
            kpt = "PTb%d" % (i % 3)
            r = kt - 4 * g
            c0 = 128 * r if r > 0 else 0
            E("tensor", "matmul", [kk, kq], [ks], out=sb_[:, c0:512], lhsT=kaug[hb][0:80, kt * 128:(kt + 1) * 128],
              rhs=qaug[hb][0:80, g * 512 + c0:(g + 1) * 512], start=True, stop=True)
            E("scalar", "activation", [ks], [kpt], out=pt_[:, c0:512], in_=sb_[:, c0:512], func=AF.Exp)
            if r >= 0:
                E("gpsimd", "tensor_tensor", [kpt, "tri"], [kpt], out=pt_[:, r * 128:(r + 1) * 128], in0=pt_[:, r * 128:(r + 1) * 128],
                  in1=tri[:], op=ALU.mult)

        def emit_pv(i):
            h, g, kt = steps[i]
            gg = h * 8 + g
            ob = OB[gg % 2]
            ko = "OB%d" % (gg % 2)
            pt_ = PTb[i % 3]
            kpt = "PTb%d" % (i % 3)
            for qi in range(4):
                if kt <= 4 * g + qi:
                    E("tensor", "matmul", [kpt, "vaug"], [ko], out=ob[:, qi * 128:qi * 128 + 65], lhsT=pt_[:, qi * 128:(qi + 1) * 128],
                      rhs=vaug[:, kt, h * VW:h * VW + 65], start=(kt == 0 and qi == 0), stop=(kt == 4 * g + qi))
            if kt == 4 * g + 3:
                E("vector", "tensor_copy", [ko], ["osb"], out=osb[:], in_=ob[:])
                obv = osb[:].rearrange("p (q c) -> p q c", q=4)
                E("vector", "reciprocal", ["osb"], ["rl"], out=rl[:].unsqueeze(2), in_=obv[:, :, 64:65])
                E("vector", "tensor_tensor", ["osb", "rl"], ["ao_all"], out=ao_all[:, 4 * g:4 * g + 4, h * 64:(h + 1) * 64], in0=obv[:, :, 0:64],
                  in1=rl[:].unsqueeze(2).broadcast_to([128, 4, 64]), op=ALU.mult)

        prologue_A(0)
        prologue_B(0)
        for i in range(len(steps) + LA):
            if i < len(steps):
                emit_score(i)
            j = i - LA
            if j >= 0:
                emit_pv(j)
                h = steps[j][0]
                pos = j - h * NPH
                if pos == NPH // 4:
                    wout_load(h)
                if h + 1 < NH:
                    if pos == NPH // 2:
                        prologue_A(h + 1)
                    if pos == NPH - 16:
                        prologue_B(h + 1)
        if debug:
            for v4 in range(4):
                DMA("sync", dbg["ao"][v4 * 1024:(v4 + 1) * 1024, :].rearrange("(t p) f -> p t f", p=128), ao_all[:, v4 * 8:(v4 + 1) * 8, :], ["ao_all"], [], None)
        S.barrier()
        A.reset(m3_mark)
        WUP_BYTES = 8 * 2 * DFF * 2
        WUP_OFF = (A.top - WUP_BYTES) // 64 * 64
        A.limit = WUP_OFF
        w_up = nc.alloc_sbuf_tensor_at("w_up_l%d" % l, [128, 8, 2 * DFF], BF16, offset=WUP_OFF)
        def wup_prefetch(j):
            kc, hf = j // 2, j % 2
            DMA("gpsimd", w_up[:, kc, hf * DFF:(hf + 1) * DFF], I["w_up"][l][kc * 128:(kc + 1) * 128, hf * DFF:(hf + 1) * DFF], [], ["w_up"], "wl%d" % (j % 4))
        xt = [A([128, D], F32) for _ in range(3)]
        junk = A([128, D], BF16)
        junk2 = A([128, 512], BF16)
        ssa = [A([128, 1], F32) for _ in range(2)]
        rstda = [A([128, 1], F32) for _ in range(2)]
        ss = [A([128, 1], F32) for _ in range(2)]
        rstd = [A([128, 1], F32) for _ in range(2)]
        an = [A([128, 512], BF16) for _ in range(2)]
        mTa = [A([128, 4, 128], BF16) for _ in range(2)]
        pmt = [A([128, 4, 128], BF16) for _ in range(3)]
        tt = [A([128, D], F32) for _ in range(2)]
        YY = [[PB[0], PB[1]], [PB[2], PB[3]]]
        TA = PT[1]

        def m3_L(t):
            DMA("sync", xt[t % 3][:], x_src[t * 128:(t + 1) * 128, :], [], ["xt%d" % (t % 3)], "xl%d" % (t % 3))
            DMA("sync", pmt[t % 3][:].rearrange("p g t -> p (g t)"), pm_s[t], [], ["pmt%d" % (t % 3)], "pml%d" % (t % 3))

        def m3_A(t):
            b2 = t % 2
            E("scalar", "activation", ["ao_all"], ["junk2", "ssa%d" % b2], out=junk2[:], in_=ao_all[:, t, :], func=AF.Square, accum_out=ssa[b2][:])
            rstd_ops(ssa[b2][:], rstda[b2][:], 1, ["ssa%d" % b2], ["rstda%d" % b2], 512)
            E("vector", "tensor_scalar", ["ao_all", "rstda%d" % b2], ["an%d" % b2], out=an[b2][:], in0=ao_all[:, t, :], scalar1=rstda[b2][:, 0:1],
              scalar2=None, op0=ALU.mult)
            for c4 in range(4):
                E("tensor", "transpose", ["an%d" % b2, "identb"], ["TA"], out=TA[:, c4 * 128:(c4 + 1) * 128], in_=an[b2][:, c4 * 128:(c4 + 1) * 128],
                  identity=identb[:])
            for c4 in range(4):
                E("scalar", "activation", ["TA", "aog"], ["mTa%d" % b2], out=mTa[b2][:, c4, :], in_=TA[:, c4 * 128:(c4 + 1) * 128], func=AF.Identity,
                  scale=aog_col[:, c4:c4 + 1])

        def m3_B(t):
            b2 = t % 2
            xs = xt[t % 3]
            kx = "xt%d" % (t % 3)
            Y = YY[b2]
            kY = ["PB%d" % (2 * b2), "PB%d" % (2 * b2 + 1)]
            for nb in range(2):
                for c8 in range(8):
                    lhs = mTa[b2][:, c8, :] if c8 < 4 else pmt[t % 3][:, c8 - 4, :]
                    E("tensor", "matmul", ["mTa%d" % b2, "pmt%d" % (t % 3), "w_out"], [kY[nb]], out=Y[nb][:], lhsT=lhs,
                      rhs=w_out[:, c8, nb * 512:(nb + 1) * 512], start=(c8 == 0), stop=(c8 == 7))
            post_norm_residual(E, rstd_ops, Y, kY, junk, "junk", ss[b2], "ss%d" % b2, rstd[b2], "rstd%d" % b2, G2, "G2",
                               tt[b2], "tt%d" % b2, xs, kx, tt[b2], "tt%d" % b2)
            DMA("sync", x_mid[t * 128:(t + 1) * 128, :], tt[b2][:], ["tt%d" % b2], [], "xst%d" % b2)

        m3_L(0)
        m3_L(1)
        m3_A(0)
        for t in range(NT):
            if t + 2 < NT:
                m3_L(t + 2)
            if t + 1 < NT:
                m3_A(t + 1)
            if t % 2 == 0:
                wup_prefetch(t // 2)
            m3_B(t)
        if stop_after == "M3":
            break

        S.barrier()
        A.reset(persist_mark)
        A.limit = WUP_OFF
        w_dn = A([128, NPAIR, D], BF16)
        G4 = A([128, D], F32)
        xt = [A([128, D], F32) for _ in range(2)]
        junk = A([128, D], BF16)
        ssa = [A([128, 1], F32) for _ in range(2)]
        rstda = [A([128, 1], F32) for _ in range(2)]
        ss = [A([128, 1], F32) for _ in range(2)]
        rstd = [A([128, 1], F32) for _ in range(2)]
        xn = [A([128, D], BF16) for _ in range(2)]
        hTg = A([128, 8, 512], BF16)
        mT = A([128, NPAIR, 512], BF16)
        acc = [[A([128, 512], F32) for _ in range(2)] for _ in range(2)]
        usb = [[A([128, 514], F32) for _ in range(2)] for _ in range(2)]
        hist = A([128, NFC, 2], F32)
        tt = [A([128, D], F32) for _ in range(2)]
        def wdn_load(c4):
            DMA("gpsimd", w_dn[:, c4:c4 + 2, :], I["w_down"][l][c4 * 128:(c4 + 2) * 128, :].rearrange("(c p) n -> p c n", p=128), [], ["w_dn"], "wl%d" % ((c4 // 2) % 4))
        DMA("sync", G4[:], g_s[1], [], ["G4"], None)
        E("gpsimd", "memset", [], ["hist"], ap=hist[:], constant=0.0)
        TF = PT[0]
        UB = [[PB[0], PB[1]], [PB[2], PB[3]]]
        Y = [PB[4], PB[5]]
        xcnt = [0]

        def xload(t):
            sl = xcnt[0] % 2
            xcnt[0] += 1
            DMA("sync", xt[sl][:], x_mid[t * 128:(t + 1) * 128, :], [], ["xt%d" % sl], "xl%d" % sl)
            return xt[sl], "xt%d" % sl

        def f_A(g, ti):
            t = g * 4 + ti
            b2 = t % 2
            xs, kx = xload(t)
            E("scalar", "activation", [kx], ["junk", "ssa%d" % b2], out=junk[:], in_=xs[:], func=AF.Square, accum_out=ssa[b2][:])
            rstd_ops(ssa[b2][:], rstda[b2][:], 1, ["ssa%d" % b2], ["rstda%d" % b2], D)
            E("vector", "tensor_scalar", [kx, "rstda%d" % b2], ["xn%d" % b2], out=xn[b2][:], in0=xs[:], scalar1=rstda[b2][:, 0:1],
              scalar2=None, op0=ALU.mult)
            for kc in range(8):
                E("tensor", "transpose", ["xn%d" % b2, "identb"], ["TF"], out=TF[:, kc * 128:(kc + 1) * 128],
                  in_=xn[b2][:, kc * 128:(kc + 1) * 128], identity=identb[:])
            for kc in range(8):
                E("scalar", "activation", ["TF", "cols2", "cols3"], ["hTg"], out=hTg[:, kc, ti * 128:(ti + 1) * 128],
                  in_=TF[:, kc * 128:(kc + 1) * 128], func=AF.Identity, scale=cols[:, 2, kc:kc + 1], bias=cols[:, 3, kc:kc + 1])

        pidx = 0
        pend = []

        def silu_flush():
            while pend:
                pb_, i_ = pend.pop(0)
                ka0 = "acc%d0" % pb_
                ka1 = "acc%d1" % pb_
                E("scalar", "activation", [ka0], [ka0], out=acc[pb_][0][:], in_=acc[pb_][0][:], func=AF.Silu)
                E("vector", "tensor_tensor", [ka0, ka1], ["mT"], out=mT[:, i_, :], in0=acc[pb_][0][:], in1=acc[pb_][1][:], op=ALU.mult)

        for ti in range(4):
            f_A(0, ti)
        for g in range(8):
            for i in range(NPAIR):
                if g == 0 and i % 2 == 0:
                    wdn_load(i)
                pb = pidx % 2
                pidx += 1
                halves = ((0, i), (1, NPAIR + i))
                for half, ch in halves:
                    U = UB[pb][half]
                    kU = "PB%d" % (2 * pb + half)
                    for kc in range(8):
                        E("tensor", "matmul", ["hTg", "w_up"], [kU], out=U[:], lhsT=w_up[:, kc, ch * 128:(ch + 1) * 128], rhs=hTg[:, kc, :],
                          start=(kc == 0), stop=(kc == 7))
                for half, ch in halves:
                    U = UB[pb][half]
                    kU = "PB%d" % (2 * pb + half)
                    ub = usb[pb][half]
                    ku = "usb%d%d" % (pb, half)
                    E("gpsimd", "tensor_copy", ["hist%d" % ch], [ku + "h"], out=ub[:, 0:2], in_=hist[:, ch, :])
                    E("scalar", "activation", [kU], [ku], out=ub[:, 2:514], in_=U[:], func=AF.Copy)
                    E("scalar", "activation", [kU, "cv2", "cv3"], ["acc%d%d" % (pb, half)], out=acc[pb][half][:], in_=U[:], func=AF.Identity,
                      scale=cv_col[:, 2, ch:ch + 1], bias=cv_col[:, 3, ch:ch + 1])
                    if g < 7:
                        E("gpsimd", "tensor_copy", [ku], ["hist%d" % ch], out=hist[:, ch, :], in_=ub[:, 512:514])
                silu_flush()
                for half, ch in halves:
                    ub = usb[pb][half]
                    ku = "usb%d%d" % (pb, half)
                    ac = acc[pb][half]
                    ka = "acc%d%d" % (pb, half)
                    E("vector", "scalar_tensor_tensor", [ku, ku + "h", ka, "cv1"], [ka], out=ac[:], in0=ub[:, 1:513], scalar=cv_col[:, 1, ch:ch + 1],
                      in1=ac[:], op0=ALU.mult, op1=ALU.add)
                for half, ch in halves:
                    ub = usb[pb][half]
                    ku = "usb%d%d" % (pb, half)
                    ac = acc[pb][half]
                    ka = "acc%d%d" % (pb, half)
                    E("vector", "scalar_tensor_tensor", [ku, ku + "h", ka, "cv0"], [ka], out=ac[:], in0=ub[:, 0:512], scalar=cv_col[:, 0, ch:ch + 1],
                      in1=ac[:], op0=ALU.mult, op1=ALU.add)
                pend.append((pb, i))
            silu_flush()
            for ti in range(4):
                t = g * 4 + ti
                b2 = t % 2
                for nb in range(2):
                    for i in range(NPAIR):
                        E("tensor", "matmul", ["mT", "w_dn"], ["PB%d" % (4 + nb)], out=Y[nb][:], lhsT=mT[:, i, ti * 128:(ti + 1) * 128],
                          rhs=w_dn[:, i, nb * 512:(nb + 1) * 512], start=(i == 0), stop=(i == NPAIR - 1))
                if g + 1 < 8:
                    f_A(g + 1, ti)
                xs, kx = xload(t)
                post_norm_residual(E, rstd_ops, Y, ["PB4", "PB5"], junk, "junk", ss[b2], "ss%d" % b2, rstd[b2], "rstd%d" % b2, G4, "G4",
                                   tt[b2], "tt%d" % b2, xs, kx, tt[b2], "tt%d" % b2)
                DMA("sync", x_dst[t * 128:(t + 1) * 128, :], tt[b2][:], ["tt%d" % b2], [], "xst%d" % b2)
        x_src = x_dst

    final = [k for k in S.dma_counts.keys()]
    nsem = S.emit(final_wait_semkeys=final)
    return nc


def post_norm_residual(E, rstd_ops, Y, kY, junk, kjunk, ss, kss, rstd, krstd, G, kG, tt, ktt, xs, kx, xo, kxo):
    ssb = ss
    E("scalar", "activation", [kY[0]], [kjunk, kss, "lk" + kY[0]], out=junk[:, 0:512], in_=Y[0][:], func=AF.Square, accum_out=ssb[:])
    E("vector", "tensor_copy", [kY[0], "lk" + kY[0]], [ktt], out=tt[:, 0:512], in_=Y[0][:])
    E("scalar", "activation", [kY[1]], [kjunk, krstd, "lk" + kY[1]], out=junk[:, 512:1024], in_=Y[1][:], func=AF.Square, accum_out=rstd[:])
    E("vector", "tensor_copy", [kY[1], "lk" + kY[1]], [ktt], out=tt[:, 512:1024], in_=Y[1][:])
    E("vector", "tensor_tensor", [kss, krstd], [kss], out=ssb[:], in0=ssb[:], in1=rstd[:], op=ALU.add)
    rstd_ops(ssb[:], rstd[:], 1, [kss], [krstd], D)
    E("vector", "scalar_tensor_tensor", [ktt, krstd, kG], [ktt], out=tt[:], in0=tt[:], scalar=rstd[:, 0:1],
      in1=G[:], op0=ALU.mult, op1=ALU.mult)
    E("gpsimd", "tensor_tensor", [ktt, kx], [kxo], out=xo[:], in0=tt[:], in1=xs[:], op=ALU.add)


_CONSTS = None


def make_in_maps(inputs):
    global _CONSTS
    if _CONSTS is None:
        _CONSTS = _const_tables()
    x = np.asarray(inputs["x"], dtype=np.float32)
    c = np.asarray(inputs["c"], dtype=np.float32)
    shared = {k: np.ascontiguousarray(np.asarray(inputs[k], dtype=np.float32)) for k in WEIGHT_SPECS if k != "c"}
    maps = []
    for b in range(8):
        m = {"x": np.ascontiguousarray(x[b]), "c": np.ascontiguousarray(c[b:b + 1])}
        m.update(shared)
        m.update(_CONSTS)
        maps.append(m)
    return maps


def kernel(**inputs):
    nc = build()
    maps = make_in_maps(inputs)
    res = run_bass_kernel_spmd(nc, maps, core_ids=list(range(8)))
    return np.stack([np.asarray(r["out"]) for r in res.results], axis=0).astype(np.float32)
```
